# Optimizing a Trainium2 kernel written in Bass

```python
import math
import jax, jax.numpy as jnp
from jax import lax
import numpy as np

D_MODEL = 1024
BATCH = 8
SEQ = 8192
DEPTH = 4

GRID_W = 64
CTX_LEN = 256
HEAD_DIM = 64
Q_BLOCK = 128
ROPE_THETA = 10000.0
EPS = 1e-6
N_MOD = 6

A_HEADS = D_MODEL // 256
A_VDIM = 2 * HEAD_DIM
B_HEADS = D_MODEL // 128
B_KV_HEADS = B_HEADS // 4
B_GROUP = B_HEADS // B_KV_HEADS
A_Q = A_HEADS * 2 * HEAD_DIM
A_K = A_HEADS * 2 * HEAD_DIM
A_V = A_HEADS * A_VDIM
B_Q = B_HEADS * HEAD_DIM
B_K = B_KV_HEADS * HEAD_DIM
B_V = B_KV_HEADS * HEAD_DIM
EVEN_SPLITS = [A_Q, A_Q + A_K, A_Q + A_K + A_V, A_Q + A_K + A_V + B_Q, A_Q + A_K + A_V + B_Q + B_K]
EVEN_IN = A_Q + A_K + A_V + B_Q + B_K + B_V
A_OUT = A_HEADS * A_VDIM
B_OUT = B_HEADS * HEAD_DIM

MLA_HEADS = D_MODEL // 64
MLA_NOPE = 64
MLA_ROPE = 32
MLA_VDIM = 64
MLA_Q_RANK = D_MODEL // 4
MLA_KV_RANK = D_MODEL // 8
ODD_IN = MLA_Q_RANK + MLA_KV_RANK + MLA_ROPE
MLA_OUT = MLA_HEADS * MLA_VDIM

N_EXPERTS = 16
EC_CAPACITY_FACTOR = 2
D_EXPERT = D_MODEL

N_EVEN = (DEPTH + 1) // 2
N_ODD = DEPTH // 2

kernel_name = 'hybrid_diffattn_gqa_mla_ec_moe_prefix_trunk'


def rmsnorm(x, g):
    xf = x.astype(jnp.float32)
    y = xf * lax.rsqrt(jnp.mean(xf * xf, axis=-1, keepdims=True) + EPS)
    return y.astype(x.dtype) * g


def modulate(h, shift, scale):
    return h * (1 + scale) + shift


def grid_angles(n_tokens, rot_dim):
    rows = n_tokens // GRID_W
    row = jnp.repeat(jnp.arange(rows, dtype=jnp.float32), GRID_W)
    col = jnp.tile(jnp.arange(GRID_W, dtype=jnp.float32), rows)
    half = rot_dim // 2
    freqs = ROPE_THETA ** (-jnp.arange(0, half, 2, dtype=jnp.float32) / half)
    return row[:, None] * freqs, col[:, None] * freqs


def rope_1d(x, ang):
    ang = ang.reshape((ang.shape[0],) + (1,) * (x.ndim - 3) + (ang.shape[1],))
    cos = jnp.cos(ang).astype(x.dtype)
    sin = jnp.sin(ang).astype(x.dtype)
    x1, x2 = jnp.split(x, 2, axis=-1)
    return jnp.concatenate([x1 * cos - x2 * sin, x2 * cos + x1 * sin], axis=-1)


def rope_2d(x, ang_row, ang_col):
    xr, xc = jnp.split(x, 2, axis=-1)
    return jnp.concatenate([rope_1d(xr, ang_row), rope_1d(xc, ang_col)], axis=-1)


def sweep_query_blocks(fn, *qs):
    bsz, t = qs[0].shape[:2]
    nb = t // Q_BLOCK
    blocks = tuple(jnp.moveaxis(q.reshape((bsz, nb, Q_BLOCK) + q.shape[2:]), 1, 0) for q in qs)
    out = lax.map(lambda qb: fn(*qb), blocks)
    return jnp.moveaxis(out, 0, 1).reshape((bsz, t) + out.shape[3:])


def diff_attend(q, k, v, lam, scale):
    s = jnp.einsum('bqhcd,bkhcd->bhcqk', q, k).astype(jnp.float32) * scale
    p = jax.nn.softmax(s, axis=-1)
    w = (p[:, :, 0] - lam * p[:, :, 1]).astype(v.dtype)
    return jnp.einsum('bhqk,bkhd->bqhd', w, v)


def gqa_attend(q, k, v, scale):
    s = jnp.einsum('bqhgd,bkhd->bhgqk', q, k).astype(jnp.float32) * scale
    p = jax.nn.softmax(s, axis=-1).astype(v.dtype)
    return jnp.einsum('bhgqk,bkhd->bqhgd', p, v)


def mla_attend(qn, qr, kn, kr, v, scale):
    s = jnp.einsum('bqhd,bkhd->bhqk', qn, kn) + jnp.einsum('bqhr,bkr->bhqk', qr, kr)
    p = jax.nn.softmax(s.astype(jnp.float32) * scale, axis=-1).astype(v.dtype)
    return jnp.einsum('bhqk,bkhd->bqhd', p, v)


def even_project(h, w_in, a_qk, b_qk):
    bsz, t = h.shape[:2]
    qa, ka, va, qb, kb, vb = jnp.split(h @ w_in, EVEN_SPLITS, axis=-1)
    qa = rmsnorm(qa.reshape(bsz, t, A_HEADS, 2, HEAD_DIM), a_qk[0])
    ka = rmsnorm(ka.reshape(bsz, t, A_HEADS, 2, HEAD_DIM), a_qk[1])
    va = va.reshape(bsz, t, A_HEADS, A_VDIM)
    qb = rmsnorm(qb.reshape(bsz, t, B_KV_HEADS, B_GROUP, HEAD_DIM), b_qk[0])
    kb = rmsnorm(kb.reshape(bsz, t, B_KV_HEADS, HEAD_DIM), b_qk[1])
    vb = vb.reshape(bsz, t, B_KV_HEADS, HEAD_DIM)
    return qa, ka, va, qb, kb, vb


def even_mixer(h_lat, h_ctx, w_in, a_qk, lam_vecs, a_subln, b_qk, w_out, layer_idx, ctx_out):
    lam_init = 0.8 - 0.6 * math.exp(-0.3 * layer_idx)
    lv = lam_vecs.astype(jnp.float32)
    lam = jnp.exp(jnp.sum(lv[0] * lv[1])) - jnp.exp(jnp.sum(lv[2] * lv[3])) + lam_init
    scale = HEAD_DIM ** -0.5
    qa_l, ka_l, va_l, qb_l, kb_l, vb_l = even_project(h_lat, w_in, a_qk, b_qk)
    qa_c, ka_c, va_c, qb_c, kb_c, vb_c = even_project(h_ctx, w_in, a_qk, b_qk)
    ang_row, ang_col = grid_angles(h_lat.shape[1], HEAD_DIM)
    qa_l, ka_l, qb_l, kb_l = (rope_2d(t, ang_row, ang_col) for t in (qa_l, ka_l, qb_l, kb_l))
    ka = jnp.concatenate([ka_l, ka_c], axis=1)
    va = jnp.concatenate([va_l, va_c], axis=1)
    kb = jnp.concatenate([kb_l, kb_c], axis=1)
    vb = jnp.concatenate([vb_l, vb_c], axis=1)
    oa_l = sweep_query_blocks(lambda q: diff_attend(q, ka, va, lam, scale), qa_l)
    ob_l = sweep_query_blocks(lambda q: gqa_attend(q, kb, vb, scale), qb_l)

    def merge(oa, ob):
        bsz, t = oa.shape[:2]
        oa = rmsnorm(oa, a_subln) * (1.0 - lam_init)
        return jnp.concatenate([oa.reshape(bsz, t, A_OUT), ob.reshape(bsz, t, B_OUT)], axis=-1) @ w_out

    y_lat = merge(oa_l, ob_l)
    y_ctx = None
    if ctx_out:
        y_ctx = merge(diff_attend(qa_c, ka_c, va_c, lam, scale), gqa_attend(qb_c, kb_c, vb_c, scale))
    return y_lat, y_ctx


def mla_project(h, w_in, q_norm, w_q_up, kv_norm, w_kv_up, qk_norm):
    bsz, t = h.shape[:2]
    cq, ckv, kr = jnp.split(h @ w_in, [MLA_Q_RANK, MLA_Q_RANK + MLA_KV_RANK], axis=-1)
    q = (rmsnorm(cq, q_norm) @ w_q_up).reshape(bsz, t, MLA_HEADS, MLA_NOPE + MLA_ROPE)
    kv = (rmsnorm(ckv, kv_norm) @ w_kv_up).reshape(bsz, t, MLA_HEADS, MLA_NOPE + MLA_VDIM)
    qn = rmsnorm(q[..., :MLA_NOPE], qk_norm[0, :MLA_NOPE])
    qr = rmsnorm(q[..., MLA_NOPE:], qk_norm[0, MLA_NOPE:])
    kn = rmsnorm(kv[..., :MLA_NOPE], qk_norm[1, :MLA_NOPE])
    v = kv[..., MLA_NOPE:]
    kr = rmsnorm(kr, qk_norm[1, MLA_NOPE:])
    return qn, qr, kn, kr, v


def odd_mixer(h_lat, h_ctx, w_in, q_norm, w_q_up, kv_norm, w_kv_up, qk_norm, w_out, ctx_out):
    scale = (MLA_NOPE + MLA_ROPE) ** -0.5
    qn_l, qr_l, kn_l, kr_l, v_l = mla_project(h_lat, w_in, q_norm, w_q_up, kv_norm, w_kv_up, qk_norm)
    qn_c, qr_c, kn_c, kr_c, v_c = mla_project(h_ctx, w_in, q_norm, w_q_up, kv_norm, w_kv_up, qk_norm)
    ang_row, ang_col = grid_angles(h_lat.shape[1], MLA_ROPE)
    qr_l = rope_2d(qr_l, ang_row, ang_col)
    kr_l = rope_2d(kr_l, ang_row, ang_col)
    kn = jnp.concatenate([kn_l, kn_c], axis=1)
    kr = jnp.concatenate([kr_l, kr_c], axis=1)
    v = jnp.concatenate([v_l, v_c], axis=1)
    o_l = sweep_query_blocks(lambda a, b: mla_attend(a, b, kn, kr, v, scale), qn_l, qr_l)
    bsz, t = h_lat.shape[:2]
    y_lat = o_l.reshape(bsz, t, MLA_OUT) @ w_out
    y_ctx = None
    if ctx_out:
        o_c = mla_attend(qn_c, qr_c, kn_c, kr_c, v_c, scale)
        y_ctx = o_c.reshape(bsz, h_ctx.shape[1], MLA_OUT) @ w_out
    return y_lat, y_ctx


def ec_moe(h, w_router, w_gate, w_up, w_down):
    bsz, t, d = h.shape
    cap = EC_CAPACITY_FACTOR * t // N_EXPERTS
    aff = jax.nn.softmax(jnp.einsum('btd,de->bte', h, w_router).astype(jnp.float32), axis=-1)
    gates, idx = lax.top_k(jnp.swapaxes(aff, 1, 2), cap)
    xs = jax.vmap(lambda hb, ib: hb[ib])(h, idx)
    a = jnp.einsum('becd,edf->becf', xs, w_gate)
    u = jnp.einsum('becd,edf->becf', xs, w_up)
    y = jnp.einsum('becf,efd->becd', jax.nn.silu(a) * u, w_down)
    y = y * gates[..., None].astype(y.dtype)
    return jax.vmap(lambda yb, ib: jnp.zeros((t, d), y.dtype).at[ib.reshape(-1)].add(yb.reshape(-1, d)))(y, idx)


def setup_inputs(seed: int = 0) -> dict:
    key = jax.random.key(seed)
    ks = iter(jax.random.split(key, 32))

    def nrm(shape, scale):
        return jax.random.normal(next(ks), shape, jnp.float32) * scale

    def gain(shape):
        return 1.0 + nrm(shape, 0.1)

    D = D_MODEL
    return {
        'x': nrm((BATCH, SEQ, D), 1.0),
        'c': nrm((BATCH, D), 1.0),
        'ctx': nrm((BATCH, CTX_LEN, D), 1.0),
        'c_ctx': nrm((D,), 1.0),
        'w_ada': nrm((DEPTH, D, N_MOD * D), 0.5 * D ** -0.5),
        'b_ada': nrm((DEPTH, N_MOD * D), 0.01),
        'norm_mix': gain((DEPTH, D)),
        'norm_ffn': gain((DEPTH, D)),
        'w_in_even': nrm((N_EVEN, D, EVEN_IN), D ** -0.5),
        'a_qk_norm': gain((N_EVEN, 2, HEAD_DIM)),
        'diff_lambda': nrm((N_EVEN, 4, HEAD_DIM), 0.1),
        'a_subln': gain((N_EVEN, A_VDIM)),
        'b_qk_norm': gain((N_EVEN, 2, HEAD_DIM)),
        'w_out_even': nrm((N_EVEN, A_OUT + B_OUT, D), (A_OUT + B_OUT) ** -0.5),
        'w_in_odd': nrm((N_ODD, D, ODD_IN), D ** -0.5),
        'mla_q_norm': gain((N_ODD, MLA_Q_RANK)),
        'w_q_up': nrm((N_ODD, MLA_Q_RANK, MLA_HEADS * (MLA_NOPE + MLA_ROPE)), MLA_Q_RANK ** -0.5),
        'mla_kv_norm': gain((N_ODD, MLA_KV_RANK)),
        'w_kv_up': nrm((N_ODD, MLA_KV_RANK, MLA_HEADS * (MLA_NOPE + MLA_VDIM)), MLA_KV_RANK ** -0.5),
        'mla_qk_norm': gain((N_ODD, 2, MLA_NOPE + MLA_ROPE)),
        'w_out_odd': nrm((N_ODD, MLA_OUT, D), MLA_OUT ** -0.5),
        'w_router': nrm((DEPTH, D, N_EXPERTS), D ** -0.5),
        'w_exp_gate': nrm((DEPTH, N_EXPERTS, D, D_EXPERT), D ** -0.5),
        'w_exp_up': nrm((DEPTH, N_EXPERTS, D, D_EXPERT), D ** -0.5),
        'w_exp_down': nrm((DEPTH, N_EXPERTS, D_EXPERT, D), D_EXPERT ** -0.5),
    }


def reference(x, c, ctx, c_ctx, w_ada, b_ada, norm_mix, norm_ffn, w_in_even, a_qk_norm, diff_lambda,
              a_subln, b_qk_norm, w_out_even, w_in_odd, mla_q_norm, w_q_up, mla_kv_norm, w_kv_up,
              mla_qk_norm, w_out_odd, w_router, w_exp_gate, w_exp_up, w_exp_down):
    bsz = x.shape[0]
    sc = jax.nn.silu(c)
    sc_ctx = jax.nn.silu(c_ctx)
    for i in range(DEPTH):
        last = i == DEPTH - 1
        m = (sc @ w_ada[i] + b_ada[i]).reshape(bsz, 1, N_MOD, D_MODEL)
        mc = (sc_ctx @ w_ada[i] + b_ada[i]).reshape(N_MOD, D_MODEL)
        h_lat = modulate(rmsnorm(x, norm_mix[i]), m[:, :, 0], m[:, :, 1])
        h_ctx = modulate(rmsnorm(ctx, norm_mix[i]), mc[0], mc[1])
        if i % 2 == 0:
            j = i // 2
            y_lat, y_ctx = even_mixer(h_lat, h_ctx, w_in_even[j], a_qk_norm[j], diff_lambda[j], a_subln[j],
                                      b_qk_norm[j], w_out_even[j], i, not last)
        else:
            j = i // 2
            y_lat, y_ctx = odd_mixer(h_lat, h_ctx, w_in_odd[j], mla_q_norm[j], w_q_up[j], mla_kv_norm[j],
                                     w_kv_up[j], mla_qk_norm[j], w_out_odd[j], not last)
        x = x + m[:, :, 2] * y_lat
        h_lat = modulate(rmsnorm(x, norm_ffn[i]), m[:, :, 3], m[:, :, 4])
        x = x + m[:, :, 5] * ec_moe(h_lat, w_router[i], w_exp_gate[i], w_exp_up[i], w_exp_down[i])
        if not last:
            ctx = ctx + mc[2] * y_ctx
            h_ctx = modulate(rmsnorm(ctx, norm_ffn[i]), mc[3], mc[4])
            ctx = ctx + mc[5] * ec_moe(h_ctx, w_router[i], w_exp_gate[i], w_exp_up[i], w_exp_down[i])
    return x
```

```python
import math
from contextlib import ExitStack
import numpy as np
import concourse.bass as bass
import concourse.mybir as mybir
from concourse.bass_utils import run_bass_kernel_spmd

F32 = mybir.dt.float32
BF16 = mybir.dt.bfloat16
I32 = mybir.dt.int32
AF = mybir.ActivationFunctionType
ALU = mybir.AluOpType
AX = mybir.AxisListType

D = 1024
NE = 16
EPS = 1e-6
GRID_W = 64
ROPE_THETA = 10000.0


class Cfg:
    def __init__(self, T=8192, C=256, depth=4):
        self.T, self.C, self.L = T, C, depth
        self.TA = T + C
        self.NTL, self.NTC = T // 128, C // 128
        self.NT = self.NTL + self.NTC
        self.capL = 2 * T // NE
        self.capC = 2 * C // NE
        self.SLOTS = self.capL + self.capC
        self.n_even = (depth + 1) // 2
        self.n_odd = depth // 2
        self.QB = 512


class Buf:
    __slots__ = ("name", "w", "r", "sid")

    def __init__(self, name):
        self.name, self.w, self.r, self.sid = name, None, {}, None


class Eng:
    def __init__(self, e, sid):
        self.e, self.sid, self.waited = e, sid, {}


class KB:
    def __init__(self, cfg):
        self.cfg = cfg
        nc = self.nc = bass.Bass("TRN2", target_bir_lowering=False)
        self.sem = {}
        self.E = {}
        for n, e in (("pe", nc.tensor), ("act", nc.scalar), ("dve", nc.vector),
                     ("pool", nc.gpsimd), ("sp", nc.sync)):
            self.E[n] = Eng(e, self.newsem("e_" + n))
        self.bufs = {}
        self.uid = 0
        self.free_sids = {}

    def dsem(self, q, name):
        fl = self.free_sids.get(q)
        if fl:
            return fl.pop()
        return self.newsem(name)

    def _release(self, nm):
        b = self.bufs.pop(nm, None)
        if b is not None and b.sid:
            for q, sid in b.sid.items():
                self.free_sids.setdefault(q, []).append(sid)

    def newsem(self, name):
        sid = len(self.sem)
        self.sem[sid] = [self.nc.alloc_semaphore(name), 0]
        return sid

    def token(self, name):
        b = Buf(name)
        self.bufs[name] = b
        return b

    def _wait(self, E, sid, val):
        if E.waited.get(sid, 0) < val:
            E.e.wait_ge(self.sem[sid][0], val)
            E.waited[sid] = val

    def _deps(self, E, rb, wb, is_pe):
        need = {}
        for b in rb:
            if b.w is not None:
                need[b.w[0]] = max(need.get(b.w[0], 0), b.w[1])
        for b in wb:
            if b.w is not None:
                need[b.w[0]] = max(need.get(b.w[0], 0), b.w[1])
            for sid, v in b.r.items():
                need[sid] = max(need.get(sid, 0), v)
        for sid, v in need.items():
            if is_pe and sid == E.sid:
                continue
            self._wait(E, sid, v)

    def _mark(self, rb, wb, mark):
        sid, v = mark
        for b in rb:
            if b.r.get(sid, 0) < v:
                b.r[sid] = v
        for b in wb:
            b.w = mark
            b.r = {}

    def _bufs_of(self, aps, extra):
        out = []
        for a in aps:
            if a is None or isinstance(a, (int, float)):
                continue
            b = self.bufs.get(a.name)
            if b is not None and b not in out:
                out.append(b)
        for b in extra:
            if b not in out:
                out.append(b)
        return out

    def op(self, en, fn, outs, ins, xr=(), xw=()):
        E = self.E[en]
        rb = self._bufs_of(ins, xr)
        wb = self._bufs_of(outs, xw)
        self._deps(E, rb, wb, en == "pe")
        ins_ = fn(E.e)
        s = self.sem[E.sid]
        s[1] += 1
        ins_.then_inc(s[0], 1)
        self._mark(rb, wb, (E.sid, s[1]))

    def dma(self, q, pairs, xr=(), xw=(), sembuf=None, **kw):
        E = self.E[q]
        outs = [p[0] for p in pairs]
        ins = [p[1] for p in pairs]
        rb = self._bufs_of(ins, xr)
        wb = self._bufs_of(outs, xw)
        sb = sembuf
        if sb is None:
            cand = self._bufs_of(outs, ()) or self._bufs_of(ins, ())
            sb = cand[0]
        if sb.sid is None:
            sb.sid = {}
        if q not in sb.sid:
            sb.sid[q] = self.dsem(q, "d_%s_%s" % (q, sb.name))
        self._deps(E, rb, wb, False)
        s = self.sem[sb.sid[q]]
        for o, i in pairs:
            E.e.dma_start(out=o, in_=i, **kw).then_inc(s[0], 16)
            s[1] += 16
        self._mark(rb, wb, (sb.sid[q], s[1]))

    def idma(self, out, in_, idx_ap, scatter, bounds, add=False, xr=(), xw=()):
        E = self.E["pool"]
        rb = self._bufs_of([in_, idx_ap], xr)
        wb = self._bufs_of([out], xw)
        cand = self._bufs_of([in_] if scatter else [out], ())
        sb = cand[0]
        if sb.sid is None:
            sb.sid = {}
        if "ind" not in sb.sid:
            sb.sid["ind"] = self.dsem("ind", "d_ind_" + sb.name)
        self._deps(E, rb, wb, False)
        s = self.sem[sb.sid["ind"]]
        off = bass.IndirectOffsetOnAxis(ap=idx_ap, axis=0)
        kw = {}
        if add:
            kw["compute_op"] = ALU.add
            kw["oob_is_err"] = True
        else:
            kw["oob_is_err"] = False
        E.e.indirect_dma_start(out=out, out_offset=off if scatter else None, in_=in_,
                               in_offset=None if scatter else off,
                               bounds_check=bounds, **kw).then_inc(s[0], 16)
        s[1] += 16
        self._mark(rb, wb, (sb.sid["ind"], s[1]))

    def barrier(self):
        for E in self.E.values():
            for sid, (h, c) in self.sem.items():
                if c > 0:
                    self._wait(E, sid, c)
        for b in self.bufs.values():
            b.w, b.r = None, {}

    def sb(self, st, name, shape, dt):
        self.uid += 1
        nm = "%s_%d" % (name, self.uid)
        t = st.enter_context(self.nc.sbuf_tensor(nm, list(shape), dt))
        self.bufs[nm] = Buf(nm)
        st.callback(self._release, nm)
        return t

    def ps(self, st, name, shape, dt=F32):
        self.uid += 1
        nm = "%s_%d" % (name, self.uid)
        t = st.enter_context(self.nc.psum_tensor(nm, list(shape), dt))
        self.bufs[nm] = Buf(nm)
        st.callback(self._release, nm)
        return t

    def mm(self, out, lhsT, rhs, start=True, stop=True):
        self.op("pe", lambda e: e.matmul(out, lhsT, rhs, start=start, stop=stop), [out], [lhsT, rhs])

    def tr(self, out, in_, ident):
        self.op("pe", lambda e: e.transpose(out, in_, ident), [out], [in_, ident])

    def act(self, out, in_, func, scale=1.0, bias=0.0, accum=None, en="act"):
        kw = {}
        if accum is not None:
            kw["accum_out"] = accum
        self.op("act", lambda e: e.activation(out=out, in_=in_, func=func, bias=bias, scale=scale, **kw),
                [out, accum], [in_, bias, scale])

    def tt(self, out, in0, in1, op, en="dve"):
        self.op(en, lambda e: e.tensor_tensor(out=out, in0=in0, in1=in1, op=op), [out], [in0, in1])

    def ts(self, out, in0, s1, s2, op0, op1=None, en="dve"):
        if op1 is None:
            self.op(en, lambda e: e.tensor_scalar(out=out, in0=in0, scalar1=s1, scalar2=None, op0=op0),
                    [out], [in0, s1])
        else:
            self.op(en, lambda e: e.tensor_scalar(out=out, in0=in0, scalar1=s1, scalar2=s2, op0=op0, op1=op1),
                    [out], [in0, s1, s2])

    def stt(self, out, in0, sc, in1, op0, op1, en="dve"):
        self.op(en, lambda e: e.scalar_tensor_tensor(out=out, in0=in0, scalar=sc, in1=in1, op0=op0, op1=op1),
                [out], [in0, sc, in1])

    def red(self, out, in_, op=ALU.add, en="dve"):
        self.op(en, lambda e: e.tensor_reduce(out=out, in_=in_, axis=AX.X, op=op), [out], [in_])

    def cp(self, out, in_, en="dve"):
        if en == "act":
            self.op("act", lambda e: e.activation(out=out, in_=in_, func=AF.Copy), [out], [in_])
        else:
            self.op(en, lambda e: e.tensor_copy(out=out, in_=in_), [out], [in_])

    def recip(self, out, in_):
        self.op("dve", lambda e: e.reciprocal(out=out, in_=in_), [out], [in_])

    def memset(self, ap, v, en="dve"):
        self.op(en, lambda e: e.memset(ap, v), [ap], [])

    def rstd(self, out, ss, inv_n):
        self.act(out, ss, AF.Sqrt, scale=inv_n, bias=EPS)
        self.recip(out, out)


def build(cfg):
    k = KB(cfg)
    nc = k.nc
    T, C, TA, L, NT, NTL, NTC = cfg.T, cfg.C, cfg.TA, cfg.L, cfg.NT, cfg.NTL, cfg.NTC
    SLOTS, capL, capC = cfg.SLOTS, cfg.capL, cfg.capC
    ne, no = cfg.n_even, cfg.n_odd

    def din(name, shape, dt=F32):
        return nc.dram_tensor(name, list(shape), dt, kind="ExternalInput").ap()

    def dint(name, shape, dt):
        return nc.dram_tensor(name, list(shape), dt, kind=("ExternalOutput" if getattr(cfg, "debug", False) else "Internal")).ap()

    x_in = din("x", [T, D]); ctx_in = din("ctx", [C, D]); cc_in = din("cc", [128, 8, 2])
    w_ada = din("w_ada", [L, D, 6 * D]); b_ada = din("b_ada", [L, 6 * D])
    norm_mix = din("norm_mix", [L, D]); norm_ffn = din("norm_ffn", [L, D])
    w_in_even = din("w_in_even", [ne, D, 2304]); a_qk = din("a_qk_norm", [ne, 2, 64])
    dlam = din("diff_lambda", [ne, 4, 64]); a_subln = din("a_subln", [ne, 128])
    b_qk = din("b_qk_norm", [ne, 2, 64]); w_out_even = din("w_out_even", [ne, D, D])
    if no:
        w_in_odd = din("w_in_odd", [no, D, 416]); q_norm = din("mla_q_norm", [no, 256])
        w_q_up = din("w_q_up", [no, 256, 1536]); kv_norm = din("mla_kv_norm", [no, 128])
        w_kv_up = din("w_kv_up", [no, 128, 2048]); qk_norm = din("mla_qk_norm", [no, 2, 96])
        w_out_odd = din("w_out_odd", [no, D, D])
    w_router = din("w_router", [L, D, NE])
    w_eg = din("w_exp_gate", [L, NE, D, D]); w_eu = din("w_exp_up", [L, NE, D, D])
    w_ed = din("w_exp_down", [L, NE, D, D])
    k_ident = din("k_ident", [128, 128]); k_tri = din("k_tri", [128, 128])
    k_tokidx = din("k_tokidx", [128, NT], I32)
    k_cos64 = din("k_cos64", [T, 64]); k_sin64 = din("k_sin64", [T, 64])
    k_cos32 = din("k_cos32", [T, 32]); k_sin32 = din("k_sin32", [T, 32])
    k_base = din("k_base", [128, NT * NE]); k_capv = din("k_capv", [128, 2 * NE])
    out_d = nc.dram_tensor("out", [T, D], F32, kind="ExternalOutput").ap()

    X = dint("X", [TA, D], F32)
    MODS = dint("MODS", [L, 2, 6 * D], F32)
    QKT = dint("QKT", [14, 128, TA], BF16)
    QTo = dint("QTo", [16, 96, TA], BF16)
    KTo = dint("KTo", [16, 96, TA], BF16)
    Vd = dint("Vd", [TA, D], BF16)
    OT = dint("OT", [D, TA], BF16)
    Hd = dint("Hd", [TA, D + 2], BF16)
    XS = dint("XS", [NE * SLOTS + 128, D + 2], BF16)
    BIG = float(NE * SLOTS + 4096)
    reg_xs = nc.gpsimd.alloc_register("bnd_xs")
    nc.gpsimd.reg_mov(reg_xs, NE * SLOTS - 1)
    reg_x = nc.gpsimd.alloc_register("bnd_x")
    nc.gpsimd.reg_mov(reg_x, TA - 1)

    gst = ExitStack()
    ident_f = k.sb(gst, "identf", [128, 128], F32)
    ident_b = k.sb(gst, "identb", [128, 128], BF16)
    tri_f = k.sb(gst, "trif", [128, 128], F32)
    tri_b = k.sb(gst, "trib", [128, 128], BF16)
    ones_f = k.sb(gst, "onesf", [128, 128], F32)
    ones_b = k.sb(gst, "onesb", [128, 128], BF16)
    tokidx = k.sb(gst, "tokidx", [128, NT], I32)
    xtok = k.token("Xtok")

    k.dma("sp", [(ident_f[:, :], k_ident)])
    k.dma("sp", [(tri_f[:, :], k_tri)])
    k.dma("sp", [(tokidx[:, :], k_tokidx)])
    k.cp(ident_b[:, :], ident_f[:, :])
    k.cp(tri_b[:, :], tri_f[:, :])
    k.memset(ones_f[:, :], 1.0)
    k.memset(ones_b[:, :], 1.0)
    k.dma("sp", [(X[0:T, :], x_in), (X[T:TA, :], ctx_in)], sembuf=xtok)

    with ExitStack() as st:
        cc = k.sb(st, "cc", [128, 8, 2], F32)
        k.dma("sp", [(cc[:, :, :], cc_in)])
        k.act(cc[:, :, :], cc[:, :, :], AF.Silu)
        wbuf = [k.sb(st, "wada", [128, 8, 512], F32) for _ in range(2)]
        mrow = k.sb(st, "mrow", [2, 6 * D], F32)
        bias = k.sb(st, "bias", [2, 6 * D], F32)
        gg = k.sb(st, "gg", [2, 2, D], F32)
        pm = [k.ps(st, "pm", [128, 512]) for _ in range(2)]
        for li in range(L):
            k.dma("sp", [(bias[0:1, :], b_ada[li:li + 1, :]), (bias[1:2, :], b_ada[li:li + 1, :])])
            k.dma("sp", [(gg[0:1, 0, :], norm_mix[li:li + 1, :]), (gg[1:2, 0, :], norm_mix[li:li + 1, :]),
                         (gg[0:1, 1, :], norm_ffn[li:li + 1, :]), (gg[1:2, 1, :], norm_ffn[li:li + 1, :])])
            for n in range(12):
                wb = wbuf[n % 2]
                k.dma("sp", [(wb[:, :, :], w_ada[li, :, n * 512:(n + 1) * 512].rearrange("(c p) n -> p c n", p=128))])
                for c in range(8):
                    k.mm(pm[n % 2][0:2, :], cc[:, c, :], wb[:, c, :], start=(c == 0), stop=(c == 7))
                k.tt(mrow[:, n * 512:(n + 1) * 512], pm[n % 2][0:2, :], bias[:, n * 512:(n + 1) * 512], ALU.add)
            k.stt(mrow[:, D:2 * D], mrow[:, D:2 * D], 1.0, gg[:, 0, :], ALU.add, ALU.mult)
            k.stt(mrow[:, 4 * D:5 * D], mrow[:, 4 * D:5 * D], 1.0, gg[:, 1, :], ALU.add, ALU.mult)
            k.dma("sp", [(MODS[li], mrow[:, :])])
        k.barrier()

    def bc_load(dst, src_row):
        k.dma("sp", [(dst, src_row.partition_broadcast(128))])

    def mod_tiles(st, li, slots):
        res = {}
        for s in slots:
            res[s] = []
            for r in range(2):
                t = k.sb(st, "mod%d" % s, [128, D], F32)
                bc_load(t[:, :], MODS[li, r, s * D:(s + 1) * D])
                res[s].append(t)
        return res

    def normmod(xt_ap, out_ap, gs, sh, tmp, ss, rs):
        k.act(tmp[:, :], xt_ap, AF.Square, accum=ss[:, 0:1])
        k.rstd(rs[:, 0:1], ss[:, 0:1], 1.0 / D)
        k.stt(tmp[:, :], xt_ap, rs[:, 0:1], gs[:, :], ALU.mult, ALU.mult)
        k.tt(out_ap, tmp[:, :], sh[:, :], ALU.add)

    def tok_groups():
        gs_ = []
        for g0 in range(0, NTL, 4):
            gs_.append(list(range(g0, min(NTL, g0 + 4))))
        gs_.append(list(range(NTL, NT)))
        return gs_

    def m1_even(li, j):
        with ExitStack() as st:
            Win = k.sb(st, "win", [128, 8, 2304], BF16)
            k.dma("pool", [(Win[:, c, :], w_in_even[j, c * 128:(c + 1) * 128, :]) for c in range(8)], max_dma_last_dim=4096)
            mods = mod_tiles(st, li, [0, 1])
            gain = k.sb(st, "gain", [128, 1664], F32)
            for (c0, n, src) in ((0, 8, a_qk[j, 0, :]), (512, 8, a_qk[j, 1, :]), (1024, 8, b_qk[j, 0, :]), (1536, 2, b_qk[j, 1, :])):
                k.dma("sp", [(gain[:, c0:c0 + n * 64].rearrange("p (g d) -> p g d", d=64),
                              src.partition_broadcast(128).unsqueeze(1).broadcast_to([128, n, 64]))])
            xts = [k.sb(st, "xt", [128, D], F32) for _ in range(2)]
            tmp = k.sb(st, "tmp", [128, D], F32)
            ss = k.sb(st, "ss", [128, 1], F32); rs = k.sb(st, "rs", [128, 1], F32)
            hb = k.sb(st, "hb", [128, D], BF16)
            hT = k.sb(st, "hT", [128, 8, 128], BF16)
            sq = k.sb(st, "sq", [128, 1664], F32)
            ssg = k.sb(st, "ssg", [128, 26], F32)
            nrm = k.sb(st, "nrm", [128, 1664], F32)
            t1 = k.sb(st, "t1", [128, 1664], F32)
            t2 = k.sb(st, "t2", [128, 1664], F32)
            cs = [k.sb(st, "cs", [128, 2, 64], F32) for _ in range(2)]
            qkb = k.sb(st, "qkb", [128, 1792], BF16)
            vst = [k.sb(st, "vst", [128, 640], BF16) for _ in range(2)]
            stage = [k.sb(st, "stage", [128, 14, 512], BF16) for _ in range(2)]
            tpT = k.ps(st, "tpT", [128, 512])
            P = k.ps(st, "P", [128, 2560])
            TQ = k.ps(st, "TQ", [128, 1024])
            tpTb = tpT[:, :].bitcast(BF16)
            TQb = TQ[:, :].bitcast(BF16)
            secs = ((0, 0, 512), (512, 512, 512), (1536, 1024, 512), (2048, 1536, 128))
            for gi, tiles in enumerate(tok_groups()):
                stg = stage[gi % 2]
                for jt, ti in enumerate(tiles):
                    kind = 0 if ti < NTL else 1
                    xt = xts[ti % 2]
                    k.dma("sp", [(xt[:, :], X[ti * 128:(ti + 1) * 128, :])])
                    if kind == 0:
                        c_ = cs[ti % 2]
                        k.dma("sp", [(c_[:, 0, :], k_cos64[ti * 128:(ti + 1) * 128, :]),
                                     (c_[:, 1, :], k_sin64[ti * 128:(ti + 1) * 128, :])])
                    normmod(xt[:, :], hb[:, :], mods[1][kind], mods[0][kind], tmp, ss, rs)
                    for c in range(8):
                        k.tr(tpTb[:, c * 128:(c + 1) * 128], hb[:, c * 128:(c + 1) * 128], ident_b[:, :])
                    k.cp(hT[:, :, :], tpTb.rearrange("p (c t) -> p c t", t=128), en="act")
                    for n in range(5):
                        w = min(2304, (n + 1) * 512) - n * 512
                        for c in range(8):
                            k.mm(P[:, n * 512:n * 512 + w], hT[:, c, :], Win[:, c, n * 512:n * 512 + w],
                                 start=(c == 0), stop=(c == 7))
                    for (ps0, s0, w) in secs:
                        k.act(sq[:, s0:s0 + w], P[:, ps0:ps0 + w], AF.Square)
                    k.red(ssg[:, :], sq[:, :].rearrange("p (g d) -> p g d", d=64))
                    k.rstd(ssg[:, :], ssg[:, :], 1.0 / 64)
                    for (ps0, s0, w) in secs:
                        g0, ng = s0 // 64, w // 64
                        k.tt(nrm[:, s0:s0 + w].rearrange("p (g d) -> p g d", d=64),
                             P[:, ps0:ps0 + w].rearrange("p (g d) -> p g d", d=64),
                             ssg[:, g0:g0 + ng].unsqueeze(2).broadcast_to([128, ng, 64]), ALU.mult)
                    k.tt(nrm[:, :], nrm[:, :], gain[:, :], ALU.mult)
                    v_ = vst[ti % 2]
                    k.cp(v_[:, 0:512], P[:, 1024:1536], en="act")
                    k.cp(v_[:, 512:640], P[:, 2176:2304], en="act")
                    k.dma("sp", [(Vd[ti * 128:(ti + 1) * 128, 0:640], v_[:, :])])
                    kbd = qkb[:, 1536:1792].rearrange("p (a b d) -> p a b d", a=2, b=2)
                    if kind == 0:
                        c_ = cs[ti % 2]
                        k.tt(t1[:, :].rearrange("p (g d) -> p g d", d=64), nrm[:, :].rearrange("p (g d) -> p g d", d=64),
                             c_[:, 0, :].unsqueeze(1).broadcast_to([128, 26, 64]), ALU.mult)
                        nv = nrm[:, :].rearrange("p (g r h j) -> p g r h j", r=2, h=2, j=16)
                        tv = t2[:, :].rearrange("p (g r h j) -> p g r h j", r=2, h=2, j=16)
                        sv = c_[:, 1, :].rearrange("p (r h j) -> p r h j", r=2, h=2)
                        for r in range(2):
                            for h in range(2):
                                k.tt(tv[:, :, r, h, :], nv[:, :, r, 1 - h, :],
                                     sv[:, r, h, :].unsqueeze(1).broadcast_to([128, 26, 16]), ALU.mult)
                        k.tt(qkb[:, 0:1536], t1[:, 0:1536], t2[:, 0:1536], ALU.add)
                        k.tt(kbd, t1[:, 1536:1664].rearrange("p (a d) -> p a d", d=64).unsqueeze(2).broadcast_to([128, 2, 2, 64]),
                             t2[:, 1536:1664].rearrange("p (a d) -> p a d", d=64).unsqueeze(2).broadcast_to([128, 2, 2, 64]), ALU.add)
                    else:
                        k.cp(qkb[:, 0:1536], nrm[:, 0:1536])
                        k.cp(kbd, nrm[:, 1536:1664].rearrange("p (a d) -> p a d", d=64).unsqueeze(2).broadcast_to([128, 2, 2, 64]))
                    for c in range(14):
                        k.tr(TQb[:, c * 128:(c + 1) * 128], qkb[:, c * 128:(c + 1) * 128], ident_b[:, :])
                    k.cp(stg[:, :, jt * 128:(jt + 1) * 128], TQb[:, 0:1792].rearrange("p (c t) -> p c t", t=128), en="act")
                n = len(tiles) * 128
                t0 = tiles[0] * 128
                k.dma("sp", [(QKT[:, :, t0:t0 + n].rearrange("c p t -> p c t"), stg[:, :, 0:n])])
            k.barrier()

    sctr = [0]

    def attention(groups, scale, lam_cols=None):
        with ExitStack() as st:
            KTb = [k.sb(st, "ktb", [128, TA], BF16) for _ in range(2)]
            VAb = [k.sb(st, "vab", [128, NT, 128], BF16) for _ in range(2)]
            Qb = [k.sb(st, "qb", [128, 512], BF16) for _ in range(3)]
            PT = [k.sb(st, "pt", [128, 512], BF16) for _ in range(3)]
            S = [k.ps(st, "S", [128, 512]) for _ in range(3)]
            accO = k.ps(st, "accO", [128, 512]); accD = k.ps(st, "accD", [128, 512])
            bR = k.ps(st, "bR", [128, 512]); bS = k.ps(st, "bS", [128, 512])
            osb = [k.sb(st, "osb", [128, 512], F32) for _ in range(2)]
            onA = [k.sb(st, "onA", [128, 512], F32) for _ in range(2)]
            rrow = k.sb(st, "rrow", [128, 512], F32)
            Dt = k.sb(st, "Dt", [128, 512], F32)
            sqd = k.sb(st, "sqd", [128, 512], F32)
            obf = [k.sb(st, "obf", [128, 512], BF16) for _ in range(2)]
            for v_ in VAb:
                k.memset(v_[:, :, :], 1.0)
            qblocks = [(b * 512, 512, 0, NT) for b in range(T // 512)] + [(T, C, NTL, NT)]
            qn = 0
            on_ = 0
            for gi, g in enumerate(groups):
                kb = KTb[gi % 2]; vb = VAb[gi % 2]
                kr, kdim, dv, typeA = g["kr"], g["kdim"], g["dv"], g["typeA"]
                k.dma("sp", [(kb[0:kr, :], g["kt"])])
                if not typeA:
                    k.memset(vb[:, :, 64:65], 1.0)
                vsrc = Vd[:, g["vcol"]:g["vcol"] + dv].rearrange("(t p) d -> p t d", p=128)
                k.dma("sp", [(vb[:, t0_:min(NT, t0_ + 8), 0:dv], vsrc[:, t0_:min(NT, t0_ + 8), :]) for t0_ in range(0, NT, 8)])
                for (q0, nq, klo, khi) in qblocks:
                    for (qap, units) in g["qcs"]:
                        qb = Qb[qn % 3]; qn += 1
                        k.dma("sp", [(qb[0:kr, 0:nq], qap[:, q0:q0 + nq])])
                        for ui, (pb, orow) in enumerate(units):
                            n = khi - klo
                            base = sctr[0]; sctr[0] += n

                            def QK(i):
                                kt = klo + i
                                k.mm(S[(base + i) % 3][:, 0:nq], kb[pb:pb + kdim, kt * 128:(kt + 1) * 128], qb[pb:pb + kdim, 0:nq])

                            QK(0)
                            if n > 1:
                                QK(1)
                            for i in range(n):
                                kt = klo + i
                                ix = (base + i) % 3
                                k.act(PT[ix][:, 0:nq], S[ix][:, 0:nq], AF.Exp, scale=scale)
                                if i + 2 < n:
                                    QK(i + 2)
                                if typeA:
                                    k.mm(accO[:, 0:nq], vb[:, kt, 0:128], PT[ix][:, 0:nq], start=(i == 0), stop=(i == n - 1))
                                    k.mm(accD[0:1, 0:nq], ones_b[:, 0:1], PT[ix][:, 0:nq], start=(i == 0), stop=(i == n - 1))
                                else:
                                    k.mm(accO[0:65, 0:nq], vb[:, kt, 0:65], PT[ix][:, 0:nq], start=(i == 0), stop=(i == n - 1))
                            if typeA:
                                c = ui
                                k.cp(osb[c][:, 0:nq], accO[:, 0:nq], en="act")
                                k.recip(rrow[0:1, 0:nq], accD[0:1, 0:nq])
                                k.mm(bR[:, 0:nq], ones_f[0:1, 0:128], rrow[0:1, 0:nq])
                                k.tt(onA[c][:, 0:nq], osb[c][:, 0:nq], bR[:, 0:nq], ALU.mult)
                                if c == 1:
                                    nlam, subg = lam_cols
                                    k.stt(Dt[:, 0:nq], onA[1][:, 0:nq], nlam[:, 0:1], onA[0][:, 0:nq], ALU.mult, ALU.add)
                                    k.act(sqd[:, 0:nq], Dt[:, 0:nq], AF.Square)
                                    k.mm(bS[0:1, 0:nq], ones_f[:, 0:1], sqd[:, 0:nq])
                                    k.rstd(rrow[0:1, 0:nq], bS[0:1, 0:nq], 1.0 / 128)
                                    k.mm(bR[:, 0:nq], ones_f[0:1, 0:128], rrow[0:1, 0:nq])
                                    ob = obf[on_ % 2]; on_ += 1
                                    k.stt(ob[:, 0:nq], Dt[:, 0:nq], subg[:, 0:1], bR[:, 0:nq], ALU.mult, ALU.mult)
                                    k.dma("sp", [(OT[orow:orow + 128, q0:q0 + nq], ob[:, 0:nq])])
                            else:
                                k.cp(osb[0][0:65, 0:nq], accO[0:65, 0:nq], en="act")
                                k.recip(rrow[64:65, 0:nq], osb[0][64:65, 0:nq])
                                k.mm(bR[0:64, 0:nq], ones_f[64:65, 0:64], rrow[64:65, 0:nq])
                                ob = obf[on_ % 2]; on_ += 1
                                k.tt(ob[0:64, 0:nq], osb[0][0:64, 0:nq], bR[0:64, 0:nq], ALU.mult)
                                k.dma("sp", [(OT[orow:orow + 64, q0:q0 + nq], ob[0:64, 0:nq])])
            k.barrier()

    def attn_even(li, j):
        lam_init = 0.8 - 0.6 * math.exp(-0.3 * li)
        with ExitStack() as st:
            dl = k.sb(st, "dl", [1, 256], F32)
            pr = k.sb(st, "pr", [1, 128], F32)
            s2 = k.sb(st, "s2", [1, 2], F32)
            nl = k.sb(st, "nl", [1, 1], F32)
            nlam = k.sb(st, "nlam", [128, 1], F32)
            subg = k.sb(st, "subg", [128, 1], F32)
            pl = k.ps(st, "pl", [128, 512])
            k.dma("sp", [(dl[0:1, :], dlam[j:j + 1].rearrange("o a d -> o (a d)"))])
            k.dma("sp", [(subg[:, 0:1], a_subln[j].rearrange("(p o) -> p o", o=1))])
            k.tt(pr[:, 0:64], dl[:, 0:64], dl[:, 64:128], ALU.mult)
            k.tt(pr[:, 64:128], dl[:, 128:192], dl[:, 192:256], ALU.mult)
            k.red(s2[:, :], pr[:, :].rearrange("o (a d) -> o a d", d=64))
            k.act(s2[:, :], s2[:, :], AF.Exp)
            k.tt(nl[:, :], s2[:, 1:2], s2[:, 0:1], ALU.subtract)
            k.ts(nl[:, :], nl[:, :], -lam_init, None, ALU.add)
            k.mm(pl[:, 0:1], ones_f[0:1, 0:128], nl[0:1, 0:1])
            k.cp(nlam[:, :], pl[:, 0:1])
            k.ts(subg[:, :], subg[:, :], 1.0 - lam_init, None, ALU.mult)
            groups = []
            for h in range(4):
                groups.append(dict(kt=QKT[4 + h], kr=128, kdim=64, vcol=h * 128, dv=128, typeA=True,
                                   qcs=[(QKT[h], [(0, h * 128), (64, h * 128)])]))
            for kv in range(2):
                qcs = []
                for qc in (8 + 2 * kv, 9 + 2 * kv):
                    hb0 = 2 * (qc - 8)
                    qcs.append((QKT[qc], [(0, 512 + hb0 * 64), (64, 512 + (hb0 + 1) * 64)]))
                groups.append(dict(kt=QKT[12 + kv], kr=128, kdim=64, vcol=512 + kv * 64, dv=64, typeA=False, qcs=qcs))
            attention(groups, 64 ** -0.5, (nlam, subg))

    def attn_odd(li, j):
        groups = []
        for h in range(16):
            groups.append(dict(kt=KTo[h], kr=96, kdim=96, vcol=h * 64, dv=64, typeA=False,
                               qcs=[(QTo[h], [(0, h * 64)])]))
        attention(groups, 96 ** -0.5)

    def mixout_ffnin(li, wout_d, AFF, IDX):
        with ExitStack() as st:
            Wout = k.sb(st, "wout", [128, 8, D], BF16)
            k.dma("pool", [(Wout[:, c, :], wout_d[c * 128:(c + 1) * 128, :]) for c in range(8)], max_dma_last_dim=4096)
            wr = k.sb(st, "wr", [128, 8, NE], F32)
            k.dma("sp", [(wr[:, :, :], w_router[li].rearrange("(c p) e -> p c e", p=128))])
            mods = mod_tiles(st, li, [2, 3, 4])
            xts = [k.sb(st, "xt", [128, D], F32) for _ in range(2)]
            oTs = [k.sb(st, "oT", [128, 8, 128], BF16) for _ in range(2)]
            tmp = k.sb(st, "tmp", [128, D], F32)
            xn = [k.sb(st, "xn", [128, D], F32) for _ in range(2)]
            hf = k.sb(st, "hf", [128, D], F32)
            hrow = [k.sb(st, "hrow", [128, D + 2], BF16) for _ in range(2)]
            hTf = k.sb(st, "hTf", [128, 8, 128], F32)
            ss = k.sb(st, "ss", [128, 1], F32); rs = k.sb(st, "rs", [128, 1], F32)
            mx = k.sb(st, "mx", [128, 1], F32); sm = k.sb(st, "sm", [128, 1], F32)
            ex = k.sb(st, "ex", [128, NE], F32)
            Y = k.ps(st, "Y", [128, 1024]); TP = k.ps(st, "TP", [128, 1024]); LG = k.ps(st, "LG", [128, 512])
            for ti in range(NT):
                kind = 0 if ti < NTL else 1
                xt = xts[ti % 2]; oT = oTs[ti % 2]; xn_ = xn[ti % 2]; hr = hrow[ti % 2]
                k.dma("sp", [(xt[:, :], X[ti * 128:(ti + 1) * 128, :])])
                k.dma("sp", [(oT[:, :, :], OT[:, ti * 128:(ti + 1) * 128].rearrange("(c p) t -> p c t", p=128))])
                for h2 in range(2):
                    for c in range(8):
                        k.mm(Y[:, h2 * 512:(h2 + 1) * 512], oT[:, c, :], Wout[:, c, h2 * 512:(h2 + 1) * 512],
                             start=(c == 0), stop=(c == 7))
                for h2 in range(2):
                    sl = slice(h2 * 512, (h2 + 1) * 512)
                    k.tt(tmp[:, sl], Y[:, sl], mods[2][kind][:, sl], ALU.mult)
                k.tt(xn_[:, :], xt[:, :], tmp[:, :], ALU.add)
                k.dma("sp", [(X[ti * 128:(ti + 1) * 128, :], xn_[:, :])])
                normmod(xn_[:, :], hf[:, :], mods[4][kind], mods[3][kind], tmp, ss, rs)
                k.cp(hr[:, 0:D], hf[:, :], en="act")
                k.cp(hr[:, D:D + 2].bitcast(I32), tokidx[:, ti:ti + 1])
                k.dma("sp", [(Hd[ti * 128:(ti + 1) * 128, :], hr[:, :])])
                for c in range(8):
                    k.tr(TP[:, c * 128:(c + 1) * 128], hf[:, c * 128:(c + 1) * 128], ident_f[:, :])
                k.cp(hTf[:, :, :], TP[:, :].rearrange("p (c t) -> p c t", t=128), en="act")
                for c in range(8):
                    k.mm(LG[:, 0:NE], hTf[:, c, :], wr[:, c, :], start=(c == 0), stop=(c == 7))
                k.red(mx[:, :], LG[:, 0:NE], op=ALU.max)
                k.ts(mx[:, :], mx[:, :], -1.0, None, ALU.mult)
                k.act(ex[:, :], LG[:, 0:NE], AF.Exp, bias=mx[:, 0:1], accum=sm[:, 0:1])
                k.recip(sm[:, :], sm[:, :])
                k.ts(AFF[:, ti, :], ex[:, :], sm[:, 0:1], None, ALU.mult)
            k.barrier()
        with ExitStack() as st:
            lo = k.sb(st, "lo", [128, 2 * NE], F32)
            tt_ = k.sb(st, "tthr", [128, 2 * NE], F32)
            ge = k.sb(st, "ge", [128, 2 * NE], F32)
            capv = k.sb(st, "capv", [128, 2 * NE], F32)
            cmp_ = k.sb(st, "cmp", [128, NT, NE], BF16)
            cntp = k.sb(st, "cntp", [128, 2 * NE], F32)
            base_t = k.sb(st, "base", [128, NT, NE], F32)
            pos = k.sb(st, "pos", [128, NT, NE], F32)
            off = k.sb(st, "off", [128, NT, NE], F32)
            val = k.sb(st, "val", [128, NT, NE], F32)
            pc = k.ps(st, "pc", [128, 512])
            ncolp = ((NT * NE + 511) // 512) * 512
            pw = k.ps(st, "pw", [128, ncolp])
            pt_ = k.ps(st, "ptot", [128, ncolp])
            k.dma("sp", [(capv[:, :], k_capv)])
            k.dma("sp", [(base_t[:, :, :], k_base.rearrange("p (t e) -> p t e", e=NE))])
            k.memset(lo[:, :], 0.0)
            sets = ((0, NTL, 0), (NTL, NT, NE))

            def compare(thr):
                for (a, b, o) in sets:
                    k.tt(cmp_[:, a:b, :], AFF[:, a:b, :], thr[:, o:o + NE].unsqueeze(1).broadcast_to([128, b - a, NE]), ALU.is_ge)

            step = 0.5
            for it in range(32):
                k.ts(tt_[:, :], lo[:, :], step, None, ALU.add)
                compare(tt_)
                for (a, b, o) in sets:
                    k.red(cntp[:, o:o + NE], cmp_[:, a:b, :].rearrange("p t e -> p e t"))
                k.mm(pc[:, 0:2 * NE], ones_f[:, :], cntp[:, :])
                k.tt(ge[:, :], pc[:, 0:2 * NE], capv[:, :], ALU.is_ge)
                k.stt(lo[:, :], ge[:, :], step, lo[:, :], ALU.mult, ALU.add)
                step *= 0.5
            compare(lo)
            cf = cmp_[:, :, :].rearrange("p t e -> p (t e)")
            ncol = NT * NE
            for c0 in range(0, ncol, 512):
                w = min(512, ncol - c0)
                k.mm(pw[:, c0:c0 + w], tri_b[:, :], cf[:, c0:c0 + w])
                k.mm(pt_[:, c0:c0 + w], ones_b[:, :], cf[:, c0:c0 + w])
            for (a, b, o) in sets:
                k.memset(off[:, a, :], 0.0)
                for t in range(a, b - 1):
                    k.tt(off[:, t + 1, :], off[:, t, :], pt_[:, t * NE:(t + 1) * NE], ALU.add)
            k.tt(pos[:, :, :], pw[:, 0:ncol].rearrange("p (t e) -> p t e", e=NE), off[:, :, :], ALU.add)
            for (a, b, o), cap in zip(sets, (capL, capC)):
                k.ts(val[:, a:b, :], pos[:, a:b, :], float(cap), None, ALU.is_lt)
            k.tt(val[:, :, :], val[:, :, :], cmp_[:, :, :], ALU.mult)
            k.tt(pos[:, :, :], pos[:, :, :], base_t[:, :, :], ALU.add)
            k.ts(pos[:, :, :], pos[:, :, :], -BIG, None, ALU.add)
            k.tt(pos[:, :, :], pos[:, :, :], val[:, :, :], ALU.mult)
            k.ts(pos[:, :, :], pos[:, :, :], BIG, None, ALU.add)
            k.cp(IDX[:, :, :], pos[:, :, :])
            k.barrier()
        with ExitStack() as st:
            hrow = [k.sb(st, "hrow", [128, D + 2], BF16) for _ in range(3)]
            for ti in range(NT):
                hr = hrow[ti % 3]
                k.dma("sp", [(hr[:, :], Hd[ti * 128:(ti + 1) * 128, :])])
                for e in range(NE):
                    k.idma(XS[:, :], hr[:, :], IDX[:, ti, e:e + 1], scatter=True, bounds=reg_xs)
            k.barrier()

    def experts(li):
        with ExitStack() as st:
            WG = [k.sb(st, "wg", [128, 8, D], BF16) for _ in range(2)]
            WU = [k.sb(st, "wu", [128, 8, D], BF16) for _ in range(2)]
            WD = [k.sb(st, "wd", [128, 8, D], BF16) for _ in range(2)]
            wrf = k.sb(st, "wrf", [128, 8, NE], F32)
            wrb = k.sb(st, "wrb", [128, 8, NE], BF16)
            k.dma("sp", [(wrf[:, :, :], w_router[li].rearrange("(c p) e -> p c e", p=128))])
            k.cp(wrb[:, :, :], wrf[:, :, :])
            mods = mod_tiles(st, li, [5])
            xsT = k.sb(st, "xsT", [128, 8, SLOTS], BF16)
            gT = k.sb(st, "gT", [128, 8, SLOTS], BF16)
            xst = [k.sb(st, "xst", [128, D + 2], BF16) for _ in range(3)]
            sa = [k.sb(st, "sa", [128, 512], F32) for _ in range(2)]
            pay = [k.sb(st, "pay", [128, D], F32) for _ in range(2)]
            mx = k.sb(st, "mx", [128, 1], F32); sm = k.sb(st, "sm", [128, 1], F32)
            ex = k.sb(st, "ex", [128, NE], F32)
            TPb = k.ps(st, "TPx", [128, 512])
            LG = k.ps(st, "LGx", [128, 512])
            PA = [k.ps(st, "PA", [128, 512]) for _ in range(2)]
            PB = [k.ps(st, "PB", [128, 512]) for _ in range(2)]
            PY = k.ps(st, "PY", [128, 1024])
            TPv = TPb[:, :].bitcast(BF16)
            stiles = []
            r = 0
            while r < capL:
                n = min(128, capL - r); stiles.append((r, n, 0)); r += n
            stiles.append((capL, capC, 1))
            nblk = (SLOTS + 511) // 512
            bw = (SLOTS + nblk - 1) // nblk
            blocks = [(b * bw, min(bw, SLOTS - b * bw)) for b in range(nblk)]
            cnt = 0
            pcnt = 0
            nst = len(stiles)
            tix = [k.sb(st, "tix", [128, 1], I32) for _ in range(2 * nst)]
            gate = [k.sb(st, "gate", [128, 1], F32) for _ in range(2 * nst)]
            for e in range(NE):
                wg, wu, wd = WG[e % 2], WU[e % 2], WD[e % 2]
                for (w_sb, w_d) in ((wg, w_eg), (wu, w_eu), (wd, w_ed)):
                    k.dma("pool", [(w_sb[:, c, :], w_d[li, e, c * 128:(c + 1) * 128, :]) for c in range(8)], max_dma_last_dim=4096)
                tinfo = []
                for si_, (r0, n, kind) in enumerate(stiles):
                    xs = xst[cnt % 3]; cnt += 1
                    tx = tix[(e % 2) * nst + si_]; gt = gate[(e % 2) * nst + si_]
                    k.dma("sp", [(xs[0:n, :], XS[e * SLOTS + r0:e * SLOTS + r0 + n, :])])
                    k.cp(tx[0:n, :], xs[0:n, D:D + 2].bitcast(I32))
                    for c in range(8):
                        k.tr(TPv[:, c * 128:c * 128 + n], xs[0:n, c * 128:(c + 1) * 128], ident_b[0:n, 0:n])
                    k.cp(xsT[:, :, r0:r0 + n], TPv.rearrange("p (c t) -> p c t", t=128)[:, :, 0:n], en="act")
                    for c in range(8):
                        k.mm(LG[0:n, 0:NE], xsT[:, c, r0:r0 + n], wrb[:, c, :], start=(c == 0), stop=(c == 7))
                    k.red(mx[0:n, :], LG[0:n, 0:NE], op=ALU.max)
                    k.ts(mx[0:n, :], mx[0:n, :], -1.0, None, ALU.mult)
                    k.act(ex[0:n, :], LG[0:n, 0:NE], AF.Exp, bias=mx[0:n, 0:1], accum=sm[0:n, 0:1])
                    k.recip(sm[0:n, :], sm[0:n, :])
                    k.tt(gt[0:n, :], ex[0:n, e:e + 1], sm[0:n, :], ALU.mult)
                    tinfo.append((r0, n, kind, tx, gt))
                for f in range(8):
                    for (b0, bn) in blocks:
                        pa = PA[pcnt % 2]; pb_ = PB[pcnt % 2]; sa_ = sa[pcnt % 2]; pcnt += 1
                        for c in range(8):
                            k.mm(pa[:, 0:bn], wg[:, c, f * 128:(f + 1) * 128], xsT[:, c, b0:b0 + bn], start=(c == 0), stop=(c == 7))
                        for c in range(8):
                            k.mm(pb_[:, 0:bn], wu[:, c, f * 128:(f + 1) * 128], xsT[:, c, b0:b0 + bn], start=(c == 0), stop=(c == 7))
                        k.act(sa_[:, 0:bn], pa[:, 0:bn], AF.Silu)
                        k.tt(gT[:, f, b0:b0 + bn], sa_[:, 0:bn], pb_[:, 0:bn], ALU.mult)
                for ti_, (r0, n, kind, tx, gt) in enumerate(tinfo):
                    py = pay[(e * len(tinfo) + ti_) % 2]
                    for h2 in range(2):
                        for f in range(8):
                            k.mm(PY[0:n, h2 * 512:(h2 + 1) * 512], gT[:, f, r0:r0 + n], wd[:, f, h2 * 512:(h2 + 1) * 512],
                                 start=(f == 0), stop=(f == 7))
                    for h2 in range(2):
                        sl = slice(h2 * 512, (h2 + 1) * 512)
                        k.stt(py[0:n, sl], PY[0:n, sl], gt[0:n, 0:1], mods[5][kind][0:n, sl], ALU.mult, ALU.mult)
                    k.idma(X[:, :], py[0:n, :], tx[0:n, 0:1], scatter=True, bounds=reg_x, add=True, xw=[xtok])
            k.barrier()

    def m1_odd(li, j):
        with ExitStack() as st:
            Win = k.sb(st, "wino", [128, 8, 416], BF16)
            Wq = k.sb(st, "wq", [128, 2, 1536], BF16)
            Wkv = k.sb(st, "wkv", [128, 2048], BF16)
            k.dma("pool", [(Win[:, c, :], w_in_odd[j, c * 128:(c + 1) * 128, :]) for c in range(8)], max_dma_last_dim=4096)
            k.dma("pool", [(Wq[:, c, :], w_q_up[j, c * 128:(c + 1) * 128, :]) for c in range(2)], max_dma_last_dim=4096)
            k.dma("pool", [(Wkv[:, :], w_kv_up[j])], max_dma_last_dim=4096)
            mods = mod_tiles(st, li, [0, 1])
            gq = k.sb(st, "gq", [128, 256], F32); gkv = k.sb(st, "gkv", [128, 128], F32)
            gqk = k.sb(st, "gqk", [128, 2, 96], F32)
            bc_load(gq[:, :], q_norm[j]); bc_load(gkv[:, :], kv_norm[j])
            bc_load(gqk[:, 0, :], qk_norm[j, 0]); bc_load(gqk[:, 1, :], qk_norm[j, 1])
            invn = k.sb(st, "invn", [128, 3], F32)
            k.memset(invn[:, 0:1], 1.0 / 256); k.memset(invn[:, 1:2], 1.0 / 128); k.memset(invn[:, 2:3], 1.0 / 32)
            xts = [k.sb(st, "xt", [128, D], F32) for _ in range(2)]
            tmp = k.sb(st, "tmp", [128, D], F32)
            ss = k.sb(st, "ss", [128, 1], F32); rs = k.sb(st, "rs", [128, 1], F32)
            hb = k.sb(st, "hb", [128, D], BF16)
            hT = k.sb(st, "hT", [128, 8, 128], BF16)
            sqp = k.sb(st, "sqp", [128, 416], F32)
            s3 = k.sb(st, "s3", [128, 3], F32)
            cn = k.sb(st, "cn", [128, 384], BF16)
            cT = k.sb(st, "cT", [128, 3, 128], BF16)
            krn = k.sb(st, "krn", [128, 32], F32)
            krr = k.sb(st, "krr", [128, 32], F32)
            kt1 = k.sb(st, "kt1", [128, 32], F32); kt2 = k.sb(st, "kt2", [128, 32], F32)
            sqq = k.sb(st, "sqq", [128, 2048], F32)
            sq16 = k.sb(st, "sq16", [128, 32], F32)
            qn_ = k.sb(st, "qn", [128, 16, 96], F32)
            qt1 = k.sb(st, "qt1", [128, 16, 32], F32); qt2 = k.sb(st, "qt2", [128, 16, 32], F32)
            qbf = k.sb(st, "qbf", [128, 16, 96], BF16)
            kbf = k.sb(st, "kbf", [128, 16, 96], BF16)
            cs = [k.sb(st, "cs", [128, 2, 32], F32) for _ in range(2)]
            vst = [k.sb(st, "vst", [128, 16, 64], BF16) for _ in range(2)]
            stq = [k.sb(st, "stq", [96, 16, 512], BF16) for _ in range(2)]
            stk = [k.sb(st, "stk", [96, 16, 512], BF16) for _ in range(2)]
            tpT = k.ps(st, "tpT", [128, 512]); P = k.ps(st, "P", [128, 512]); cTp = k.ps(st, "cTp", [128, 512])
            Q3 = k.ps(st, "Q3", [128, 1536]); KV = k.ps(st, "KV", [128, 1024])
            tpTb = tpT[:, :].bitcast(BF16); cTb = cTp[:, :].bitcast(BF16)
            Q3b = Q3[:, :].bitcast(BF16); KVb = KV[:, :].bitcast(BF16)

            def rope32(dst, src, c_, shp, t1, t2):
                G = shp
                k.tt(t1, src, c_[:, 0, :].unsqueeze(1).broadcast_to([128, G, 32]), ALU.mult)
                sv = c_[:, 1, :].rearrange("p (r h j) -> p r h j", r=2, h=2)
                s5 = src.rearrange("p g (r h j) -> p g r h j", r=2, h=2)
                t5 = t2.rearrange("p g (r h j) -> p g r h j", r=2, h=2)
                for r in range(2):
                    for h in range(2):
                        k.tt(t5[:, :, r, h, :], s5[:, :, r, 1 - h, :], sv[:, r, h, :].unsqueeze(1).broadcast_to([128, G, 8]), ALU.mult)
                k.tt(dst, t1, t2, ALU.add)

            for gi, tiles in enumerate(tok_groups()):
                sq_, sk_ = stq[gi % 2], stk[gi % 2]
                for jt, ti in enumerate(tiles):
                    kind = 0 if ti < NTL else 1
                    xt = xts[ti % 2]
                    k.dma("sp", [(xt[:, :], X[ti * 128:(ti + 1) * 128, :])])
                    c_ = cs[ti % 2]
                    if kind == 0:
                        k.dma("sp", [(c_[:, 0, :], k_cos32[ti * 128:(ti + 1) * 128, :]),
                                     (c_[:, 1, :], k_sin32[ti * 128:(ti + 1) * 128, :])])
                    normmod(xt[:, :], hb[:, :], mods[1][kind], mods[0][kind], tmp, ss, rs)
                    for c in range(8):
                        k.tr(tpTb[:, c * 128:(c + 1) * 128], hb[:, c * 128:(c + 1) * 128], ident_b[:, :])
                    k.cp(hT[:, :, :], tpTb.rearrange("p (c t) -> p c t", t=128), en="act")
                    for c in range(8):
                        k.mm(P[:, 0:416], hT[:, c, :], Win[:, c, :], start=(c == 0), stop=(c == 7))
                    k.act(sqp[:, :], P[:, 0:416], AF.Square)
                    k.red(s3[:, 0:1], sqp[:, 0:256]); k.red(s3[:, 1:2], sqp[:, 256:384]); k.red(s3[:, 2:3], sqp[:, 384:416])
                    k.tt(s3[:, :], s3[:, :], invn[:, :], ALU.mult)
                    k.rstd(s3[:, :], s3[:, :], 1.0)
                    k.stt(cn[:, 0:256], P[:, 0:256], s3[:, 0:1], gq[:, :], ALU.mult, ALU.mult)
                    k.stt(cn[:, 256:384], P[:, 256:384], s3[:, 1:2], gkv[:, :], ALU.mult, ALU.mult)
                    k.stt(krn[:, :], P[:, 384:416], s3[:, 2:3], gqk[:, 1, 64:96], ALU.mult, ALU.mult)
                    if kind == 0:
                        rope32(krr[:, :].unsqueeze(1), krn[:, :].unsqueeze(1), c_, 1, kt1[:, :].unsqueeze(1), kt2[:, :].unsqueeze(1))
                    else:
                        k.cp(krr[:, :], krn[:, :])
                    for c in range(3):
                        k.tr(cTb[:, c * 128:(c + 1) * 128], cn[:, c * 128:(c + 1) * 128], ident_b[:, :])
                    k.cp(cT[:, :, :], cTb[:, 0:384].rearrange("p (c t) -> p c t", t=128), en="act")
                    for n in range(3):
                        for c in range(2):
                            k.mm(Q3[:, n * 512:(n + 1) * 512], cT[:, c, :], Wq[:, c, n * 512:(n + 1) * 512], start=(c == 0), stop=(c == 1))
                    for n in range(3):
                        k.act(sqq[:, n * 512:(n + 1) * 512], Q3[:, n * 512:(n + 1) * 512], AF.Square)
                    sv3 = sqq[:, 0:1536].rearrange("p (h d) -> p h d", d=96)
                    q3v = Q3[:, :].rearrange("p (h d) -> p h d", d=96)
                    k.red(sq16[:, 0:16], sv3[:, :, 0:64]); k.red(sq16[:, 16:32], sv3[:, :, 64:96])
                    k.rstd(sq16[:, 0:16], sq16[:, 0:16], 1.0 / 64)
                    k.rstd(sq16[:, 16:32], sq16[:, 16:32], 1.0 / 32)
                    k.tt(qn_[:, :, 0:64], q3v[:, :, 0:64], sq16[:, 0:16].unsqueeze(2).broadcast_to([128, 16, 64]), ALU.mult)
                    k.tt(qn_[:, :, 64:96], q3v[:, :, 64:96], sq16[:, 16:32].unsqueeze(2).broadcast_to([128, 16, 32]), ALU.mult)
                    k.tt(qn_[:, :, :], qn_[:, :, :], gqk[:, 0, :].unsqueeze(1).broadcast_to([128, 16, 96]), ALU.mult)
                    k.cp(qbf[:, :, 0:64], qn_[:, :, 0:64])
                    if kind == 0:
                        rope32(qbf[:, :, 64:96], qn_[:, :, 64:96], c_, 16, qt1[:, :, :], qt2[:, :, :])
                    else:
                        k.cp(qbf[:, :, 64:96], qn_[:, :, 64:96])
                    for h in range(16):
                        k.tr(Q3b[0:96, h * 128:(h + 1) * 128], qbf[:, h, :], ident_b[:, :])
                    k.cp(sq_[:, :, jt * 128:(jt + 1) * 128], Q3b[0:96, 0:2048].rearrange("p (h t) -> p h t", t=128), en="act")
                    v_ = vst[ti % 2]
                    for hf_ in range(2):
                        for n in range(2):
                            c0 = hf_ * 1024 + n * 512
                            k.mm(KV[:, n * 512:(n + 1) * 512], cT[:, 2, :], Wkv[:, c0:c0 + 512])
                        kv3 = KV[:, :].rearrange("p (h d) -> p h d", d=128)
                        for n in range(2):
                            k.act(sqq[:, n * 512:(n + 1) * 512], KV[:, n * 512:(n + 1) * 512], AF.Square)
                        k.red(sq16[:, 0:8], sqq[:, 0:1024].rearrange("p (h d) -> p h d", d=128)[:, :, 0:64])
                        k.rstd(sq16[:, 0:8], sq16[:, 0:8], 1.0 / 64)
                        hs = slice(hf_ * 8, hf_ * 8 + 8)
                        k.tt(qn_[:, 0:8, 0:64], kv3[:, :, 0:64], sq16[:, 0:8].unsqueeze(2).broadcast_to([128, 8, 64]), ALU.mult)
                        k.tt(kbf[:, hs, 0:64], qn_[:, 0:8, 0:64], gqk[:, 1, 0:64].unsqueeze(1).broadcast_to([128, 8, 64]), ALU.mult)
                        k.cp(v_[:, hs, :], kv3[:, :, 64:128], en="act")
                    k.cp(kbf[:, :, 64:96], krr[:, :].unsqueeze(1).broadcast_to([128, 16, 32]))
                    k.dma("sp", [(Vd[ti * 128:(ti + 1) * 128, :], v_[:, :, :].rearrange("p h d -> p (h d)"))])
                    for h in range(16):
                        k.tr(KVb[0:96, h * 128:(h + 1) * 128], kbf[:, h, :], ident_b[:, :])
                    k.cp(sk_[:, :, jt * 128:(jt + 1) * 128], KVb[0:96, 0:2048].rearrange("p (h t) -> p h t", t=128), en="act")
                n = len(tiles) * 128
                t0 = tiles[0] * 128
                k.dma("sp", [(QTo[:, :, t0:t0 + n].rearrange("h p t -> p h t"), sq_[:, :, 0:n])])
                k.dma("sp", [(KTo[:, :, t0:t0 + n].rearrange("h p t -> p h t"), sk_[:, :, 0:n])])
            k.barrier()

    lst = ExitStack()
    AFF = k.sb(lst, "AFF", [128, NT, NE], F32)
    IDX = k.sb(lst, "IDX", [128, NT, NE], I32)
    k.barrier()
    upto = getattr(cfg, "upto", None)
    for li in range(L):
        j = li // 2
        if li % 2 == 0:
            m1_even(li, j)
            if upto == "m1" and li == L - 1:
                break
            attn_even(li, j)
            if upto == "attn" and li == L - 1:
                break
            mixout_ffnin(li, w_out_even[j], AFF, IDX)
        else:
            m1_odd(li, j)
            if upto == "m1" and li == L - 1:
                break
            attn_odd(li, j)
            if upto == "attn" and li == L - 1:
                break
            mixout_ffnin(li, w_out_odd[j], AFF, IDX)
        if upto == "ffnin" and li == L - 1:
            break
        experts(li)
    k.barrier()
    k.dma("sp", [(out_d, X[0:T, :])], sembuf=xtok)
    k.barrier()
    lst.close()
    gst.close()
    k.dbg = dict(X=X, MODS=MODS, QKT=QKT, Vd=Vd, OT=OT, Hd=Hd, XS=XS, QTo=QTo, KTo=KTo)
    return k


def host_consts(cfg):
    T, NT, NTL = cfg.T, cfg.NT, cfg.NTL
    c = {}
    c["k_ident"] = np.eye(128, dtype=np.float32)
    c["k_tri"] = np.triu(np.ones((128, 128), np.float32), 1)
    c["k_tokidx"] = (np.arange(NT, dtype=np.int32)[None, :] * 128 + np.arange(128, dtype=np.int32)[:, None]).astype(np.int32)
    t = np.arange(T)
    row = (t // GRID_W).astype(np.float32); col = (t % GRID_W).astype(np.float32)
    for rd, nm in ((64, "64"), (32, "32")):
        half = rd // 2
        fr = (ROPE_THETA ** (-np.arange(0, half, 2, dtype=np.float32) / half)).astype(np.float32)
        ar = (row[:, None] * fr[None, :]).astype(np.float32); ac = (col[:, None] * fr[None, :]).astype(np.float32)
        cr, sr, cc_, sc_ = np.cos(ar), np.sin(ar), np.cos(ac), np.sin(ac)
        c["k_cos" + nm] = np.concatenate([cr, cr, cc_, cc_], axis=1).astype(np.float32)
        c["k_sin" + nm] = np.concatenate([-sr, sr, -sc_, sc_], axis=1).astype(np.float32)
    base = np.zeros((128, NT, NE), np.float32)
    base[:, :, :] = (np.arange(NE, dtype=np.float32) * cfg.SLOTS)[None, None, :]
    base[:, NTL:, :] += cfg.capL
    c["k_base"] = base.reshape(128, NT * NE)
    capv = np.zeros((128, 2 * NE), np.float32); capv[:, :NE] = cfg.capL; capv[:, NE:] = cfg.capC
    c["k_capv"] = capv
    return c


def make_in_maps(cfg, inputs):
    consts = host_consts(cfg)
    B = inputs["x"].shape[0]
    wnames = ["w_ada", "b_ada", "norm_mix", "norm_ffn", "w_in_even", "a_qk_norm", "diff_lambda", "a_subln",
              "b_qk_norm", "w_out_even", "w_router", "w_exp_gate", "w_exp_up", "w_exp_down"]
    if cfg.n_odd:
        wnames += ["w_in_odd", "mla_q_norm", "w_q_up", "mla_kv_norm", "w_kv_up", "mla_qk_norm", "w_out_odd"]
    shared = {n: np.ascontiguousarray(np.asarray(inputs[n], dtype=np.float32)) for n in wnames}
    shared.update(consts)
    maps = []
    cctx = np.asarray(inputs["c_ctx"], np.float32)
    for b in range(B):
        m = dict(shared)
        m["x"] = np.ascontiguousarray(np.asarray(inputs["x"][b], np.float32))
        m["ctx"] = np.ascontiguousarray(np.asarray(inputs["ctx"][b], np.float32))
        cb = np.asarray(inputs["c"][b], np.float32)
        m["cc"] = np.ascontiguousarray(np.stack([cb.reshape(8, 128).T, cctx.reshape(8, 128).T], axis=-1))
        maps.append(m)
    return maps


def kernel(**inputs):
    cfg = Cfg()
    kb = build(cfg)
    maps = make_in_maps(cfg, inputs)
    res = run_bass_kernel_spmd(kb.nc, maps, core_ids=list(range(len(maps))))
    return np.stack([np.asarray(r["out"], dtype=np.float32) for r in res.results], axis=0)
```

```python
import math
from contextlib import ExitStack
import numpy as np
import concourse.bass as bass
import concourse.mybir as mybir
from concourse.bass_utils import run_bass_kernel_spmd

F32 = mybir.dt.float32
BF16 = mybir.dt.bfloat16
I32 = mybir.dt.int32
AF = mybir.ActivationFunctionType
ALU = mybir.AluOpType
AX = mybir.AxisListType

D = 1024
NE = 16
EPS = 1e-6
GRID_W = 64
ROPE_THETA = 10000.0


class Cfg:
    def __init__(self, T=8192, C=256, depth=4):
        self.T, self.C, self.L = T, C, depth
        self.TA = T + C
        self.NTL, self.NTC = T // 128, C // 128
        self.NT = self.NTL + self.NTC
        self.capL = 2 * T // NE
        self.capC = 2 * C // NE
        self.SLOTS = self.capL + self.capC
        self.n_even = (depth + 1) // 2
        self.n_odd = depth // 2
        self.QB = 512


class Buf:
    __slots__ = ("name", "w", "r", "sid")

    def __init__(self, name):
        self.name, self.w, self.r, self.sid = name, None, {}, None


class Eng:
    def __init__(self, e, sid):
        self.e, self.sid, self.waited = e, sid, {}


class KB:
    def __init__(self, cfg):
        self.cfg = cfg
        nc = self.nc = bass.Bass("TRN2", target_bir_lowering=False)
        self.sem = {}
        self.E = {}
        for n, e in (("pe", nc.tensor), ("act", nc.scalar), ("dve", nc.vector),
                     ("pool", nc.gpsimd), ("sp", nc.sync)):
            self.E[n] = Eng(e, self.newsem("e_" + n))
        self.bufs = {}
        self.uid = 0
        self.free_sids = {}

    def dsem(self, q, name):
        fl = self.free_sids.get(q)
        if fl:
            return fl.pop()
        return self.newsem(name)

    def _release(self, nm):
        b = self.bufs.pop(nm, None)
        if b is not None and b.sid:
            for q, sid in b.sid.items():
                self.free_sids.setdefault(q, []).append(sid)

    def newsem(self, name):
        sid = len(self.sem)
        self.sem[sid] = [self.nc.alloc_semaphore(name), 0]
        return sid

    def token(self, name):
        b = Buf(name)
        self.bufs[name] = b
        return b

    def _wait(self, E, sid, val):
        if E.waited.get(sid, 0) < val:
            E.e.wait_ge(self.sem[sid][0], val)
            E.waited[sid] = val

    def _deps(self, E, rb, wb, is_pe):
        need = {}
        for b in rb:
            if b.w is not None:
                need[b.w[0]] = max(need.get(b.w[0], 0), b.w[1])
        for b in wb:
            if b.w is not None:
                need[b.w[0]] = max(need.get(b.w[0], 0), b.w[1])
            for sid, v in b.r.items():
                need[sid] = max(need.get(sid, 0), v)
        for sid, v in need.items():
            if is_pe and sid == E.sid:
                continue
            self._wait(E, sid, v)

    def _mark(self, rb, wb, mark):
        sid, v = mark
        for b in rb:
            if b.r.get(sid, 0) < v:
                b.r[sid] = v
        for b in wb:
            b.w = mark
            b.r = {}

    def _bufs_of(self, aps, extra):
        out = []
        for a in aps:
            if a is None or isinstance(a, (int, float)):
                continue
            b = self.bufs.get(a.name)
            if b is not None and b not in out:
                out.append(b)
        for b in extra:
            if b not in out:
                out.append(b)
        return out

    def op(self, en, fn, outs, ins, xr=(), xw=()):
        E = self.E[en]
        rb = self._bufs_of(ins, xr)
        wb = self._bufs_of(outs, xw)
        self._deps(E, rb, wb, en == "pe")
        ins_ = fn(E.e)
        s = self.sem[E.sid]
        s[1] += 1
        ins_.then_inc(s[0], 1)
        self._mark(rb, wb, (E.sid, s[1]))

    def dma(self, q, pairs, xr=(), xw=(), sembuf=None, **kw):
        E = self.E[q]
        outs = [p[0] for p in pairs]
        ins = [p[1] for p in pairs]
        rb = self._bufs_of(ins, xr)
        wb = self._bufs_of(outs, xw)
        sb = sembuf
        if sb is None:
            cand = self._bufs_of(outs, ()) or self._bufs_of(ins, ())
            sb = cand[0]
        if sb.sid is None:
            sb.sid = {}
        if q not in sb.sid:
            sb.sid[q] = self.dsem(q, "d_%s_%s" % (q, sb.name))
        self._deps(E, rb, wb, False)
        s = self.sem[sb.sid[q]]
        for o, i in pairs:
            E.e.dma_start(out=o, in_=i, **kw).then_inc(s[0], 16)
            s[1] += 16
        self._mark(rb, wb, (sb.sid[q], s[1]))

    def idma(self, out, in_, idx_ap, scatter, bounds, add=False, xr=(), xw=()):
        E = self.E["pool"]
        rb = self._bufs_of([in_, idx_ap], xr)
        wb = self._bufs_of([out], xw)
        cand = self._bufs_of([in_] if scatter else [out], ())
        sb = cand[0]
        if sb.sid is None:
            sb.sid = {}
        if "ind" not in sb.sid:
            sb.sid["ind"] = self.dsem("ind", "d_ind_" + sb.name)
        self._deps(E, rb, wb, False)
        s = self.sem[sb.sid["ind"]]
        off = bass.IndirectOffsetOnAxis(ap=idx_ap, axis=0)
        kw = {}
        if add:
            kw["compute_op"] = ALU.add
            kw["oob_is_err"] = True
        else:
            kw["oob_is_err"] = False
        E.e.indirect_dma_start(out=out, out_offset=off if scatter else None, in_=in_,
                               in_offset=None if scatter else off,
                               bounds_check=bounds, **kw).then_inc(s[0], 16)
        s[1] += 16
        self._mark(rb, wb, (sb.sid["ind"], s[1]))

    def barrier(self):
        for E in self.E.values():
            for sid, (h, c) in self.sem.items():
                if c > 0:
                    self._wait(E, sid, c)
        for b in self.bufs.values():
            b.w, b.r = None, {}

    def sb(self, st, name, shape, dt):
        self.uid += 1
        nm = "%s_%d" % (name, self.uid)
        t = st.enter_context(self.nc.sbuf_tensor(nm, list(shape), dt))
        self.bufs[nm] = Buf(nm)
        st.callback(self._release, nm)
        return t

    def ps(self, st, name, shape, dt=F32):
        self.uid += 1
        nm = "%s_%d" % (name, self.uid)
        t = st.enter_context(self.nc.psum_tensor(nm, list(shape), dt))
        self.bufs[nm] = Buf(nm)
        st.callback(self._release, nm)
        return t

    def mm(self, out, lhsT, rhs, start=True, stop=True):
        self.op("pe", lambda e: e.matmul(out, lhsT, rhs, start=start, stop=stop), [out], [lhsT, rhs])

    def tr(self, out, in_, ident):
        self.op("pe", lambda e: e.transpose(out, in_, ident), [out], [in_, ident])

    def act(self, out, in_, func, scale=1.0, bias=0.0, accum=None, en="act"):
        kw = {}
        if accum is not None:
            kw["accum_out"] = accum
        self.op("act", lambda e: e.activation(out=out, in_=in_, func=func, bias=bias, scale=scale, **kw),
                [out, accum], [in_, bias, scale])

    def tt(self, out, in0, in1, op, en="dve"):
        self.op(en, lambda e: e.tensor_tensor(out=out, in0=in0, in1=in1, op=op), [out], [in0, in1])

    def ts(self, out, in0, s1, s2, op0, op1=None, en="dve"):
        if op1 is None:
            self.op(en, lambda e: e.tensor_scalar(out=out, in0=in0, scalar1=s1, scalar2=None, op0=op0),
                    [out], [in0, s1])
        else:
            self.op(en, lambda e: e.tensor_scalar(out=out, in0=in0, scalar1=s1, scalar2=s2, op0=op0, op1=op1),
                    [out], [in0, s1, s2])

    def stt(self, out, in0, sc, in1, op0, op1, en="dve"):
        self.op(en, lambda e: e.scalar_tensor_tensor(out=out, in0=in0, scalar=sc, in1=in1, op0=op0, op1=op1),
                [out], [in0, sc, in1])

    def red(self, out, in_, op=ALU.add, en="dve"):
        self.op(en, lambda e: e.tensor_reduce(out=out, in_=in_, axis=AX.X, op=op), [out], [in_])

    def cp(self, out, in_, en="dve"):
        if en == "act":
            self.op("act", lambda e: e.activation(out=out, in_=in_, func=AF.Copy), [out], [in_])
        else:
            self.op(en, lambda e: e.tensor_copy(out=out, in_=in_), [out], [in_])

    def recip(self, out, in_):
        self.op("dve", lambda e: e.reciprocal(out=out, in_=in_), [out], [in_])

    def memset(self, ap, v, en="dve"):
        self.op(en, lambda e: e.memset(ap, v), [ap], [])

    def rstd(self, out, ss, inv_n):
        self.act(out, ss, AF.Sqrt, scale=inv_n, bias=EPS)
        self.recip(out, out)


def build(cfg):
    k = KB(cfg)
    nc = k.nc
    T, C, TA, L, NT, NTL, NTC = cfg.T, cfg.C, cfg.TA, cfg.L, cfg.NT, cfg.NTL, cfg.NTC
    SLOTS, capL, capC = cfg.SLOTS, cfg.capL, cfg.capC
    ne, no = cfg.n_even, cfg.n_odd

    def din(name, shape, dt=F32):
        return nc.dram_tensor(name, list(shape), dt, kind="ExternalInput").ap()

    def dint(name, shape, dt):
        return nc.dram_tensor(name, list(shape), dt, kind=("ExternalOutput" if getattr(cfg, "debug", False) else "Internal")).ap()

    x_in = din("x", [T, D]); ctx_in = din("ctx", [C, D]); cc_in = din("cc", [128, 8, 2])
    w_ada = din("w_ada", [L, D, 6 * D]); b_ada = din("b_ada", [L, 6 * D])
    norm_mix = din("norm_mix", [L, D]); norm_ffn = din("norm_ffn", [L, D])
    w_in_even = din("w_in_even", [ne, D, 2304]); a_qk = din("a_qk_norm", [ne, 2, 64])
    dlam = din("diff_lambda", [ne, 4, 64]); a_subln = din("a_subln", [ne, 128])
    b_qk = din("b_qk_norm", [ne, 2, 64]); w_out_even = din("w_out_even", [ne, D, D])
    if no:
        w_in_odd = din("w_in_odd", [no, D, 416]); q_norm = din("mla_q_norm", [no, 256])
        w_q_up = din("w_q_up", [no, 256, 1536]); kv_norm = din("mla_kv_norm", [no, 128])
        w_kv_up = din("w_kv_up", [no, 128, 2048]); qk_norm = din("mla_qk_norm", [no, 2, 96])
        w_out_odd = din("w_out_odd", [no, D, D])
    w_router = din("w_router", [L, D, NE])
    w_eg = din("w_exp_gate", [L, NE, D, D]); w_eu = din("w_exp_up", [L, NE, D, D])
    w_ed = din("w_exp_down", [L, NE, D, D])
    k_ident = din("k_ident", [128, 128]); k_tri = din("k_tri", [128, 128])
    k_tokidx = din("k_tokidx", [128, NT], I32)
    k_cos64 = din("k_cos64", [T, 64]); k_sin64 = din("k_sin64", [T, 64])
    k_cos32 = din("k_cos32", [T, 32]); k_sin32 = din("k_sin32", [T, 32])
    k_base = din("k_base", [128, NT * NE]); k_capv = din("k_capv", [128, 2 * NE])
    out_d = nc.dram_tensor("out", [T, D], F32, kind="ExternalOutput").ap()

    X = dint("X", [TA, D], F32)
    MODS = dint("MODS", [L, 2, 6 * D], F32)
    QKT = dint("QKT", [14, 128, TA], BF16)
    QTo = dint("QTo", [16, 96, TA], BF16)
    KTo = dint("KTo", [16, 96, TA], BF16)
    Vd = dint("Vd", [TA, D], BF16)
    OT = dint("OT", [D, TA], BF16)
    Hd = dint("Hd", [TA, D + 2], BF16)
    XS = dint("XS", [NE * SLOTS + 128, D + 2], BF16)
    BIG = float(NE * SLOTS + 4096)
    reg_xs = nc.gpsimd.alloc_register("bnd_xs")
    nc.gpsimd.reg_mov(reg_xs, NE * SLOTS - 1)
    reg_x = nc.gpsimd.alloc_register("bnd_x")
    nc.gpsimd.reg_mov(reg_x, TA - 1)

    gst = ExitStack()
    ident_f = k.sb(gst, "identf", [128, 128], F32)
    ident_b = k.sb(gst, "identb", [128, 128], BF16)
    tri_f = k.sb(gst, "trif", [128, 128], F32)
    tri_b = k.sb(gst, "trib", [128, 128], BF16)
    ones_f = k.sb(gst, "onesf", [128, 128], F32)
    ones_b = k.sb(gst, "onesb", [128, 128], BF16)
    tokidx = k.sb(gst, "tokidx", [128, NT], I32)
    xtok = k.token("Xtok")

    k.dma("sp", [(ident_f[:, :], k_ident)])
    k.dma("sp", [(tri_f[:, :], k_tri)])
    k.dma("sp", [(tokidx[:, :], k_tokidx)])
    k.cp(ident_b[:, :], ident_f[:, :])
    k.cp(tri_b[:, :], tri_f[:, :])
    k.memset(ones_f[:, :], 1.0)
    k.memset(ones_b[:, :], 1.0)
    k.dma("sp", [(X[0:T, :], x_in), (X[T:TA, :], ctx_in)], sembuf=xtok)

    with ExitStack() as st:
        cc = k.sb(st, "cc", [128, 8, 2], F32)
        k.dma("sp", [(cc[:, :, :], cc_in)])
        k.act(cc[:, :, :], cc[:, :, :], AF.Silu)
        wbuf = [k.sb(st, "wada", [128, 8, 512], F32) for _ in range(2)]
        mrow = k.sb(st, "mrow", [2, 6 * D], F32)
        bias = k.sb(st, "bias", [2, 6 * D], F32)
        gg = k.sb(st, "gg", [2, 2, D], F32)
        pm = [k.ps(st, "pm", [128, 512]) for _ in range(2)]
        for li in range(L):
            k.dma("sp", [(bias[0:1, :], b_ada[li:li + 1, :]), (bias[1:2, :], b_ada[li:li + 1, :])])
            k.dma("sp", [(gg[0:1, 0, :], norm_mix[li:li + 1, :]), (gg[1:2, 0, :], norm_mix[li:li + 1, :]),
                         (gg[0:1, 1, :], norm_ffn[li:li + 1, :]), (gg[1:2, 1, :], norm_ffn[li:li + 1, :])])
            for n in range(12):
                wb = wbuf[n % 2]
                k.dma("sp", [(wb[:, :, :], w_ada[li, :, n * 512:(n + 1) * 512].rearrange("(c p) n -> p c n", p=128))])
                for c in range(8):
                    k.mm(pm[n % 2][0:2, :], cc[:, c, :], wb[:, c, :], start=(c == 0), stop=(c == 7))
                k.tt(mrow[:, n * 512:(n + 1) * 512], pm[n % 2][0:2, :], bias[:, n * 512:(n + 1) * 512], ALU.add)
            k.stt(mrow[:, D:2 * D], mrow[:, D:2 * D], 1.0, gg[:, 0, :], ALU.add, ALU.mult)
            k.stt(mrow[:, 4 * D:5 * D], mrow[:, 4 * D:5 * D], 1.0, gg[:, 1, :], ALU.add, ALU.mult)
            k.dma("sp", [(MODS[li], mrow[:, :])])
        k.barrier()

    def bc_load(dst, src_row):
        k.dma("sp", [(dst, src_row.partition_broadcast(128))])

    def mod_tiles(st, li, slots):
        res = {}
        for s in slots:
            res[s] = []
            for r in range(2):
                t = k.sb(st, "mod%d" % s, [128, D], F32)
                bc_load(t[:, :], MODS[li, r, s * D:(s + 1) * D])
                res[s].append(t)
        return res

    def normmod(xt_ap, out_ap, gs, sh, tmp, ss, rs):
        k.act(tmp[:, :], xt_ap, AF.Square, accum=ss[:, 0:1])
        k.rstd(rs[:, 0:1], ss[:, 0:1], 1.0 / D)
        k.stt(tmp[:, :], xt_ap, rs[:, 0:1], gs[:, :], ALU.mult, ALU.mult)
        k.tt(out_ap, tmp[:, :], sh[:, :], ALU.add)

    def tok_groups():
        gs_ = []
        for g0 in range(0, NTL, 4):
            gs_.append(list(range(g0, min(NTL, g0 + 4))))
        gs_.append(list(range(NTL, NT)))
        return gs_

    def m1_even(li, j):
        with ExitStack() as st:
            Win = k.sb(st, "win", [128, 8, 2304], BF16)
            k.dma("pool", [(Win[:, c, :], w_in_even[j, c * 128:(c + 1) * 128, :]) for c in range(8)], max_dma_last_dim=4096)
            mods = mod_tiles(st, li, [0, 1])
            gain = k.sb(st, "gain", [128, 1664], F32)
            for (c0, n, src) in ((0, 8, a_qk[j, 0, :]), (512, 8, a_qk[j, 1, :]), (1024, 8, b_qk[j, 0, :]), (1536, 2, b_qk[j, 1, :])):
                k.dma("sp", [(gain[:, c0:c0 + n * 64].rearrange("p (g d) -> p g d", d=64),
                              src.partition_broadcast(128).unsqueeze(1).broadcast_to([128, n, 64]))])
            xts = [k.sb(st, "xt", [128, D], F32) for _ in range(2)]
            tmp = k.sb(st, "tmp", [128, D], F32)
            ss = k.sb(st, "ss", [128, 1], F32); rs = k.sb(st, "rs", [128, 1], F32)
            hb = k.sb(st, "hb", [128, D], BF16)
            hT = k.sb(st, "hT", [128, 8, 128], BF16)
            sq = k.sb(st, "sq", [128, 1664], F32)
            ssg = k.sb(st, "ssg", [128, 26], F32)
            nrm = k.sb(st, "nrm", [128, 1664], F32)
            t1 = k.sb(st, "t1", [128, 1664], F32)
            t2 = k.sb(st, "t2", [128, 1664], F32)
            cs = [k.sb(st, "cs", [128, 2, 64], F32) for _ in range(2)]
            qkb = k.sb(st, "qkb", [128, 1792], BF16)
            vst = [k.sb(st, "vst", [128, 640], BF16) for _ in range(2)]
            stage = [k.sb(st, "stage", [128, 14, 512], BF16) for _ in range(2)]
            tpT = k.ps(st, "tpT", [128, 512])
            P = k.ps(st, "P", [128, 2560])
            TQ = k.ps(st, "TQ", [128, 1024])
            tpTb = tpT[:, :].bitcast(BF16)
            TQb = TQ[:, :].bitcast(BF16)
            secs = ((0, 0, 512), (512, 512, 512), (1536, 1024, 512), (2048, 1536, 128))
            for gi, tiles in enumerate(tok_groups()):
                stg = stage[gi % 2]
                for jt, ti in enumerate(tiles):
                    kind = 0 if ti < NTL else 1
                    xt = xts[ti % 2]
                    k.dma("sp", [(xt[:, :], X[ti * 128:(ti + 1) * 128, :])])
                    if kind == 0:
                        c_ = cs[ti % 2]
                        k.dma("sp", [(c_[:, 0, :], k_cos64[ti * 128:(ti + 1) * 128, :]),
                                     (c_[:, 1, :], k_sin64[ti * 128:(ti + 1) * 128, :])])
                    normmod(xt[:, :], hb[:, :], mods[1][kind], mods[0][kind], tmp, ss, rs)
                    for c in range(8):
                        k.tr(tpTb[:, c * 128:(c + 1) * 128], hb[:, c * 128:(c + 1) * 128], ident_b[:, :])
                    k.cp(hT[:, :, :], tpTb.rearrange("p (c t) -> p c t", t=128), en="act")
                    for n in range(5):
                        w = min(2304, (n + 1) * 512) - n * 512
                        for c in range(8):
                            k.mm(P[:, n * 512:n * 512 + w], hT[:, c, :], Win[:, c, n * 512:n * 512 + w],
                                 start=(c == 0), stop=(c == 7))
                    for (ps0, s0, w) in secs:
                        k.act(sq[:, s0:s0 + w], P[:, ps0:ps0 + w], AF.Square)
                    k.red(ssg[:, :], sq[:, :].rearrange("p (g d) -> p g d", d=64))
                    k.rstd(ssg[:, :], ssg[:, :], 1.0 / 64)
                    for (ps0, s0, w) in secs:
                        g0, ng = s0 // 64, w // 64
                        k.tt(nrm[:, s0:s0 + w].rearrange("p (g d) -> p g d", d=64),
                             P[:, ps0:ps0 + w].rearrange("p (g d) -> p g d", d=64),
                             ssg[:, g0:g0 + ng].unsqueeze(2).broadcast_to([128, ng, 64]), ALU.mult)
                    k.tt(nrm[:, :], nrm[:, :], gain[:, :], ALU.mult)
                    v_ = vst[ti % 2]
                    k.cp(v_[:, 0:512], P[:, 1024:1536], en="act")
                    k.cp(v_[:, 512:640], P[:, 2176:2304], en="act")
                    k.dma("sp", [(Vd[ti * 128:(ti + 1) * 128, 0:640], v_[:, :])])
                    kbd = qkb[:, 1536:1792].rearrange("p (a b d) -> p a b d", a=2, b=2)
                    if kind == 0:
                        c_ = cs[ti % 2]
                        k.tt(t1[:, :].rearrange("p (g d) -> p g d", d=64), nrm[:, :].rearrange("p (g d) -> p g d", d=64),
                             c_[:, 0, :].unsqueeze(1).broadcast_to([128, 26, 64]), ALU.mult)
                        nv = nrm[:, :].rearrange("p (g r h j) -> p g r h j", r=2, h=2, j=16)
                        tv = t2[:, :].rearrange("p (g r h j) -> p g r h j", r=2, h=2, j=16)
                        sv = c_[:, 1, :].rearrange("p (r h j) -> p r h j", r=2, h=2)
                        for r in range(2):
                            for h in range(2):
                                k.tt(tv[:, :, r, h, :], nv[:, :, r, 1 - h, :],
                                     sv[:, r, h, :].unsqueeze(1).broadcast_to([128, 26, 16]), ALU.mult)
                        k.tt(qkb[:, 0:1536], t1[:, 0:1536], t2[:, 0:1536], ALU.add)
                        k.tt(kbd, t1[:, 1536:1664].rearrange("p (a d) -> p a d", d=64).unsqueeze(2).broadcast_to([128, 2, 2, 64]),
                             t2[:, 1536:1664].rearrange("p (a d) -> p a d", d=64).unsqueeze(2).broadcast_to([128, 2, 2, 64]), ALU.add)
                    else:
                        k.cp(qkb[:, 0:1536], nrm[:, 0:1536])
                        k.cp(kbd, nrm[:, 1536:1664].rearrange("p (a d) -> p a d", d=64).unsqueeze(2).broadcast_to([128, 2, 2, 64]))
                    for c in range(14):
                        k.tr(TQb[:, c * 128:(c + 1) * 128], qkb[:, c * 128:(c + 1) * 128], ident_b[:, :])
                    k.cp(stg[:, :, jt * 128:(jt + 1) * 128], TQb[:, 0:1792].rearrange("p (c t) -> p c t", t=128), en="act")
                n = len(tiles) * 128
                t0 = tiles[0] * 128
                k.dma("sp", [(QKT[:, :, t0:t0 + n].rearrange("c p t -> p c t"), stg[:, :, 0:n])])
            k.barrier()

    sctr = [0]

    def attention(groups, scale, lam_cols=None):
        with ExitStack() as st:
            KTb = [k.sb(st, "ktb", [128, TA], BF16) for _ in range(2)]
            VAb = [k.sb(st, "vab", [128, NT, 128], BF16) for _ in range(2)]
            Qb = [k.sb(st, "qb", [128, 512], BF16) for _ in range(3)]
            half_k = any(g["kdim"] == 64 for g in groups)
            if half_k:
                Qz = [[k.sb(st, "qz%d" % h_, [128, 512], BF16) for _ in range(3)] for h_ in range(2)]
                for h_ in range(2):
                    for q_ in Qz[h_]:
                        k.memset(q_[:, :], 0.0)
                qzn = [0, 0]
            PT = [k.sb(st, "pt", [128, 512], BF16) for _ in range(3)]
            S = [k.ps(st, "S", [128, 512]) for _ in range(3)]
            accO = k.ps(st, "accO", [128, 512]); accD = k.ps(st, "accD", [128, 512])
            bR = k.ps(st, "bR", [128, 512]); bS = k.ps(st, "bS", [128, 512])
            osb = [k.sb(st, "osb", [128, 512], F32) for _ in range(2)]
            onA = [k.sb(st, "onA", [128, 512], F32) for _ in range(2)]
            rrow = k.sb(st, "rrow", [128, 512], F32)
            Dt = k.sb(st, "Dt", [128, 512], F32)
            sqd = k.sb(st, "sqd", [128, 512], F32)
            obf = [k.sb(st, "obf", [128, 512], BF16) for _ in range(2)]
            for v_ in VAb:
                k.memset(v_[:, :, :], 1.0)
            qblocks = [(b * 512, 512, 0, NT) for b in range(T // 512)] + [(T, C, NTL, NT)]
            qn = 0
            on_ = 0
            for gi, g in enumerate(groups):
                kb = KTb[gi % 2]; vb = VAb[gi % 2]
                kr, kdim, dv, typeA = g["kr"], g["kdim"], g["dv"], g["typeA"]
                k.dma("sp", [(kb[0:kr, :], g["kt"])])
                if not typeA:
                    k.memset(vb[:, :, 64:65], 1.0)
                vsrc = Vd[:, g["vcol"]:g["vcol"] + dv].rearrange("(t p) d -> p t d", p=128)
                k.dma("sp", [(vb[:, t0_:min(NT, t0_ + 8), 0:dv], vsrc[:, t0_:min(NT, t0_ + 8), :]) for t0_ in range(0, NT, 8)])
                for (q0, nq, klo, khi) in qblocks:
                    for (qap, units) in g["qcs"]:
                        if kdim != 64:
                            qb = Qb[qn % 3]; qn += 1
                            k.dma("sp", [(qb[0:kr, 0:nq], qap[:, q0:q0 + nq])])
                        for ui, (pb, orow) in enumerate(units):
                            n = khi - klo
                            base = sctr[0]; sctr[0] += n
                            if kdim == 64:
                                hsel = pb // 64
                                qb = Qz[hsel][qzn[hsel] % 3]; qzn[hsel] += 1
                                k.dma("sp", [(qb[pb:pb + 64, 0:nq], qap[pb:pb + 64, q0:q0 + nq])])
                                k0, k1 = 0, 128
                            else:
                                k0, k1 = pb, pb + kdim

                            def QK(i, qb=qb, k0=k0, k1=k1, base=base):
                                kt = klo + i
                                k.mm(S[(base + i) % 3][:, 0:nq], kb[k0:k1, kt * 128:(kt + 1) * 128], qb[k0:k1, 0:nq])

                            QK(0)
                            if n > 1:
                                QK(1)
                            for i in range(n):
                                kt = klo + i
                                ix = (base + i) % 3
                                k.act(PT[ix][:, 0:nq], S[ix][:, 0:nq], AF.Exp, scale=scale)
                                if i + 2 < n:
                                    QK(i + 2)
                                if typeA:
                                    k.mm(accO[:, 0:nq], vb[:, kt, 0:128], PT[ix][:, 0:nq], start=(i == 0), stop=(i == n - 1))
                                    k.mm(accD[:, 0:nq], ones_b[:, 0:128], PT[ix][:, 0:nq], start=(i == 0), stop=(i == n - 1))
                                else:
                                    k.mm(accO[0:65, 0:nq], vb[:, kt, 0:65], PT[ix][:, 0:nq], start=(i == 0), stop=(i == n - 1))
                            if typeA:
                                c = ui
                                k.cp(osb[c][:, 0:nq], accO[:, 0:nq], en="act")
                                k.recip(rrow[:, 0:nq], accD[:, 0:nq])
                                k.tt(onA[c][:, 0:nq], osb[c][:, 0:nq], rrow[:, 0:nq], ALU.mult)
                                if c == 1:
                                    nlam, subg = lam_cols
                                    k.stt(Dt[:, 0:nq], onA[1][:, 0:nq], nlam[:, 0:1], onA[0][:, 0:nq], ALU.mult, ALU.add)
                                    k.act(sqd[:, 0:nq], Dt[:, 0:nq], AF.Square)
                                    k.mm(bS[0:1, 0:nq], ones_f[:, 0:1], sqd[:, 0:nq])
                                    k.rstd(rrow[0:1, 0:nq], bS[0:1, 0:nq], 1.0 / 128)
                                    k.mm(bR[:, 0:nq], ones_f[0:1, 0:128], rrow[0:1, 0:nq])
                                    ob = obf[on_ % 2]; on_ += 1
                                    k.stt(ob[:, 0:nq], Dt[:, 0:nq], subg[:, 0:1], bR[:, 0:nq], ALU.mult, ALU.mult)
                                    k.dma("sp", [(OT[orow:orow + 128, q0:q0 + nq], ob[:, 0:nq])])
                            else:
                                k.cp(osb[0][0:65, 0:nq], accO[0:65, 0:nq], en="act")
                                k.recip(rrow[64:65, 0:nq], osb[0][64:65, 0:nq])
                                k.mm(bR[0:64, 0:nq], ones_f[64:65, 0:64], rrow[64:65, 0:nq])
                                ob = obf[on_ % 2]; on_ += 1
                                k.tt(ob[0:64, 0:nq], osb[0][0:64, 0:nq], bR[0:64, 0:nq], ALU.mult)
                                k.dma("sp", [(OT[orow:orow + 64, q0:q0 + nq], ob[0:64, 0:nq])])
            k.barrier()

    def attn_even(li, j):
        lam_init = 0.8 - 0.6 * math.exp(-0.3 * li)
        with ExitStack() as st:
            dl = k.sb(st, "dl", [1, 256], F32)
            pr = k.sb(st, "pr", [1, 128], F32)
            s2 = k.sb(st, "s2", [1, 2], F32)
            nl = k.sb(st, "nl", [1, 1], F32)
            nlam = k.sb(st, "nlam", [128, 1], F32)
            subg = k.sb(st, "subg", [128, 1], F32)
            pl = k.ps(st, "pl", [128, 512])
            k.dma("sp", [(dl[0:1, :], dlam[j:j + 1].rearrange("o a d -> o (a d)"))])
            k.dma("sp", [(subg[:, 0:1], a_subln[j].rearrange("(p o) -> p o", o=1))])
            k.tt(pr[:, 0:64], dl[:, 0:64], dl[:, 64:128], ALU.mult)
            k.tt(pr[:, 64:128], dl[:, 128:192], dl[:, 192:256], ALU.mult)
            k.red(s2[:, :], pr[:, :].rearrange("o (a d) -> o a d", d=64))
            k.act(s2[:, :], s2[:, :], AF.Exp)
            k.tt(nl[:, :], s2[:, 1:2], s2[:, 0:1], ALU.subtract)
            k.ts(nl[:, :], nl[:, :], -lam_init, None, ALU.add)
            k.mm(pl[:, 0:1], ones_f[0:1, 0:128], nl[0:1, 0:1])
            k.cp(nlam[:, :], pl[:, 0:1])
            k.ts(subg[:, :], subg[:, :], 1.0 - lam_init, None, ALU.mult)
            groups = []
            for h in range(4):
                groups.append(dict(kt=QKT[4 + h], kr=128, kdim=64, vcol=h * 128, dv=128, typeA=True,
                                   qcs=[(QKT[h], [(0, h * 128), (64, h * 128)])]))
            for kv in range(2):
                qcs = []
                for qc in (8 + 2 * kv, 9 + 2 * kv):
                    hb0 = 2 * (qc - 8)
                    qcs.append((QKT[qc], [(0, 512 + hb0 * 64), (64, 512 + (hb0 + 1) * 64)]))
                groups.append(dict(kt=QKT[12 + kv], kr=128, kdim=64, vcol=512 + kv * 64, dv=64, typeA=False, qcs=qcs))
            attention(groups, 64 ** -0.5, (nlam, subg))

    def attn_odd(li, j):
        groups = []
        for h in range(16):
            groups.append(dict(kt=KTo[h], kr=96, kdim=96, vcol=h * 64, dv=64, typeA=False,
                               qcs=[(QTo[h], [(0, h * 64)])]))
        attention(groups, 96 ** -0.5)

    def mixout_ffnin(li, wout_d, AFF, IDX):
        with ExitStack() as st:
            Wout = k.sb(st, "wout", [128, 8, D], BF16)
            k.dma("pool", [(Wout[:, c, :], wout_d[c * 128:(c + 1) * 128, :]) for c in range(8)], max_dma_last_dim=4096)
            wr = k.sb(st, "wr", [128, 8, NE], F32)
            k.dma("sp", [(wr[:, :, :], w_router[li].rearrange("(c p) e -> p c e", p=128))])
            mods = mod_tiles(st, li, [2, 3, 4])
            xts = [k.sb(st, "xt", [128, D], F32) for _ in range(2)]
            oTs = [k.sb(st, "oT", [128, 8, 128], BF16) for _ in range(2)]
            tmp = k.sb(st, "tmp", [128, D], F32)
            xn = [k.sb(st, "xn", [128, D], F32) for _ in range(2)]
            hf = k.sb(st, "hf", [128, D], F32)
            hrow = [k.sb(st, "hrow", [128, D + 2], BF16) for _ in range(2)]
            hTf = k.sb(st, "hTf", [128, 8, 128], F32)
            ss = k.sb(st, "ss", [128, 1], F32); rs = k.sb(st, "rs", [128, 1], F32)
            mx = k.sb(st, "mx", [128, 1], F32); sm = k.sb(st, "sm", [128, 1], F32)
            ex = k.sb(st, "ex", [128, NE], F32)
            Y = k.ps(st, "Y", [128, 1024]); TP = k.ps(st, "TP", [128, 1024]); LG = k.ps(st, "LG", [128, 512])
            for ti in range(NT):
                kind = 0 if ti < NTL else 1
                xt = xts[ti % 2]; oT = oTs[ti % 2]; xn_ = xn[ti % 2]; hr = hrow[ti % 2]
                k.dma("sp", [(xt[:, :], X[ti * 128:(ti + 1) * 128, :])])
                k.dma("sp", [(oT[:, :, :], OT[:, ti * 128:(ti + 1) * 128].rearrange("(c p) t -> p c t", p=128))])
                for h2 in range(2):
                    for c in range(8):
                        k.mm(Y[:, h2 * 512:(h2 + 1) * 512], oT[:, c, :], Wout[:, c, h2 * 512:(h2 + 1) * 512],
                             start=(c == 0), stop=(c == 7))
                for h2 in range(2):
                    sl = slice(h2 * 512, (h2 + 1) * 512)
                    k.tt(tmp[:, sl], Y[:, sl], mods[2][kind][:, sl], ALU.mult)
                k.tt(xn_[:, :], xt[:, :], tmp[:, :], ALU.add)
                k.dma("sp", [(X[ti * 128:(ti + 1) * 128, :], xn_[:, :])])
                normmod(xn_[:, :], hf[:, :], mods[4][kind], mods[3][kind], tmp, ss, rs)
                k.cp(hr[:, 0:D], hf[:, :], en="act")
                k.cp(hr[:, D:D + 2].bitcast(I32), tokidx[:, ti:ti + 1])
                k.dma("sp", [(Hd[ti * 128:(ti + 1) * 128, :], hr[:, :])])
                for c in range(8):
                    k.tr(TP[:, c * 128:(c + 1) * 128], hf[:, c * 128:(c + 1) * 128], ident_f[:, :])
                k.cp(hTf[:, :, :], TP[:, :].rearrange("p (c t) -> p c t", t=128), en="act")
                for c in range(8):
                    k.mm(LG[:, 0:NE], hTf[:, c, :], wr[:, c, :], start=(c == 0), stop=(c == 7))
                k.red(mx[:, :], LG[:, 0:NE], op=ALU.max)
                k.ts(mx[:, :], mx[:, :], -1.0, None, ALU.mult)
                k.act(ex[:, :], LG[:, 0:NE], AF.Exp, bias=mx[:, 0:1], accum=sm[:, 0:1])
                k.recip(sm[:, :], sm[:, :])
                k.ts(AFF[:, ti, :], ex[:, :], sm[:, 0:1], None, ALU.mult)
            k.barrier()
        with ExitStack() as st:
            lo = k.sb(st, "lo", [128, 2 * NE], F32)
            tt_ = k.sb(st, "tthr", [128, 2 * NE], F32)
            ge = k.sb(st, "ge", [128, 2 * NE], F32)
            capv = k.sb(st, "capv", [128, 2 * NE], F32)
            cmp_ = k.sb(st, "cmp", [128, NT, NE], BF16)
            cntp = k.sb(st, "cntp", [128, 2 * NE], F32)
            base_t = k.sb(st, "base", [128, NT, NE], F32)
            pos = k.sb(st, "pos", [128, NT, NE], F32)
            off = k.sb(st, "off", [128, NT, NE], F32)
            val = k.sb(st, "val", [128, NT, NE], F32)
            pc = k.ps(st, "pc", [128, 512])
            ncolp = ((NT * NE + 511) // 512) * 512
            pw = k.ps(st, "pw", [128, ncolp])
            pt_ = k.ps(st, "ptot", [128, ncolp])
            k.dma("sp", [(capv[:, :], k_capv)])
            k.dma("sp", [(base_t[:, :, :], k_base.rearrange("p (t e) -> p t e", e=NE))])
            k.memset(lo[:, :], 0.0)
            sets = ((0, NTL, 0), (NTL, NT, NE))

            def compare(thr):
                for (a, b, o) in sets:
                    k.tt(cmp_[:, a:b, :], AFF[:, a:b, :], thr[:, o:o + NE].unsqueeze(1).broadcast_to([128, b - a, NE]), ALU.is_ge)

            step = 0.5
            for it in range(32):
                k.ts(tt_[:, :], lo[:, :], step, None, ALU.add)
                compare(tt_)
                for (a, b, o) in sets:
                    k.red(cntp[:, o:o + NE], cmp_[:, a:b, :].rearrange("p t e -> p e t"))
                k.mm(pc[:, 0:2 * NE], ones_f[:, :], cntp[:, :])
                k.tt(ge[:, :], pc[:, 0:2 * NE], capv[:, :], ALU.is_ge)
                k.stt(lo[:, :], ge[:, :], step, lo[:, :], ALU.mult, ALU.add)
                step *= 0.5
            compare(lo)
            cf = cmp_[:, :, :].rearrange("p t e -> p (t e)")
            ncol = NT * NE
            for c0 in range(0, ncol, 512):
                w = min(512, ncol - c0)
                k.mm(pw[:, c0:c0 + w], tri_b[:, :], cf[:, c0:c0 + w])
                k.mm(pt_[:, c0:c0 + w], ones_b[:, :], cf[:, c0:c0 + w])
            for (a, b, o) in sets:
                k.memset(off[:, a, :], 0.0)
                for t in range(a, b - 1):
                    k.tt(off[:, t + 1, :], off[:, t, :], pt_[:, t * NE:(t + 1) * NE], ALU.add)
            k.tt(pos[:, :, :], pw[:, 0:ncol].rearrange("p (t e) -> p t e", e=NE), off[:, :, :], ALU.add)
            for (a, b, o), cap in zip(sets, (capL, capC)):
                k.ts(val[:, a:b, :], pos[:, a:b, :], float(cap), None, ALU.is_lt)
            k.tt(val[:, :, :], val[:, :, :], cmp_[:, :, :], ALU.mult)
            k.tt(pos[:, :, :], pos[:, :, :], base_t[:, :, :], ALU.add)
            k.ts(pos[:, :, :], pos[:, :, :], -BIG, None, ALU.add)
            k.tt(pos[:, :, :], pos[:, :, :], val[:, :, :], ALU.mult)
            k.ts(pos[:, :, :], pos[:, :, :], BIG, None, ALU.add)
            k.cp(IDX[:, :, :], pos[:, :, :])
            k.barrier()
        with ExitStack() as st:
            hrow = [k.sb(st, "hrow", [128, D + 2], BF16) for _ in range(3)]
            for ti in range(NT):
                hr = hrow[ti % 3]
                k.dma("sp", [(hr[:, :], Hd[ti * 128:(ti + 1) * 128, :])])
                for e in range(NE):
                    k.idma(XS[:, :], hr[:, :], IDX[:, ti, e:e + 1], scatter=True, bounds=reg_xs)
            k.barrier()

    def experts(li):
        with ExitStack() as st:
            WG = [k.sb(st, "wg", [128, 8, D], BF16) for _ in range(2)]
            WU = [k.sb(st, "wu", [128, 8, D], BF16) for _ in range(2)]
            WD = [k.sb(st, "wd", [128, 8, D], BF16) for _ in range(2)]
            wrf = k.sb(st, "wrf", [128, 8, NE], F32)
            wrb = k.sb(st, "wrb", [128, 8, NE], BF16)
            k.dma("sp", [(wrf[:, :, :], w_router[li].rearrange("(c p) e -> p c e", p=128))])
            k.cp(wrb[:, :, :], wrf[:, :, :])
            mods = mod_tiles(st, li, [5])
            xsT = k.sb(st, "xsT", [128, 8, SLOTS], BF16)
            gT = k.sb(st, "gT", [128, 8, SLOTS], BF16)
            xst = [k.sb(st, "xst", [128, D + 2], BF16) for _ in range(3)]
            sa = [k.sb(st, "sa", [128, 512], F32) for _ in range(2)]
            pay = [k.sb(st, "pay", [128, D], F32) for _ in range(2)]
            mx = k.sb(st, "mx", [128, 1], F32); sm = k.sb(st, "sm", [128, 1], F32)
            ex = k.sb(st, "ex", [128, NE], F32)
            TPb = k.ps(st, "TPx", [128, 512])
            LG = k.ps(st, "LGx", [128, 512])
            PA = [k.ps(st, "PA", [128, 512]) for _ in range(2)]
            PB = [k.ps(st, "PB", [128, 512]) for _ in range(2)]
            PY = k.ps(st, "PY", [128, 1024])
            TPv = TPb[:, :].bitcast(BF16)
            stiles = []
            r = 0
            while r < capL:
                n = min(128, capL - r); stiles.append((r, n, 0)); r += n
            stiles.append((capL, capC, 1))
            nblk = (SLOTS + 511) // 512
            bw = (SLOTS + nblk - 1) // nblk
            blocks = [(b * bw, min(bw, SLOTS - b * bw)) for b in range(nblk)]
            cnt = 0
            pcnt = 0
            nst = len(stiles)
            tix = [k.sb(st, "tix", [128, 1], I32) for _ in range(2 * nst)]
            gate = [k.sb(st, "gate", [128, 1], F32) for _ in range(2 * nst)]
            for e in range(NE):
                wg, wu, wd = WG[e % 2], WU[e % 2], WD[e % 2]
                for (w_sb, w_d) in ((wg, w_eg), (wu, w_eu), (wd, w_ed)):
                    k.dma("pool", [(w_sb[:, c, :], w_d[li, e, c * 128:(c + 1) * 128, :]) for c in range(8)], max_dma_last_dim=4096)
                tinfo = []
                for si_, (r0, n, kind) in enumerate(stiles):
                    xs = xst[cnt % 3]; cnt += 1
                    tx = tix[(e % 2) * nst + si_]; gt = gate[(e % 2) * nst + si_]
                    k.dma("sp", [(xs[0:n, :], XS[e * SLOTS + r0:e * SLOTS + r0 + n, :])])
                    k.cp(tx[0:n, :], xs[0:n, D:D + 2].bitcast(I32))
                    for c in range(8):
                        k.tr(TPv[:, c * 128:c * 128 + n], xs[0:n, c * 128:(c + 1) * 128], ident_b[0:n, 0:n])
                    k.cp(xsT[:, :, r0:r0 + n], TPv.rearrange("p (c t) -> p c t", t=128)[:, :, 0:n], en="act")
                    for c in range(8):
                        k.mm(LG[0:n, 0:NE], xsT[:, c, r0:r0 + n], wrb[:, c, :], start=(c == 0), stop=(c == 7))
                    k.red(mx[0:n, :], LG[0:n, 0:NE], op=ALU.max)
                    k.ts(mx[0:n, :], mx[0:n, :], -1.0, None, ALU.mult)
                    k.act(ex[0:n, :], LG[0:n, 0:NE], AF.Exp, bias=mx[0:n, 0:1], accum=sm[0:n, 0:1])
                    k.recip(sm[0:n, :], sm[0:n, :])
                    k.tt(gt[0:n, :], ex[0:n, e:e + 1], sm[0:n, :], ALU.mult)
                    tinfo.append((r0, n, kind, tx, gt))
                for f in range(8):
                    for (b0, bn) in blocks:
                        pa = PA[pcnt % 2]; pb_ = PB[pcnt % 2]; sa_ = sa[pcnt % 2]; pcnt += 1
                        for c in range(8):
                            k.mm(pa[:, 0:bn], wg[:, c, f * 128:(f + 1) * 128], xsT[:, c, b0:b0 + bn], start=(c == 0), stop=(c == 7))
                        for c in range(8):
                            k.mm(pb_[:, 0:bn], wu[:, c, f * 128:(f + 1) * 128], xsT[:, c, b0:b0 + bn], start=(c == 0), stop=(c == 7))
                        k.act(sa_[:, 0:bn], pa[:, 0:bn], AF.Silu)
                        k.tt(gT[:, f, b0:b0 + bn], sa_[:, 0:bn], pb_[:, 0:bn], ALU.mult)
                for ti_, (r0, n, kind, tx, gt) in enumerate(tinfo):
                    py = pay[(e * len(tinfo) + ti_) % 2]
                    for h2 in range(2):
                        for f in range(8):
                            k.mm(PY[0:n, h2 * 512:(h2 + 1) * 512], gT[:, f, r0:r0 + n], wd[:, f, h2 * 512:(h2 + 1) * 512],
                                 start=(f == 0), stop=(f == 7))
                    for h2 in range(2):
                        sl = slice(h2 * 512, (h2 + 1) * 512)
                        k.stt(py[0:n, sl], PY[0:n, sl], gt[0:n, 0:1], mods[5][kind][0:n, sl], ALU.mult, ALU.mult)
                    k.idma(X[:, :], py[0:n, :], tx[0:n, 0:1], scatter=True, bounds=reg_x, add=True, xw=[xtok])
            k.barrier()

    def m1_odd(li, j):
        with ExitStack() as st:
            Win = k.sb(st, "wino", [128, 8, 416], BF16)
            Wq = k.sb(st, "wq", [128, 2, 1536], BF16)
            Wkv = k.sb(st, "wkv", [128, 2048], BF16)
            k.dma("pool", [(Win[:, c, :], w_in_odd[j, c * 128:(c + 1) * 128, :]) for c in range(8)], max_dma_last_dim=4096)
            k.dma("pool", [(Wq[:, c, :], w_q_up[j, c * 128:(c + 1) * 128, :]) for c in range(2)], max_dma_last_dim=4096)
            k.dma("pool", [(Wkv[:, :], w_kv_up[j])], max_dma_last_dim=4096)
            mods = mod_tiles(st, li, [0, 1])
            gq = k.sb(st, "gq", [128, 256], F32); gkv = k.sb(st, "gkv", [128, 128], F32)
            gqk = k.sb(st, "gqk", [128, 2, 96], F32)
            bc_load(gq[:, :], q_norm[j]); bc_load(gkv[:, :], kv_norm[j])
            bc_load(gqk[:, 0, :], qk_norm[j, 0]); bc_load(gqk[:, 1, :], qk_norm[j, 1])
            invn = k.sb(st, "invn", [128, 3], F32)
            k.memset(invn[:, 0:1], 1.0 / 256); k.memset(invn[:, 1:2], 1.0 / 128); k.memset(invn[:, 2:3], 1.0 / 32)
            xts = [k.sb(st, "xt", [128, D], F32) for _ in range(2)]
            tmp = k.sb(st, "tmp", [128, D], F32)
            ss = k.sb(st, "ss", [128, 1], F32); rs = k.sb(st, "rs", [128, 1], F32)
            hb = k.sb(st, "hb", [128, D], BF16)
            hT = k.sb(st, "hT", [128, 8, 128], BF16)
            sqp = k.sb(st, "sqp", [128, 416], F32)
            s3 = k.sb(st, "s3", [128, 3], F32)
            cn = k.sb(st, "cn", [128, 384], BF16)
            cT = k.sb(st, "cT", [128, 3, 128], BF16)
            krn = k.sb(st, "krn", [128, 32], F32)
            krr = k.sb(st, "krr", [128, 32], F32)
            kt1 = k.sb(st, "kt1", [128, 32], F32); kt2 = k.sb(st, "kt2", [128, 32], F32)
            sqq = k.sb(st, "sqq", [128, 2048], F32)
            sq16 = k.sb(st, "sq16", [128, 32], F32)
            qn_ = k.sb(st, "qn", [128, 16, 96], F32)
            qt1 = k.sb(st, "qt1", [128, 16, 32], F32); qt2 = k.sb(st, "qt2", [128, 16, 32], F32)
            qbf = k.sb(st, "qbf", [128, 16, 96], BF16)
            kbf = k.sb(st, "kbf", [128, 16, 96], BF16)
            cs = [k.sb(st, "cs", [128, 2, 32], F32) for _ in range(2)]
            vst = [k.sb(st, "vst", [128, 16, 64], BF16) for _ in range(2)]
            stq = [k.sb(st, "stq", [96, 16, 512], BF16) for _ in range(2)]
            stk = [k.sb(st, "stk", [96, 16, 512], BF16) for _ in range(2)]
            tpT = k.ps(st, "tpT", [128, 512]); P = k.ps(st, "P", [128, 512]); cTp = k.ps(st, "cTp", [128, 512])
            Q3 = k.ps(st, "Q3", [128, 1536]); KV = k.ps(st, "KV", [128, 1024])
            tpTb = tpT[:, :].bitcast(BF16); cTb = cTp[:, :].bitcast(BF16)
            Q3b = Q3[:, :].bitcast(BF16); KVb = KV[:, :].bitcast(BF16)

            def rope32(dst, src, c_, shp, t1, t2):
                G = shp
                k.tt(t1, src, c_[:, 0, :].unsqueeze(1).broadcast_to([128, G, 32]), ALU.mult)
                sv = c_[:, 1, :].rearrange("p (r h j) -> p r h j", r=2, h=2)
                s5 = src.rearrange("p g (r h j) -> p g r h j", r=2, h=2)
                t5 = t2.rearrange("p g (r h j) -> p g r h j", r=2, h=2)
                for r in range(2):
                    for h in range(2):
                        k.tt(t5[:, :, r, h, :], s5[:, :, r, 1 - h, :], sv[:, r, h, :].unsqueeze(1).broadcast_to([128, G, 8]), ALU.mult)
                k.tt(dst, t1, t2, ALU.add)

            for gi, tiles in enumerate(tok_groups()):
                sq_, sk_ = stq[gi % 2], stk[gi % 2]
                for jt, ti in enumerate(tiles):
                    kind = 0 if ti < NTL else 1
                    xt = xts[ti % 2]
                    k.dma("sp", [(xt[:, :], X[ti * 128:(ti + 1) * 128, :])])
                    c_ = cs[ti % 2]
                    if kind == 0:
                        k.dma("sp", [(c_[:, 0, :], k_cos32[ti * 128:(ti + 1) * 128, :]),
                                     (c_[:, 1, :], k_sin32[ti * 128:(ti + 1) * 128, :])])
                    normmod(xt[:, :], hb[:, :], mods[1][kind], mods[0][kind], tmp, ss, rs)
                    for c in range(8):
                        k.tr(tpTb[:, c * 128:(c + 1) * 128], hb[:, c * 128:(c + 1) * 128], ident_b[:, :])
                    k.cp(hT[:, :, :], tpTb.rearrange("p (c t) -> p c t", t=128), en="act")
                    for c in range(8):
                        k.mm(P[:, 0:416], hT[:, c, :], Win[:, c, :], start=(c == 0), stop=(c == 7))
                    k.act(sqp[:, :], P[:, 0:416], AF.Square)
                    k.red(s3[:, 0:1], sqp[:, 0:256]); k.red(s3[:, 1:2], sqp[:, 256:384]); k.red(s3[:, 2:3], sqp[:, 384:416])
                    k.tt(s3[:, :], s3[:, :], invn[:, :], ALU.mult)
                    k.rstd(s3[:, :], s3[:, :], 1.0)
                    k.stt(cn[:, 0:256], P[:, 0:256], s3[:, 0:1], gq[:, :], ALU.mult, ALU.mult)
                    k.stt(cn[:, 256:384], P[:, 256:384], s3[:, 1:2], gkv[:, :], ALU.mult, ALU.mult)
                    k.stt(krn[:, :], P[:, 384:416], s3[:, 2:3], gqk[:, 1, 64:96], ALU.mult, ALU.mult)
                    if kind == 0:
                        rope32(krr[:, :].unsqueeze(1), krn[:, :].unsqueeze(1), c_, 1, kt1[:, :].unsqueeze(1), kt2[:, :].unsqueeze(1))
                    else:
                        k.cp(krr[:, :], krn[:, :])
                    for c in range(3):
                        k.tr(cTb[:, c * 128:(c + 1) * 128], cn[:, c * 128:(c + 1) * 128], ident_b[:, :])
                    k.cp(cT[:, :, :], cTb[:, 0:384].rearrange("p (c t) -> p c t", t=128), en="act")
                    for n in range(3):
                        for c in range(2):
                            k.mm(Q3[:, n * 512:(n + 1) * 512], cT[:, c, :], Wq[:, c, n * 512:(n + 1) * 512], start=(c == 0), stop=(c == 1))
                    for n in range(3):
                        k.act(sqq[:, n * 512:(n + 1) * 512], Q3[:, n * 512:(n + 1) * 512], AF.Square)
                    sv3 = sqq[:, 0:1536].rearrange("p (h d) -> p h d", d=96)
                    q3v = Q3[:, :].rearrange("p (h d) -> p h d", d=96)
                    k.red(sq16[:, 0:16], sv3[:, :, 0:64]); k.red(sq16[:, 16:32], sv3[:, :, 64:96])
                    k.rstd(sq16[:, 0:16], sq16[:, 0:16], 1.0 / 64)
                    k.rstd(sq16[:, 16:32], sq16[:, 16:32], 1.0 / 32)
                    k.tt(qn_[:, :, 0:64], q3v[:, :, 0:64], sq16[:, 0:16].unsqueeze(2).broadcast_to([128, 16, 64]), ALU.mult)
                    k.tt(qn_[:, :, 64:96], q3v[:, :, 64:96], sq16[:, 16:32].unsqueeze(2).broadcast_to([128, 16, 32]), ALU.mult)
                    k.tt(qn_[:, :, :], qn_[:, :, :], gqk[:, 0, :].unsqueeze(1).broadcast_to([128, 16, 96]), ALU.mult)
                    k.cp(qbf[:, :, 0:64], qn_[:, :, 0:64])
                    if kind == 0:
                        rope32(qbf[:, :, 64:96], qn_[:, :, 64:96], c_, 16, qt1[:, :, :], qt2[:, :, :])
                    else:
                        k.cp(qbf[:, :, 64:96], qn_[:, :, 64:96])
                    for h in range(16):
                        k.tr(Q3b[0:96, h * 128:(h + 1) * 128], qbf[:, h, :], ident_b[:, :])
                    k.cp(sq_[:, :, jt * 128:(jt + 1) * 128], Q3b[0:96, 0:2048].rearrange("p (h t) -> p h t", t=128), en="act")
                    v_ = vst[ti % 2]
                    for hf_ in range(2):
                        for n in range(2):
                            c0 = hf_ * 1024 + n * 512
                            k.mm(KV[:, n * 512:(n + 1) * 512], cT[:, 2, :], Wkv[:, c0:c0 + 512])
                        kv3 = KV[:, :].rearrange("p (h d) -> p h d", d=128)
                        for n in range(2):
                            k.act(sqq[:, n * 512:(n + 1) * 512], KV[:, n * 512:(n + 1) * 512], AF.Square)
                        k.red(sq16[:, 0:8], sqq[:, 0:1024].rearrange("p (h d) -> p h d", d=128)[:, :, 0:64])
                        k.rstd(sq16[:, 0:8], sq16[:, 0:8], 1.0 / 64)
                        hs = slice(hf_ * 8, hf_ * 8 + 8)
                        k.tt(qn_[:, 0:8, 0:64], kv3[:, :, 0:64], sq16[:, 0:8].unsqueeze(2).broadcast_to([128, 8, 64]), ALU.mult)
                        k.tt(kbf[:, hs, 0:64], qn_[:, 0:8, 0:64], gqk[:, 1, 0:64].unsqueeze(1).broadcast_to([128, 8, 64]), ALU.mult)
                        k.cp(v_[:, hs, :], kv3[:, :, 64:128], en="act")
                    k.cp(kbf[:, :, 64:96], krr[:, :].unsqueeze(1).broadcast_to([128, 16, 32]))
                    k.dma("sp", [(Vd[ti * 128:(ti + 1) * 128, :], v_[:, :, :].rearrange("p h d -> p (h d)"))])
                    for h in range(16):
                        k.tr(KVb[0:96, h * 128:(h + 1) * 128], kbf[:, h, :], ident_b[:, :])
                    k.cp(sk_[:, :, jt * 128:(jt + 1) * 128], KVb[0:96, 0:2048].rearrange("p (h t) -> p h t", t=128), en="act")
                n = len(tiles) * 128
                t0 = tiles[0] * 128
                k.dma("sp", [(QTo[:, :, t0:t0 + n].rearrange("h p t -> p h t"), sq_[:, :, 0:n])])
                k.dma("sp", [(KTo[:, :, t0:t0 + n].rearrange("h p t -> p h t"), sk_[:, :, 0:n])])
            k.barrier()

    lst = ExitStack()
    AFF = k.sb(lst, "AFF", [128, NT, NE], F32)
    IDX = k.sb(lst, "IDX", [128, NT, NE], I32)
    k.barrier()
    upto = getattr(cfg, "upto", None)
    for li in range(L):
        j = li // 2
        if li % 2 == 0:
            m1_even(li, j)
            if upto == "m1" and li == L - 1:
                break
            attn_even(li, j)
            if upto == "attn" and li == L - 1:
                break
            mixout_ffnin(li, w_out_even[j], AFF, IDX)
        else:
            m1_odd(li, j)
            if upto == "m1" and li == L - 1:
                break
            attn_odd(li, j)
            if upto == "attn" and li == L - 1:
                break
            mixout_ffnin(li, w_out_odd[j], AFF, IDX)
        if upto == "ffnin" and li == L - 1:
            break
        experts(li)
    k.barrier()
    k.dma("sp", [(out_d, X[0:T, :])], sembuf=xtok)
    k.barrier()
    lst.close()
    gst.close()
    k.dbg = dict(X=X, MODS=MODS, QKT=QKT, Vd=Vd, OT=OT, Hd=Hd, XS=XS, QTo=QTo, KTo=KTo)
    return k


def host_consts(cfg):
    T, NT, NTL = cfg.T, cfg.NT, cfg.NTL
    c = {}
    c["k_ident"] = np.eye(128, dtype=np.float32)
    c["k_tri"] = np.triu(np.ones((128, 128), np.float32), 1)
    c["k_tokidx"] = (np.arange(NT, dtype=np.int32)[None, :] * 128 + np.arange(128, dtype=np.int32)[:, None]).astype(np.int32)
    t = np.arange(T)
    row = (t // GRID_W).astype(np.float32); col = (t % GRID_W).astype(np.float32)
    for rd, nm in ((64, "64"), (32, "32")):
        half = rd // 2
        fr = (ROPE_THETA ** (-np.arange(0, half, 2, dtype=np.float32) / half)).astype(np.float32)
        ar = (row[:, None] * fr[None, :]).astype(np.float32); ac = (col[:, None] * fr[None, :]).astype(np.float32)
        cr, sr, cc_, sc_ = np.cos(ar), np.sin(ar), np.cos(ac), np.sin(ac)
        c["k_cos" + nm] = np.concatenate([cr, cr, cc_, cc_], axis=1).astype(np.float32)
        c["k_sin" + nm] = np.concatenate([-sr, sr, -sc_, sc_], axis=1).astype(np.float32)
    base = np.zeros((128, NT, NE), np.float32)
    base[:, :, :] = (np.arange(NE, dtype=np.float32) * cfg.SLOTS)[None, None, :]
    base[:, NTL:, :] += cfg.capL
    c["k_base"] = base.reshape(128, NT * NE)
    capv = np.zeros((128, 2 * NE), np.float32); capv[:, :NE] = cfg.capL; capv[:, NE:] = cfg.capC
    c["k_capv"] = capv
    return c


def make_in_maps(cfg, inputs):
    consts = host_consts(cfg)
    B = inputs["x"].shape[0]
    wnames = ["w_ada", "b_ada", "norm_mix", "norm_ffn", "w_in_even", "a_qk_norm", "diff_lambda", "a_subln",
              "b_qk_norm", "w_out_even", "w_router", "w_exp_gate", "w_exp_up", "w_exp_down"]
    if cfg.n_odd:
        wnames += ["w_in_odd", "mla_q_norm", "w_q_up", "mla_kv_norm", "w_kv_up", "mla_qk_norm", "w_out_odd"]
    shared = {n: np.ascontiguousarray(np.asarray(inputs[n], dtype=np.float32)) for n in wnames}
    shared.update(consts)
    maps = []
    cctx = np.asarray(inputs["c_ctx"], np.float32)
    for b in range(B):
        m = dict(shared)
        m["x"] = np.ascontiguousarray(np.asarray(inputs["x"][b], np.float32))
        m["ctx"] = np.ascontiguousarray(np.asarray(inputs["ctx"][b], np.float32))
        cb = np.asarray(inputs["c"][b], np.float32)
        m["cc"] = np.ascontiguousarray(np.stack([cb.reshape(8, 128).T, cctx.reshape(8, 128).T], axis=-1))
        maps.append(m)
    return maps


def kernel(**inputs):
    cfg = Cfg()
    kb = build(cfg)
    maps = make_in_maps(cfg, inputs)
    res = run_bass_kernel_spmd(kb.nc, maps, core_ids=list(range(len(maps))))
    return np.stack([np.asarray(r["out"], dtype=np.float32) for r in res.results], axis=0)
```

```python
import math
from contextlib import ExitStack
import numpy as np
import concourse.bass as bass
import concourse.mybir as mybir
from concourse.bass_utils import run_bass_kernel_spmd

F32 = mybir.dt.float32
BF16 = mybir.dt.bfloat16
I32 = mybir.dt.int32
AF = mybir.ActivationFunctionType
ALU = mybir.AluOpType
AX = mybir.AxisListType

D = 1024
NE = 16
EPS = 1e-6
GRID_W = 64
ROPE_THETA = 10000.0


class Cfg:
    def __init__(self, T=8192, C=256, depth=4):
        self.T, self.C, self.L = T, C, depth
        self.TA = T + C
        self.NTL, self.NTC = T // 128, C // 128
        self.NT = self.NTL + self.NTC
        self.capL = 2 * T // NE
        self.capC = 2 * C // NE
        self.SLOTS = self.capL + self.capC
        self.n_even = (depth + 1) // 2
        self.n_odd = depth // 2
        self.QB = 512


class Buf:
    __slots__ = ("name", "w", "r", "sid")

    def __init__(self, name):
        self.name, self.w, self.r, self.sid = name, None, {}, None


class Eng:
    def __init__(self, e, sid):
        self.e, self.sid, self.waited = e, sid, {}


class KB:
    def __init__(self, cfg):
        self.cfg = cfg
        nc = self.nc = bass.Bass("TRN2", target_bir_lowering=False)
        self.sem = {}
        self.E = {}
        for n, e in (("pe", nc.tensor), ("act", nc.scalar), ("dve", nc.vector),
                     ("pool", nc.gpsimd), ("sp", nc.sync)):
            self.E[n] = Eng(e, self.newsem("e_" + n))
        self.bufs = {}
        self.uid = 0
        self.free_sids = {}

    def dsem(self, q, name):
        fl = self.free_sids.get(q)
        if fl:
            return fl.pop()
        return self.newsem(name)

    def _release(self, nm):
        b = self.bufs.pop(nm, None)
        if b is not None and b.sid:
            for q, sid in b.sid.items():
                self.free_sids.setdefault(q, []).append(sid)

    def newsem(self, name):
        sid = len(self.sem)
        self.sem[sid] = [self.nc.alloc_semaphore(name), 0]
        return sid

    def token(self, name):
        b = Buf(name)
        self.bufs[name] = b
        return b

    def _wait(self, E, sid, val):
        if E.waited.get(sid, 0) < val:
            E.e.wait_ge(self.sem[sid][0], val)
            E.waited[sid] = val

    def _deps(self, E, rb, wb, is_pe):
        need = {}
        for b in rb:
            if b.w is not None:
                need[b.w[0]] = max(need.get(b.w[0], 0), b.w[1])
        for b in wb:
            if b.w is not None:
                need[b.w[0]] = max(need.get(b.w[0], 0), b.w[1])
            for sid, v in b.r.items():
                need[sid] = max(need.get(sid, 0), v)
        for sid, v in need.items():
            if is_pe and sid == E.sid:
                continue
            self._wait(E, sid, v)

    def _mark(self, rb, wb, mark):
        sid, v = mark
        for b in rb:
            if b.r.get(sid, 0) < v:
                b.r[sid] = v
        for b in wb:
            b.w = mark
            b.r = {}

    def _bufs_of(self, aps, extra):
        out = []
        for a in aps:
            if a is None or isinstance(a, (int, float)):
                continue
            b = self.bufs.get(a.name)
            if b is not None and b not in out:
                out.append(b)
        for b in extra:
            if b not in out:
                out.append(b)
        return out

    def op(self, en, fn, outs, ins, xr=(), xw=()):
        E = self.E[en]
        rb = self._bufs_of(ins, xr)
        wb = self._bufs_of(outs, xw)
        self._deps(E, rb, wb, en == "pe")
        ins_ = fn(E.e)
        s = self.sem[E.sid]
        s[1] += 1
        ins_.then_inc(s[0], 1)
        self._mark(rb, wb, (E.sid, s[1]))

    def dma(self, q, pairs, xr=(), xw=(), sembuf=None, **kw):
        E = self.E[q]
        outs = [p[0] for p in pairs]
        ins = [p[1] for p in pairs]
        rb = self._bufs_of(ins, xr)
        wb = self._bufs_of(outs, xw)
        sb = sembuf
        if sb is None:
            cand = self._bufs_of(outs, ()) or self._bufs_of(ins, ())
            sb = cand[0]
        if sb.sid is None:
            sb.sid = {}
        if q not in sb.sid:
            sb.sid[q] = self.dsem(q, "d_%s_%s" % (q, sb.name))
        self._deps(E, rb, wb, False)
        s = self.sem[sb.sid[q]]
        for o, i in pairs:
            E.e.dma_start(out=o, in_=i, **kw).then_inc(s[0], 16)
            s[1] += 16
        self._mark(rb, wb, (sb.sid[q], s[1]))

    def idma(self, out, in_, idx_ap, scatter, bounds, add=False, xr=(), xw=()):
        E = self.E["pool"]
        rb = self._bufs_of([in_, idx_ap], xr)
        wb = self._bufs_of([out], xw)
        cand = self._bufs_of([in_] if scatter else [out], ())
        sb = cand[0]
        if sb.sid is None:
            sb.sid = {}
        if "ind" not in sb.sid:
            sb.sid["ind"] = self.dsem("ind", "d_ind_" + sb.name)
        self._deps(E, rb, wb, False)
        s = self.sem[sb.sid["ind"]]
        off = bass.IndirectOffsetOnAxis(ap=idx_ap, axis=0)
        kw = {}
        if add:
            kw["compute_op"] = ALU.add
            kw["oob_is_err"] = True
        else:
            kw["oob_is_err"] = False
        E.e.indirect_dma_start(out=out, out_offset=off if scatter else None, in_=in_,
                               in_offset=None if scatter else off,
                               bounds_check=bounds, **kw).then_inc(s[0], 16)
        s[1] += 16
        self._mark(rb, wb, (sb.sid["ind"], s[1]))

    def barrier(self):
        for E in self.E.values():
            for sid, (h, c) in self.sem.items():
                if c > 0:
                    self._wait(E, sid, c)
        for b in self.bufs.values():
            b.w, b.r = None, {}

    def sb(self, st, name, shape, dt):
        self.uid += 1
        nm = "%s_%d" % (name, self.uid)
        t = st.enter_context(self.nc.sbuf_tensor(nm, list(shape), dt))
        self.bufs[nm] = Buf(nm)
        st.callback(self._release, nm)
        return t

    def ps(self, st, name, shape, dt=F32):
        self.uid += 1
        nm = "%s_%d" % (name, self.uid)
        t = st.enter_context(self.nc.psum_tensor(nm, list(shape), dt))
        self.bufs[nm] = Buf(nm)
        st.callback(self._release, nm)
        return t

    def mm(self, out, lhsT, rhs, start=True, stop=True):
        self.op("pe", lambda e: e.matmul(out, lhsT, rhs, start=start, stop=stop), [out], [lhsT, rhs])

    def tr(self, out, in_, ident):
        self.op("pe", lambda e: e.transpose(out, in_, ident), [out], [in_, ident])

    def act(self, out, in_, func, scale=1.0, bias=0.0, accum=None, en="act"):
        kw = {}
        if accum is not None:
            kw["accum_out"] = accum
        self.op("act", lambda e: e.activation(out=out, in_=in_, func=func, bias=bias, scale=scale, **kw),
                [out, accum], [in_, bias, scale])

    def tt(self, out, in0, in1, op, en="dve"):
        self.op(en, lambda e: e.tensor_tensor(out=out, in0=in0, in1=in1, op=op), [out], [in0, in1])

    def ts(self, out, in0, s1, s2, op0, op1=None, en="dve"):
        if op1 is None:
            self.op(en, lambda e: e.tensor_scalar(out=out, in0=in0, scalar1=s1, scalar2=None, op0=op0),
                    [out], [in0, s1])
        else:
            self.op(en, lambda e: e.tensor_scalar(out=out, in0=in0, scalar1=s1, scalar2=s2, op0=op0, op1=op1),
                    [out], [in0, s1, s2])

    def stt(self, out, in0, sc, in1, op0, op1, en="dve"):
        self.op(en, lambda e: e.scalar_tensor_tensor(out=out, in0=in0, scalar=sc, in1=in1, op0=op0, op1=op1),
                [out], [in0, sc, in1])

    def red(self, out, in_, op=ALU.add, en="dve"):
        self.op(en, lambda e: e.tensor_reduce(out=out, in_=in_, axis=AX.X, op=op), [out], [in_])

    def cp(self, out, in_, en="dve"):
        if en == "act":
            self.op("act", lambda e: e.activation(out=out, in_=in_, func=AF.Copy), [out], [in_])
        else:
            self.op(en, lambda e: e.tensor_copy(out=out, in_=in_), [out], [in_])

    def recip(self, out, in_):
        self.op("dve", lambda e: e.reciprocal(out=out, in_=in_), [out], [in_])

    def memset(self, ap, v, en="dve"):
        self.op(en, lambda e: e.memset(ap, v), [ap], [])

    def rstd(self, out, ss, inv_n):
        self.act(out, ss, AF.Sqrt, scale=inv_n, bias=EPS)
        self.recip(out, out)


def build(cfg):
    k = KB(cfg)
    nc = k.nc
    T, C, TA, L, NT, NTL, NTC = cfg.T, cfg.C, cfg.TA, cfg.L, cfg.NT, cfg.NTL, cfg.NTC
    SLOTS, capL, capC = cfg.SLOTS, cfg.capL, cfg.capC
    ne, no = cfg.n_even, cfg.n_odd

    def din(name, shape, dt=F32):
        return nc.dram_tensor(name, list(shape), dt, kind="ExternalInput").ap()

    def dint(name, shape, dt):
        return nc.dram_tensor(name, list(shape), dt, kind=("ExternalOutput" if getattr(cfg, "debug", False) else "Internal")).ap()

    x_in = din("x", [T, D]); ctx_in = din("ctx", [C, D]); cc_in = din("cc", [128, 8, 2])
    w_ada = din("w_ada", [L, D, 6 * D]); b_ada = din("b_ada", [L, 6 * D])
    norm_mix = din("norm_mix", [L, D]); norm_ffn = din("norm_ffn", [L, D])
    w_in_even = din("w_in_even", [ne, D, 2304]); a_qk = din("a_qk_norm", [ne, 2, 64])
    dlam = din("diff_lambda", [ne, 4, 64]); a_subln = din("a_subln", [ne, 128])
    b_qk = din("b_qk_norm", [ne, 2, 64]); w_out_even = din("w_out_even", [ne, D, D])
    if no:
        w_in_odd = din("w_in_odd", [no, D, 416]); q_norm = din("mla_q_norm", [no, 256])
        w_q_up = din("w_q_up", [no, 256, 1536]); kv_norm = din("mla_kv_norm", [no, 128])
        w_kv_up = din("w_kv_up", [no, 128, 2048]); qk_norm = din("mla_qk_norm", [no, 2, 96])
        w_out_odd = din("w_out_odd", [no, D, D])
    w_router = din("w_router", [L, D, NE])
    w_eg = din("w_exp_gate", [L, NE, D, D]); w_eu = din("w_exp_up", [L, NE, D, D])
    w_ed = din("w_exp_down", [L, NE, D, D])
    k_ident = din("k_ident", [128, 128]); k_tri = din("k_tri", [128, 128])
    k_tokidx = din("k_tokidx", [128, NT], I32)
    k_cos64 = din("k_cos64", [T, 64]); k_sin64 = din("k_sin64", [T, 64])
    k_cos32 = din("k_cos32", [T, 32]); k_sin32 = din("k_sin32", [T, 32])
    k_base = din("k_base", [128, NT * NE]); k_capv = din("k_capv", [128, 2 * NE])
    out_d = nc.dram_tensor("out", [T, D], F32, kind="ExternalOutput").ap()

    X = dint("X", [TA, D], F32)
    MODS = dint("MODS", [L, 2, 6 * D], F32)
    QKT = dint("QKT", [14, 128, TA], BF16)
    QTo = dint("QTo", [16, 96, TA], BF16)
    KTo = dint("KTo", [16, 96, TA], BF16)
    Vd = dint("Vd", [TA, D], BF16)
    OT = dint("OT", [D, TA], BF16)
    Hd = dint("Hd", [TA, D + 2], BF16)
    XS = dint("XS", [NE * SLOTS + 128, D + 2], BF16)
    BIG = float(NE * SLOTS + 4096)
    reg_xs = nc.gpsimd.alloc_register("bnd_xs")
    nc.gpsimd.reg_mov(reg_xs, NE * SLOTS - 1)
    reg_x = nc.gpsimd.alloc_register("bnd_x")
    nc.gpsimd.reg_mov(reg_x, TA - 1)

    gst = ExitStack()
    ident_f = k.sb(gst, "identf", [128, 128], F32)
    ident_b = k.sb(gst, "identb", [128, 128], BF16)
    tri_f = k.sb(gst, "trif", [128, 128], F32)
    tri_b = k.sb(gst, "trib", [128, 128], BF16)
    ones_f = k.sb(gst, "onesf", [128, 128], F32)
    ones_b = k.sb(gst, "onesb", [128, 128], BF16)
    tokidx = k.sb(gst, "tokidx", [128, NT], I32)
    xtok = k.token("Xtok")

    k.dma("sp", [(ident_f[:, :], k_ident)])
    k.dma("sp", [(tri_f[:, :], k_tri)])
    k.dma("sp", [(tokidx[:, :], k_tokidx)])
    k.cp(ident_b[:, :], ident_f[:, :])
    k.cp(tri_b[:, :], tri_f[:, :])
    k.memset(ones_f[:, :], 1.0)
    k.memset(ones_b[:, :], 1.0)
    k.dma("sp", [(X[0:T, :], x_in), (X[T:TA, :], ctx_in)], sembuf=xtok)

    with ExitStack() as st:
        cc = k.sb(st, "cc", [128, 8, 2], F32)
        k.dma("sp", [(cc[:, :, :], cc_in)])
        k.act(cc[:, :, :], cc[:, :, :], AF.Silu)
        wbuf = [k.sb(st, "wada", [128, 8, 512], F32) for _ in range(2)]
        mrow = k.sb(st, "mrow", [2, 6 * D], F32)
        bias = k.sb(st, "bias", [2, 6 * D], F32)
        gg = k.sb(st, "gg", [2, 2, D], F32)
        pm = [k.ps(st, "pm", [128, 512]) for _ in range(2)]
        for li in range(L):
            k.dma("sp", [(bias[0:1, :], b_ada[li:li + 1, :]), (bias[1:2, :], b_ada[li:li + 1, :])])
            k.dma("sp", [(gg[0:1, 0, :], norm_mix[li:li + 1, :]), (gg[1:2, 0, :], norm_mix[li:li + 1, :]),
                         (gg[0:1, 1, :], norm_ffn[li:li + 1, :]), (gg[1:2, 1, :], norm_ffn[li:li + 1, :])])
            for n in range(12):
                wb = wbuf[n % 2]
                k.dma("sp", [(wb[:, :, :], w_ada[li, :, n * 512:(n + 1) * 512].rearrange("(c p) n -> p c n", p=128))])
                for c in range(8):
                    k.mm(pm[n % 2][0:2, :], cc[:, c, :], wb[:, c, :], start=(c == 0), stop=(c == 7))
                k.tt(mrow[:, n * 512:(n + 1) * 512], pm[n % 2][0:2, :], bias[:, n * 512:(n + 1) * 512], ALU.add)
            k.stt(mrow[:, D:2 * D], mrow[:, D:2 * D], 1.0, gg[:, 0, :], ALU.add, ALU.mult)
            k.stt(mrow[:, 4 * D:5 * D], mrow[:, 4 * D:5 * D], 1.0, gg[:, 1, :], ALU.add, ALU.mult)
            k.dma("sp", [(MODS[li], mrow[:, :])])
        k.barrier()

    def bc_load(dst, src_row):
        k.dma("sp", [(dst, src_row.partition_broadcast(128))])

    def mod_tiles(st, li, slots):
        res = {}
        for s in slots:
            res[s] = []
            for r in range(2):
                t = k.sb(st, "mod%d" % s, [128, D], F32)
                bc_load(t[:, :], MODS[li, r, s * D:(s + 1) * D])
                res[s].append(t)
        return res

    def normmod(xt_ap, out_ap, gs, sh, tmp, ss, rs):
        k.act(tmp[:, :], xt_ap, AF.Square, accum=ss[:, 0:1])
        k.rstd(rs[:, 0:1], ss[:, 0:1], 1.0 / D)
        k.stt(tmp[:, :], xt_ap, rs[:, 0:1], gs[:, :], ALU.mult, ALU.mult)
        k.tt(out_ap, tmp[:, :], sh[:, :], ALU.add)

    def tok_groups():
        gs_ = []
        for g0 in range(0, NTL, 4):
            gs_.append(list(range(g0, min(NTL, g0 + 4))))
        gs_.append(list(range(NTL, NT)))
        return gs_

    def m1_even(li, j):
        with ExitStack() as st:
            Win = k.sb(st, "win", [128, 8, 2304], BF16)
            k.dma("pool", [(Win[:, c, :], w_in_even[j, c * 128:(c + 1) * 128, :]) for c in range(8)], max_dma_last_dim=4096)
            mods = mod_tiles(st, li, [0, 1])
            gain = k.sb(st, "gain", [128, 1664], F32)
            for (c0, n, src) in ((0, 8, a_qk[j, 0, :]), (512, 8, a_qk[j, 1, :]), (1024, 8, b_qk[j, 0, :]), (1536, 2, b_qk[j, 1, :])):
                k.dma("sp", [(gain[:, c0:c0 + n * 64].rearrange("p (g d) -> p g d", d=64),
                              src.partition_broadcast(128).unsqueeze(1).broadcast_to([128, n, 64]))])
            xts = [k.sb(st, "xt", [128, D], F32) for _ in range(2)]
            tmp = k.sb(st, "tmp", [128, D], F32)
            ss = k.sb(st, "ss", [128, 1], F32); rs = k.sb(st, "rs", [128, 1], F32)
            hb = k.sb(st, "hb", [128, D], BF16)
            hT = k.sb(st, "hT", [128, 8, 128], BF16)
            sq = k.sb(st, "sq", [128, 1664], F32)
            ssg = k.sb(st, "ssg", [128, 26], F32)
            nrm = k.sb(st, "nrm", [128, 1664], F32)
            t1 = k.sb(st, "t1", [128, 1664], F32)
            t2 = k.sb(st, "t2", [128, 1664], F32)
            cs = [k.sb(st, "cs", [128, 2, 64], F32) for _ in range(2)]
            qkb = k.sb(st, "qkb", [128, 1792], BF16)
            vst = [k.sb(st, "vst", [128, 640], BF16) for _ in range(2)]
            stage = [k.sb(st, "stage", [128, 14, 512], BF16) for _ in range(2)]
            tpT = k.ps(st, "tpT", [128, 512])
            P = k.ps(st, "P", [128, 2560])
            TQ = k.ps(st, "TQ", [128, 1024])
            tpTb = tpT[:, :].bitcast(BF16)
            TQb = TQ[:, :].bitcast(BF16)
            secs = ((0, 0, 512), (512, 512, 512), (1536, 1024, 512), (2048, 1536, 128))
            for gi, tiles in enumerate(tok_groups()):
                stg = stage[gi % 2]
                for jt, ti in enumerate(tiles):
                    kind = 0 if ti < NTL else 1
                    xt = xts[ti % 2]
                    k.dma("sp", [(xt[:, :], X[ti * 128:(ti + 1) * 128, :])])
                    if kind == 0:
                        c_ = cs[ti % 2]
                        k.dma("sp", [(c_[:, 0, :], k_cos64[ti * 128:(ti + 1) * 128, :]),
                                     (c_[:, 1, :], k_sin64[ti * 128:(ti + 1) * 128, :])])
                    normmod(xt[:, :], hb[:, :], mods[1][kind], mods[0][kind], tmp, ss, rs)
                    for c in range(8):
                        k.tr(tpTb[:, c * 128:(c + 1) * 128], hb[:, c * 128:(c + 1) * 128], ident_b[:, :])
                    k.cp(hT[:, :, :], tpTb.rearrange("p (c t) -> p c t", t=128), en="act")
                    for n in range(5):
                        w = min(2304, (n + 1) * 512) - n * 512
                        for c in range(8):
                            k.mm(P[:, n * 512:n * 512 + w], hT[:, c, :], Win[:, c, n * 512:n * 512 + w],
                                 start=(c == 0), stop=(c == 7))
                    for (ps0, s0, w) in secs:
                        k.act(sq[:, s0:s0 + w], P[:, ps0:ps0 + w], AF.Square)
                    k.red(ssg[:, :], sq[:, :].rearrange("p (g d) -> p g d", d=64))
                    k.rstd(ssg[:, :], ssg[:, :], 1.0 / 64)
                    for (ps0, s0, w) in secs:
                        g0, ng = s0 // 64, w // 64
                        k.tt(nrm[:, s0:s0 + w].rearrange("p (g d) -> p g d", d=64),
                             P[:, ps0:ps0 + w].rearrange("p (g d) -> p g d", d=64),
                             ssg[:, g0:g0 + ng].unsqueeze(2).broadcast_to([128, ng, 64]), ALU.mult)
                    k.tt(nrm[:, :], nrm[:, :], gain[:, :], ALU.mult)
                    v_ = vst[ti % 2]
                    k.cp(v_[:, 0:512], P[:, 1024:1536], en="act")
                    k.cp(v_[:, 512:640], P[:, 2176:2304], en="act")
                    k.dma("sp", [(Vd[ti * 128:(ti + 1) * 128, 0:640], v_[:, :])])
                    kbd = qkb[:, 1536:1792].rearrange("p (a b d) -> p a b d", a=2, b=2)
                    if kind == 0:
                        c_ = cs[ti % 2]
                        k.tt(t1[:, :].rearrange("p (g d) -> p g d", d=64), nrm[:, :].rearrange("p (g d) -> p g d", d=64),
                             c_[:, 0, :].unsqueeze(1).broadcast_to([128, 26, 64]), ALU.mult)
                        nv = nrm[:, :].rearrange("p (g r h j) -> p g r h j", r=2, h=2, j=16)
                        tv = t2[:, :].rearrange("p (g r h j) -> p g r h j", r=2, h=2, j=16)
                        sv = c_[:, 1, :].rearrange("p (r h j) -> p r h j", r=2, h=2)
                        for r in range(2):
                            for h in range(2):
                                k.tt(tv[:, :, r, h, :], nv[:, :, r, 1 - h, :],
                                     sv[:, r, h, :].unsqueeze(1).broadcast_to([128, 26, 16]), ALU.mult)
                        k.tt(qkb[:, 0:1536], t1[:, 0:1536], t2[:, 0:1536], ALU.add)
                        k.tt(kbd, t1[:, 1536:1664].rearrange("p (a d) -> p a d", d=64).unsqueeze(2).broadcast_to([128, 2, 2, 64]),
                             t2[:, 1536:1664].rearrange("p (a d) -> p a d", d=64).unsqueeze(2).broadcast_to([128, 2, 2, 64]), ALU.add)
                    else:
                        k.cp(qkb[:, 0:1536], nrm[:, 0:1536])
                        k.cp(kbd, nrm[:, 1536:1664].rearrange("p (a d) -> p a d", d=64).unsqueeze(2).broadcast_to([128, 2, 2, 64]))
                    for c in range(14):
                        k.tr(TQb[:, c * 128:(c + 1) * 128], qkb[:, c * 128:(c + 1) * 128], ident_b[:, :])
                    k.cp(stg[:, :, jt * 128:(jt + 1) * 128], TQb[:, 0:1792].rearrange("p (c t) -> p c t", t=128), en="act")
                n = len(tiles) * 128
                t0 = tiles[0] * 128
                k.dma("sp", [(QKT[:, :, t0:t0 + n].rearrange("c p t -> p c t"), stg[:, :, 0:n])])
            k.barrier()

    sctr = [0]

    def attention(groups, scale, lam_cols=None):
        with ExitStack() as st:
            KTb = [k.sb(st, "ktb", [128, TA], BF16) for _ in range(2)]
            VAb = [k.sb(st, "vab", [128, NT, 128], BF16) for _ in range(2)]
            Qb = [k.sb(st, "qb", [128, 512], BF16) for _ in range(3)]
            half_k = any(g["kdim"] == 64 for g in groups)
            if half_k:
                Qz = [[k.sb(st, "qz%d" % h_, [128, 512], BF16) for _ in range(3)] for h_ in range(2)]
                for h_ in range(2):
                    for q_ in Qz[h_]:
                        k.memset(q_[:, :], 0.0)
                qzn = [0, 0]
            PT = [k.sb(st, "pt", [128, 512], BF16) for _ in range(3)]
            S = [k.ps(st, "S", [128, 512]) for _ in range(3)]
            accO = k.ps(st, "accO", [128, 512]); accD = k.ps(st, "accD", [128, 512])
            bR = k.ps(st, "bR", [128, 512]); bS = k.ps(st, "bS", [128, 512])
            osb = [k.sb(st, "osb", [128, 512], F32) for _ in range(2)]
            onA = [k.sb(st, "onA", [128, 512], F32) for _ in range(2)]
            rrow = k.sb(st, "rrow", [128, 512], F32)
            Dt = k.sb(st, "Dt", [128, 512], F32)
            sqd = k.sb(st, "sqd", [128, 512], F32)
            obf = [k.sb(st, "obf", [128, 512], BF16) for _ in range(2)]
            for v_ in VAb:
                k.memset(v_[:, :, :], 1.0)
            qblocks = [(b * 512, 512, 0, NT) for b in range(T // 512)] + [(T, C, NTL, NT)]
            qn = 0
            on_ = 0
            def load_kv(gi_):
                g_ = groups[gi_]
                kb_ = KTb[gi_ % 2]; vb_ = VAb[gi_ % 2]
                k.dma("sp", [(kb_[0:g_["kr"], :], g_["kt"])])
                if not g_["typeA"]:
                    k.memset(vb_[:, :, 64:65], 1.0)
                dv_ = g_["dv"]
                vsrc = Vd[:, g_["vcol"]:g_["vcol"] + dv_].rearrange("(t p) d -> p t d", p=128)
                k.dma("sp", [(vb_[:, t0_:min(NT, t0_ + 8), 0:dv_], vsrc[:, t0_:min(NT, t0_ + 8), :]) for t0_ in range(0, NT, 8)])

            load_kv(0)
            for gi, g in enumerate(groups):
                kb = KTb[gi % 2]; vb = VAb[gi % 2]
                kr, kdim, dv, typeA = g["kr"], g["kdim"], g["dv"], g["typeA"]
                if gi + 1 < len(groups):
                    load_kv(gi + 1)
                for (q0, nq, klo, khi) in qblocks:
                    for (qap, units) in g["qcs"]:
                        if kdim != 64:
                            qb = Qb[qn % 3]; qn += 1
                            k.dma("sp", [(qb[0:kr, 0:nq], qap[:, q0:q0 + nq])])
                        for ui, (pb, orow) in enumerate(units):
                            n = khi - klo
                            base = sctr[0]; sctr[0] += n
                            if kdim == 64:
                                hsel = pb // 64
                                qb = Qz[hsel][qzn[hsel] % 3]; qzn[hsel] += 1
                                k.dma("sp", [(qb[pb:pb + 64, 0:nq], qap[pb:pb + 64, q0:q0 + nq])])
                                k0, k1 = 0, 128
                            else:
                                k0, k1 = pb, pb + kdim

                            def QK(i, qb=qb, k0=k0, k1=k1, base=base):
                                kt = klo + i
                                k.mm(S[(base + i) % 3][:, 0:nq], kb[k0:k1, kt * 128:(kt + 1) * 128], qb[k0:k1, 0:nq])

                            QK(0)
                            if n > 1:
                                QK(1)
                            for i in range(n):
                                kt = klo + i
                                ix = (base + i) % 3
                                k.act(PT[ix][:, 0:nq], S[ix][:, 0:nq], AF.Exp, scale=scale)
                                if i + 2 < n:
                                    QK(i + 2)
                                if typeA:
                                    k.mm(accO[:, 0:nq], vb[:, kt, 0:128], PT[ix][:, 0:nq], start=(i == 0), stop=(i == n - 1))
                                    k.mm(accD[:, 0:nq], ones_b[:, 0:128], PT[ix][:, 0:nq], start=(i == 0), stop=(i == n - 1))
                                else:
                                    k.mm(accO[0:65, 0:nq], vb[:, kt, 0:65], PT[ix][:, 0:nq], start=(i == 0), stop=(i == n - 1))
                            if typeA:
                                c = ui
                                k.cp(osb[c][:, 0:nq], accO[:, 0:nq], en="act")
                                k.recip(rrow[:, 0:nq], accD[:, 0:nq])
                                k.tt(onA[c][:, 0:nq], osb[c][:, 0:nq], rrow[:, 0:nq], ALU.mult)
                                if c == 1:
                                    nlam, subg = lam_cols
                                    k.stt(Dt[:, 0:nq], onA[1][:, 0:nq], nlam[:, 0:1], onA[0][:, 0:nq], ALU.mult, ALU.add)
                                    k.act(sqd[:, 0:nq], Dt[:, 0:nq], AF.Square)
                                    k.mm(bS[0:1, 0:nq], ones_f[:, 0:1], sqd[:, 0:nq])
                                    k.rstd(rrow[0:1, 0:nq], bS[0:1, 0:nq], 1.0 / 128)
                                    k.mm(bR[:, 0:nq], ones_f[0:1, 0:128], rrow[0:1, 0:nq])
                                    ob = obf[on_ % 2]; on_ += 1
                                    k.stt(ob[:, 0:nq], Dt[:, 0:nq], subg[:, 0:1], bR[:, 0:nq], ALU.mult, ALU.mult)
                                    k.dma("sp", [(OT[orow:orow + 128, q0:q0 + nq], ob[:, 0:nq])])
                            else:
                                k.cp(osb[0][0:65, 0:nq], accO[0:65, 0:nq], en="act")
                                k.recip(rrow[64:65, 0:nq], osb[0][64:65, 0:nq])
                                k.mm(bR[0:64, 0:nq], ones_f[64:65, 0:64], rrow[64:65, 0:nq])
                                ob = obf[on_ % 2]; on_ += 1
                                k.tt(ob[0:64, 0:nq], osb[0][0:64, 0:nq], bR[0:64, 0:nq], ALU.mult)
                                k.dma("sp", [(OT[orow:orow + 64, q0:q0 + nq], ob[0:64, 0:nq])])
            k.barrier()

    def attn_even(li, j):
        lam_init = 0.8 - 0.6 * math.exp(-0.3 * li)
        with ExitStack() as st:
            dl = k.sb(st, "dl", [1, 256], F32)
            pr = k.sb(st, "pr", [1, 128], F32)
            s2 = k.sb(st, "s2", [1, 2], F32)
            nl = k.sb(st, "nl", [1, 1], F32)
            nlam = k.sb(st, "nlam", [128, 1], F32)
            subg = k.sb(st, "subg", [128, 1], F32)
            pl = k.ps(st, "pl", [128, 512])
            k.dma("sp", [(dl[0:1, :], dlam[j:j + 1].rearrange("o a d -> o (a d)"))])
            k.dma("sp", [(subg[:, 0:1], a_subln[j].rearrange("(p o) -> p o", o=1))])
            k.tt(pr[:, 0:64], dl[:, 0:64], dl[:, 64:128], ALU.mult)
            k.tt(pr[:, 64:128], dl[:, 128:192], dl[:, 192:256], ALU.mult)
            k.red(s2[:, :], pr[:, :].rearrange("o (a d) -> o a d", d=64))
            k.act(s2[:, :], s2[:, :], AF.Exp)
            k.tt(nl[:, :], s2[:, 1:2], s2[:, 0:1], ALU.subtract)
            k.ts(nl[:, :], nl[:, :], -lam_init, None, ALU.add)
            k.mm(pl[:, 0:1], ones_f[0:1, 0:128], nl[0:1, 0:1])
            k.cp(nlam[:, :], pl[:, 0:1])
            k.ts(subg[:, :], subg[:, :], 1.0 - lam_init, None, ALU.mult)
            groups = []
            for h in range(4):
                groups.append(dict(kt=QKT[4 + h], kr=128, kdim=64, vcol=h * 128, dv=128, typeA=True,
                                   qcs=[(QKT[h], [(0, h * 128), (64, h * 128)])]))
            for kv in range(2):
                qcs = []
                for qc in (8 + 2 * kv, 9 + 2 * kv):
                    hb0 = 2 * (qc - 8)
                    qcs.append((QKT[qc], [(0, 512 + hb0 * 64), (64, 512 + (hb0 + 1) * 64)]))
                groups.append(dict(kt=QKT[12 + kv], kr=128, kdim=64, vcol=512 + kv * 64, dv=64, typeA=False, qcs=qcs))
            attention(groups, 64 ** -0.5, (nlam, subg))

    def attn_odd(li, j):
        groups = []
        for h in range(16):
            groups.append(dict(kt=KTo[h], kr=96, kdim=96, vcol=h * 64, dv=64, typeA=False,
                               qcs=[(QTo[h], [(0, h * 64)])]))
        attention(groups, 96 ** -0.5)

    def mixout_ffnin(li, wout_d, AFF, IDX):
        with ExitStack() as st:
            Wout = k.sb(st, "wout", [128, 8, D], BF16)
            k.dma("pool", [(Wout[:, c, :], wout_d[c * 128:(c + 1) * 128, :]) for c in range(8)], max_dma_last_dim=4096)
            wr = k.sb(st, "wr", [128, 8, NE], F32)
            k.dma("sp", [(wr[:, :, :], w_router[li].rearrange("(c p) e -> p c e", p=128))])
            mods = mod_tiles(st, li, [2, 3, 4])
            xts = [k.sb(st, "xt", [128, D], F32) for _ in range(2)]
            oTs = [k.sb(st, "oT", [128, 8, 128], BF16) for _ in range(2)]
            tmp = k.sb(st, "tmp", [128, D], F32)
            xn = [k.sb(st, "xn", [128, D], F32) for _ in range(2)]
            hf = k.sb(st, "hf", [128, D], F32)
            hrow = [k.sb(st, "hrow", [128, D + 2], BF16) for _ in range(2)]
            hTf = k.sb(st, "hTf", [128, 8, 128], F32)
            ss = k.sb(st, "ss", [128, 1], F32); rs = k.sb(st, "rs", [128, 1], F32)
            mx = k.sb(st, "mx", [128, 1], F32); sm = k.sb(st, "sm", [128, 1], F32)
            ex = k.sb(st, "ex", [128, NE], F32)
            Y = k.ps(st, "Y", [128, 1024]); TP = k.ps(st, "TP", [128, 1024]); LG = k.ps(st, "LG", [128, 512])
            for ti in range(NT):
                kind = 0 if ti < NTL else 1
                xt = xts[ti % 2]; oT = oTs[ti % 2]; xn_ = xn[ti % 2]; hr = hrow[ti % 2]
                k.dma("sp", [(xt[:, :], X[ti * 128:(ti + 1) * 128, :])])
                k.dma("sp", [(oT[:, :, :], OT[:, ti * 128:(ti + 1) * 128].rearrange("(c p) t -> p c t", p=128))])
                for h2 in range(2):
                    for c in range(8):
                        k.mm(Y[:, h2 * 512:(h2 + 1) * 512], oT[:, c, :], Wout[:, c, h2 * 512:(h2 + 1) * 512],
                             start=(c == 0), stop=(c == 7))
                for h2 in range(2):
                    sl = slice(h2 * 512, (h2 + 1) * 512)
                    k.tt(tmp[:, sl], Y[:, sl], mods[2][kind][:, sl], ALU.mult)
                k.tt(xn_[:, :], xt[:, :], tmp[:, :], ALU.add)
                k.dma("sp", [(X[ti * 128:(ti + 1) * 128, :], xn_[:, :])])
                normmod(xn_[:, :], hf[:, :], mods[4][kind], mods[3][kind], tmp, ss, rs)
                k.cp(hr[:, 0:D], hf[:, :], en="act")
                k.cp(hr[:, D:D + 2].bitcast(I32), tokidx[:, ti:ti + 1])
                k.dma("sp", [(Hd[ti * 128:(ti + 1) * 128, :], hr[:, :])])
                for c in range(8):
                    k.tr(TP[:, c * 128:(c + 1) * 128], hf[:, c * 128:(c + 1) * 128], ident_f[:, :])
                k.cp(hTf[:, :, :], TP[:, :].rearrange("p (c t) -> p c t", t=128), en="act")
                for c in range(8):
                    k.mm(LG[:, 0:NE], hTf[:, c, :], wr[:, c, :], start=(c == 0), stop=(c == 7))
                k.red(mx[:, :], LG[:, 0:NE], op=ALU.max)
                k.ts(mx[:, :], mx[:, :], -1.0, None, ALU.mult)
                k.act(ex[:, :], LG[:, 0:NE], AF.Exp, bias=mx[:, 0:1], accum=sm[:, 0:1])
                k.recip(sm[:, :], sm[:, :])
                k.ts(AFF[:, ti, :], ex[:, :], sm[:, 0:1], None, ALU.mult)
            k.barrier()
        with ExitStack() as st:
            lo = k.sb(st, "lo", [128, 2 * NE], F32)
            tt_ = k.sb(st, "tthr", [128, 2 * NE], F32)
            ge = k.sb(st, "ge", [128, 2 * NE], F32)
            capv = k.sb(st, "capv", [128, 2 * NE], F32)
            cmp_ = k.sb(st, "cmp", [128, NT, NE], BF16)
            cntp = k.sb(st, "cntp", [128, 2 * NE], F32)
            base_t = k.sb(st, "base", [128, NT, NE], F32)
            pos = k.sb(st, "pos", [128, NT, NE], F32)
            off = k.sb(st, "off", [128, NT, NE], F32)
            val = k.sb(st, "val", [128, NT, NE], F32)
            pc = k.ps(st, "pc", [128, 512])
            ncolp = ((NT * NE + 511) // 512) * 512
            pw = k.ps(st, "pw", [128, ncolp])
            pt_ = k.ps(st, "ptot", [128, ncolp])
            k.dma("sp", [(capv[:, :], k_capv)])
            k.dma("sp", [(base_t[:, :, :], k_base.rearrange("p (t e) -> p t e", e=NE))])
            k.memset(lo[:, :], 0.0)
            sets = ((0, NTL, 0), (NTL, NT, NE))

            def compare(thr):
                for (a, b, o) in sets:
                    k.tt(cmp_[:, a:b, :], AFF[:, a:b, :], thr[:, o:o + NE].unsqueeze(1).broadcast_to([128, b - a, NE]), ALU.is_ge)

            step = 0.5
            for it in range(32):
                k.ts(tt_[:, :], lo[:, :], step, None, ALU.add)
                compare(tt_)
                for (a, b, o) in sets:
                    k.red(cntp[:, o:o + NE], cmp_[:, a:b, :].rearrange("p t e -> p e t"))
                k.mm(pc[:, 0:2 * NE], ones_f[:, :], cntp[:, :])
                k.tt(ge[:, :], pc[:, 0:2 * NE], capv[:, :], ALU.is_ge)
                k.stt(lo[:, :], ge[:, :], step, lo[:, :], ALU.mult, ALU.add)
                step *= 0.5
            compare(lo)
            cf = cmp_[:, :, :].rearrange("p t e -> p (t e)")
            ncol = NT * NE
            for c0 in range(0, ncol, 512):
                w = min(512, ncol - c0)
                k.mm(pw[:, c0:c0 + w], tri_b[:, :], cf[:, c0:c0 + w])
                k.mm(pt_[:, c0:c0 + w], ones_b[:, :], cf[:, c0:c0 + w])
            for (a, b, o) in sets:
                k.memset(off[:, a, :], 0.0)
                for t in range(a, b - 1):
                    k.tt(off[:, t + 1, :], off[:, t, :], pt_[:, t * NE:(t + 1) * NE], ALU.add)
            k.tt(pos[:, :, :], pw[:, 0:ncol].rearrange("p (t e) -> p t e", e=NE), off[:, :, :], ALU.add)
            for (a, b, o), cap in zip(sets, (capL, capC)):
                k.ts(val[:, a:b, :], pos[:, a:b, :], float(cap), None, ALU.is_lt)
            k.tt(val[:, :, :], val[:, :, :], cmp_[:, :, :], ALU.mult)
            k.tt(pos[:, :, :], pos[:, :, :], base_t[:, :, :], ALU.add)
            k.ts(pos[:, :, :], pos[:, :, :], -BIG, None, ALU.add)
            k.tt(pos[:, :, :], pos[:, :, :], val[:, :, :], ALU.mult)
            k.ts(pos[:, :, :], pos[:, :, :], BIG, None, ALU.add)
            k.cp(IDX[:, :, :], pos[:, :, :])
            k.barrier()
        with ExitStack() as st:
            hrow = [k.sb(st, "hrow", [128, D + 2], BF16) for _ in range(3)]
            for ti in range(NT):
                hr = hrow[ti % 3]
                k.dma("sp", [(hr[:, :], Hd[ti * 128:(ti + 1) * 128, :])])
                for e in range(NE):
                    k.idma(XS[:, :], hr[:, :], IDX[:, ti, e:e + 1], scatter=True, bounds=reg_xs)
            k.barrier()

    def experts(li):
        with ExitStack() as st:
            WG = [k.sb(st, "wg", [128, 8, D], BF16) for _ in range(2)]
            WU = [k.sb(st, "wu", [128, 8, D], BF16) for _ in range(2)]
            WD = [k.sb(st, "wd", [128, 8, D], BF16) for _ in range(2)]
            wrf = k.sb(st, "wrf", [128, 8, NE], F32)
            wrb = k.sb(st, "wrb", [128, 8, NE], BF16)
            k.dma("sp", [(wrf[:, :, :], w_router[li].rearrange("(c p) e -> p c e", p=128))])
            k.cp(wrb[:, :, :], wrf[:, :, :])
            mods = mod_tiles(st, li, [5])
            xsT = k.sb(st, "xsT", [128, 8, SLOTS], BF16)
            gT = k.sb(st, "gT", [128, 8, SLOTS], BF16)
            xst = [k.sb(st, "xst", [128, D + 2], BF16) for _ in range(3)]
            sa = [k.sb(st, "sa", [128, 512], F32) for _ in range(2)]
            pay = [k.sb(st, "pay", [128, D], F32) for _ in range(2)]
            mx = k.sb(st, "mx", [128, 1], F32); sm = k.sb(st, "sm", [128, 1], F32)
            ex = k.sb(st, "ex", [128, NE], F32)
            TPb = k.ps(st, "TPx", [128, 512])
            LG = k.ps(st, "LGx", [128, 512])
            PA = [k.ps(st, "PA", [128, 512]) for _ in range(2)]
            PB = [k.ps(st, "PB", [128, 512]) for _ in range(2)]
            PY = k.ps(st, "PY", [128, 1024])
            TPv = TPb[:, :].bitcast(BF16)
            stiles = []
            r = 0
            while r < capL:
                n = min(128, capL - r); stiles.append((r, n, 0)); r += n
            stiles.append((capL, capC, 1))
            nblk = (SLOTS + 511) // 512
            bw = (SLOTS + nblk - 1) // nblk
            blocks = [(b * bw, min(bw, SLOTS - b * bw)) for b in range(nblk)]
            cnt = 0
            pcnt = 0
            nst = len(stiles)
            tix = [k.sb(st, "tix", [128, 1], I32) for _ in range(2 * nst)]
            gate = [k.sb(st, "gate", [128, 1], F32) for _ in range(2 * nst)]
            def load_w(e_):
                for (w_sb, w_d) in ((WG[e_ % 2], w_eg), (WU[e_ % 2], w_eu), (WD[e_ % 2], w_ed)):
                    k.dma("pool", [(w_sb[:, c, :], w_d[li, e_, c * 128:(c + 1) * 128, :]) for c in range(8)], max_dma_last_dim=4096)

            load_w(0)
            for e in range(NE):
                wg, wu, wd = WG[e % 2], WU[e % 2], WD[e % 2]
                if e + 1 < NE:
                    load_w(e + 1)
                tinfo = []
                for si_, (r0, n, kind) in enumerate(stiles):
                    xs = xst[cnt % 3]; cnt += 1
                    tx = tix[(e % 2) * nst + si_]; gt = gate[(e % 2) * nst + si_]
                    k.dma("sp", [(xs[0:n, :], XS[e * SLOTS + r0:e * SLOTS + r0 + n, :])])
                    k.cp(tx[0:n, :], xs[0:n, D:D + 2].bitcast(I32))
                    for c in range(8):
                        k.tr(TPv[:, c * 128:c * 128 + n], xs[0:n, c * 128:(c + 1) * 128], ident_b[0:n, 0:n])
                    k.cp(xsT[:, :, r0:r0 + n], TPv.rearrange("p (c t) -> p c t", t=128)[:, :, 0:n], en="act")
                    for c in range(8):
                        k.mm(LG[0:n, 0:NE], xsT[:, c, r0:r0 + n], wrb[:, c, :], start=(c == 0), stop=(c == 7))
                    k.red(mx[0:n, :], LG[0:n, 0:NE], op=ALU.max)
                    k.ts(mx[0:n, :], mx[0:n, :], -1.0, None, ALU.mult)
                    k.act(ex[0:n, :], LG[0:n, 0:NE], AF.Exp, bias=mx[0:n, 0:1], accum=sm[0:n, 0:1])
                    k.recip(sm[0:n, :], sm[0:n, :])
                    k.tt(gt[0:n, :], ex[0:n, e:e + 1], sm[0:n, :], ALU.mult)
                    tinfo.append((r0, n, kind, tx, gt))
                for f in range(8):
                    for (b0, bn) in blocks:
                        pa = PA[pcnt % 2]; pb_ = PB[pcnt % 2]; sa_ = sa[pcnt % 2]; pcnt += 1
                        for c in range(8):
                            k.mm(pa[:, 0:bn], wg[:, c, f * 128:(f + 1) * 128], xsT[:, c, b0:b0 + bn], start=(c == 0), stop=(c == 7))
                        for c in range(8):
                            k.mm(pb_[:, 0:bn], wu[:, c, f * 128:(f + 1) * 128], xsT[:, c, b0:b0 + bn], start=(c == 0), stop=(c == 7))
                        k.act(sa_[:, 0:bn], pa[:, 0:bn], AF.Silu)
                        k.tt(gT[:, f, b0:b0 + bn], sa_[:, 0:bn], pb_[:, 0:bn], ALU.mult)
                for ti_, (r0, n, kind, tx, gt) in enumerate(tinfo):
                    py = pay[(e * len(tinfo) + ti_) % 2]
                    for h2 in range(2):
                        for f in range(8):
                            k.mm(PY[0:n, h2 * 512:(h2 + 1) * 512], gT[:, f, r0:r0 + n], wd[:, f, h2 * 512:(h2 + 1) * 512],
                                 start=(f == 0), stop=(f == 7))
                    for h2 in range(2):
                        sl = slice(h2 * 512, (h2 + 1) * 512)
                        k.stt(py[0:n, sl], PY[0:n, sl], gt[0:n, 0:1], mods[5][kind][0:n, sl], ALU.mult, ALU.mult)
                    k.idma(X[:, :], py[0:n, :], tx[0:n, 0:1], scatter=True, bounds=reg_x, add=True, xw=[xtok])
            k.barrier()

    def m1_odd(li, j):
        with ExitStack() as st:
            Win = k.sb(st, "wino", [128, 8, 416], BF16)
            Wq = k.sb(st, "wq", [128, 2, 1536], BF16)
            Wkv = k.sb(st, "wkv", [128, 2048], BF16)
            k.dma("pool", [(Win[:, c, :], w_in_odd[j, c * 128:(c + 1) * 128, :]) for c in range(8)], max_dma_last_dim=4096)
            k.dma("pool", [(Wq[:, c, :], w_q_up[j, c * 128:(c + 1) * 128, :]) for c in range(2)], max_dma_last_dim=4096)
            k.dma("pool", [(Wkv[:, :], w_kv_up[j])], max_dma_last_dim=4096)
            mods = mod_tiles(st, li, [0, 1])
            gq = k.sb(st, "gq", [128, 256], F32); gkv = k.sb(st, "gkv", [128, 128], F32)
            gqk = k.sb(st, "gqk", [128, 2, 96], F32)
            bc_load(gq[:, :], q_norm[j]); bc_load(gkv[:, :], kv_norm[j])
            bc_load(gqk[:, 0, :], qk_norm[j, 0]); bc_load(gqk[:, 1, :], qk_norm[j, 1])
            invn = k.sb(st, "invn", [128, 3], F32)
            k.memset(invn[:, 0:1], 1.0 / 256); k.memset(invn[:, 1:2], 1.0 / 128); k.memset(invn[:, 2:3], 1.0 / 32)
            xts = [k.sb(st, "xt", [128, D], F32) for _ in range(2)]
            tmp = k.sb(st, "tmp", [128, D], F32)
            ss = k.sb(st, "ss", [128, 1], F32); rs = k.sb(st, "rs", [128, 1], F32)
            hb = k.sb(st, "hb", [128, D], BF16)
            hT = k.sb(st, "hT", [128, 8, 128], BF16)
            sqp = k.sb(st, "sqp", [128, 416], F32)
            s3 = k.sb(st, "s3", [128, 3], F32)
            cn = k.sb(st, "cn", [128, 384], BF16)
            cT = k.sb(st, "cT", [128, 3, 128], BF16)
            krn = k.sb(st, "krn", [128, 32], F32)
            krr = k.sb(st, "krr", [128, 32], F32)
            kt1 = k.sb(st, "kt1", [128, 32], F32); kt2 = k.sb(st, "kt2", [128, 32], F32)
            sqq = k.sb(st, "sqq", [128, 2048], F32)
            sq16 = k.sb(st, "sq16", [128, 32], F32)
            qn_ = k.sb(st, "qn", [128, 16, 96], F32)
            qt1 = k.sb(st, "qt1", [128, 16, 32], F32); qt2 = k.sb(st, "qt2", [128, 16, 32], F32)
            qbf = k.sb(st, "qbf", [128, 16, 96], BF16)
            kbf = k.sb(st, "kbf", [128, 16, 96], BF16)
            cs = [k.sb(st, "cs", [128, 2, 32], F32) for _ in range(2)]
            vst = [k.sb(st, "vst", [128, 16, 64], BF16) for _ in range(2)]
            stq = [k.sb(st, "stq", [96, 16, 512], BF16) for _ in range(2)]
            stk = [k.sb(st, "stk", [96, 16, 512], BF16) for _ in range(2)]
            tpT = k.ps(st, "tpT", [128, 512]); P = k.ps(st, "P", [128, 512]); cTp = k.ps(st, "cTp", [128, 512])
            Q3 = k.ps(st, "Q3", [128, 1536]); KV = k.ps(st, "KV", [128, 1024])
            tpTb = tpT[:, :].bitcast(BF16); cTb = cTp[:, :].bitcast(BF16)
            Q3b = Q3[:, :].bitcast(BF16); KVb = KV[:, :].bitcast(BF16)

            def rope32(dst, src, c_, shp, t1, t2):
                G = shp
                k.tt(t1, src, c_[:, 0, :].unsqueeze(1).broadcast_to([128, G, 32]), ALU.mult)
                sv = c_[:, 1, :].rearrange("p (r h j) -> p r h j", r=2, h=2)
                s5 = src.rearrange("p g (r h j) -> p g r h j", r=2, h=2)
                t5 = t2.rearrange("p g (r h j) -> p g r h j", r=2, h=2)
                for r in range(2):
                    for h in range(2):
                        k.tt(t5[:, :, r, h, :], s5[:, :, r, 1 - h, :], sv[:, r, h, :].unsqueeze(1).broadcast_to([128, G, 8]), ALU.mult)
                k.tt(dst, t1, t2, ALU.add)

            for gi, tiles in enumerate(tok_groups()):
                sq_, sk_ = stq[gi % 2], stk[gi % 2]
                for jt, ti in enumerate(tiles):
                    kind = 0 if ti < NTL else 1
                    xt = xts[ti % 2]
                    k.dma("sp", [(xt[:, :], X[ti * 128:(ti + 1) * 128, :])])
                    c_ = cs[ti % 2]
                    if kind == 0:
                        k.dma("sp", [(c_[:, 0, :], k_cos32[ti * 128:(ti + 1) * 128, :]),
                                     (c_[:, 1, :], k_sin32[ti * 128:(ti + 1) * 128, :])])
                    normmod(xt[:, :], hb[:, :], mods[1][kind], mods[0][kind], tmp, ss, rs)
                    for c in range(8):
                        k.tr(tpTb[:, c * 128:(c + 1) * 128], hb[:, c * 128:(c + 1) * 128], ident_b[:, :])
                    k.cp(hT[:, :, :], tpTb.rearrange("p (c t) -> p c t", t=128), en="act")
                    for c in range(8):
                        k.mm(P[:, 0:416], hT[:, c, :], Win[:, c, :], start=(c == 0), stop=(c == 7))
                    k.act(sqp[:, :], P[:, 0:416], AF.Square)
                    k.red(s3[:, 0:1], sqp[:, 0:256]); k.red(s3[:, 1:2], sqp[:, 256:384]); k.red(s3[:, 2:3], sqp[:, 384:416])
                    k.tt(s3[:, :], s3[:, :], invn[:, :], ALU.mult)
                    k.rstd(s3[:, :], s3[:, :], 1.0)
                    k.stt(cn[:, 0:256], P[:, 0:256], s3[:, 0:1], gq[:, :], ALU.mult, ALU.mult)
                    k.stt(cn[:, 256:384], P[:, 256:384], s3[:, 1:2], gkv[:, :], ALU.mult, ALU.mult)
                    k.stt(krn[:, :], P[:, 384:416], s3[:, 2:3], gqk[:, 1, 64:96], ALU.mult, ALU.mult)
                    if kind == 0:
                        rope32(krr[:, :].unsqueeze(1), krn[:, :].unsqueeze(1), c_, 1, kt1[:, :].unsqueeze(1), kt2[:, :].unsqueeze(1))
                    else:
                        k.cp(krr[:, :], krn[:, :])
                    for c in range(3):
                        k.tr(cTb[:, c * 128:(c + 1) * 128], cn[:, c * 128:(c + 1) * 128], ident_b[:, :])
                    k.cp(cT[:, :, :], cTb[:, 0:384].rearrange("p (c t) -> p c t", t=128), en="act")
                    for n in range(3):
                        for c in range(2):
                            k.mm(Q3[:, n * 512:(n + 1) * 512], cT[:, c, :], Wq[:, c, n * 512:(n + 1) * 512], start=(c == 0), stop=(c == 1))
                    for n in range(3):
                        k.act(sqq[:, n * 512:(n + 1) * 512], Q3[:, n * 512:(n + 1) * 512], AF.Square)
                    sv3 = sqq[:, 0:1536].rearrange("p (h d) -> p h d", d=96)
                    q3v = Q3[:, :].rearrange("p (h d) -> p h d", d=96)
                    k.red(sq16[:, 0:16], sv3[:, :, 0:64]); k.red(sq16[:, 16:32], sv3[:, :, 64:96])
                    k.rstd(sq16[:, 0:16], sq16[:, 0:16], 1.0 / 64)
                    k.rstd(sq16[:, 16:32], sq16[:, 16:32], 1.0 / 32)
                    k.tt(qn_[:, :, 0:64], q3v[:, :, 0:64], sq16[:, 0:16].unsqueeze(2).broadcast_to([128, 16, 64]), ALU.mult)
                    k.tt(qn_[:, :, 64:96], q3v[:, :, 64:96], sq16[:, 16:32].unsqueeze(2).broadcast_to([128, 16, 32]), ALU.mult)
                    k.tt(qn_[:, :, :], qn_[:, :, :], gqk[:, 0, :].unsqueeze(1).broadcast_to([128, 16, 96]), ALU.mult)
                    k.cp(qbf[:, :, 0:64], qn_[:, :, 0:64])
                    if kind == 0:
                        rope32(qbf[:, :, 64:96], qn_[:, :, 64:96], c_, 16, qt1[:, :, :], qt2[:, :, :])
                    else:
                        k.cp(qbf[:, :, 64:96], qn_[:, :, 64:96])
                    for h in range(16):
                        k.tr(Q3b[0:96, h * 128:(h + 1) * 128], qbf[:, h, :], ident_b[:, :])
                    k.cp(sq_[:, :, jt * 128:(jt + 1) * 128], Q3b[0:96, 0:2048].rearrange("p (h t) -> p h t", t=128), en="act")
                    v_ = vst[ti % 2]
                    for hf_ in range(2):
                        for n in range(2):
                            c0 = hf_ * 1024 + n * 512
                            k.mm(KV[:, n * 512:(n + 1) * 512], cT[:, 2, :], Wkv[:, c0:c0 + 512])
                        kv3 = KV[:, :].rearrange("p (h d) -> p h d", d=128)
                        for n in range(2):
                            k.act(sqq[:, n * 512:(n + 1) * 512], KV[:, n * 512:(n + 1) * 512], AF.Square)
                        k.red(sq16[:, 0:8], sqq[:, 0:1024].rearrange("p (h d) -> p h d", d=128)[:, :, 0:64])
                        k.rstd(sq16[:, 0:8], sq16[:, 0:8], 1.0 / 64)
                        hs = slice(hf_ * 8, hf_ * 8 + 8)
                        k.tt(qn_[:, 0:8, 0:64], kv3[:, :, 0:64], sq16[:, 0:8].unsqueeze(2).broadcast_to([128, 8, 64]), ALU.mult)
                        k.tt(kbf[:, hs, 0:64], qn_[:, 0:8, 0:64], gqk[:, 1, 0:64].unsqueeze(1).broadcast_to([128, 8, 64]), ALU.mult)
                        k.cp(v_[:, hs, :], kv3[:, :, 64:128], en="act")
                    k.cp(kbf[:, :, 64:96], krr[:, :].unsqueeze(1).broadcast_to([128, 16, 32]))
                    k.dma("sp", [(Vd[ti * 128:(ti + 1) * 128, :], v_[:, :, :].rearrange("p h d -> p (h d)"))])
                    for h in range(16):
                        k.tr(KVb[0:96, h * 128:(h + 1) * 128], kbf[:, h, :], ident_b[:, :])
                    k.cp(sk_[:, :, jt * 128:(jt + 1) * 128], KVb[0:96, 0:2048].rearrange("p (h t) -> p h t", t=128), en="act")
                n = len(tiles) * 128
                t0 = tiles[0] * 128
                k.dma("sp", [(QTo[:, :, t0:t0 + n].rearrange("h p t -> p h t"), sq_[:, :, 0:n])])
                k.dma("sp", [(KTo[:, :, t0:t0 + n].rearrange("h p t -> p h t"), sk_[:, :, 0:n])])
            k.barrier()

    lst = ExitStack()
    AFF = k.sb(lst, "AFF", [128, NT, NE], F32)
    IDX = k.sb(lst, "IDX", [128, NT, NE], I32)
    k.barrier()
    upto = getattr(cfg, "upto", None)
    for li in range(L):
        j = li // 2
        if li % 2 == 0:
            m1_even(li, j)
            if upto == "m1" and li == L - 1:
                break
            attn_even(li, j)
            if upto == "attn" and li == L - 1:
                break
            mixout_ffnin(li, w_out_even[j], AFF, IDX)
        else:
            m1_odd(li, j)
            if upto == "m1" and li == L - 1:
                break
            attn_odd(li, j)
            if upto == "attn" and li == L - 1:
                break
            mixout_ffnin(li, w_out_odd[j], AFF, IDX)
        if upto == "ffnin" and li == L - 1:
            break
        experts(li)
    k.barrier()
    k.dma("sp", [(out_d, X[0:T, :])], sembuf=xtok)
    k.barrier()
    lst.close()
    gst.close()
    k.dbg = dict(X=X, MODS=MODS, QKT=QKT, Vd=Vd, OT=OT, Hd=Hd, XS=XS, QTo=QTo, KTo=KTo)
    return k


def host_consts(cfg):
    T, NT, NTL = cfg.T, cfg.NT, cfg.NTL
    c = {}
    c["k_ident"] = np.eye(128, dtype=np.float32)
    c["k_tri"] = np.triu(np.ones((128, 128), np.float32), 1)
    c["k_tokidx"] = (np.arange(NT, dtype=np.int32)[None, :] * 128 + np.arange(128, dtype=np.int32)[:, None]).astype(np.int32)
    t = np.arange(T)
    row = (t // GRID_W).astype(np.float32); col = (t % GRID_W).astype(np.float32)
    for rd, nm in ((64, "64"), (32, "32")):
        half = rd // 2
        fr = (ROPE_THETA ** (-np.arange(0, half, 2, dtype=np.float32) / half)).astype(np.float32)
        ar = (row[:, None] * fr[None, :]).astype(np.float32); ac = (col[:, None] * fr[None, :]).astype(np.float32)
        cr, sr, cc_, sc_ = np.cos(ar), np.sin(ar), np.cos(ac), np.sin(ac)
        c["k_cos" + nm] = np.concatenate([cr, cr, cc_, cc_], axis=1).astype(np.float32)
        c["k_sin" + nm] = np.concatenate([-sr, sr, -sc_, sc_], axis=1).astype(np.float32)
    base = np.zeros((128, NT, NE), np.float32)
    base[:, :, :] = (np.arange(NE, dtype=np.float32) * cfg.SLOTS)[None, None, :]
    base[:, NTL:, :] += cfg.capL
    c["k_base"] = base.reshape(128, NT * NE)
    capv = np.zeros((128, 2 * NE), np.float32); capv[:, :NE] = cfg.capL; capv[:, NE:] = cfg.capC
    c["k_capv"] = capv
    return c


def make_in_maps(cfg, inputs):
    consts = host_consts(cfg)
    B = inputs["x"].shape[0]
    wnames = ["w_ada", "b_ada", "norm_mix", "norm_ffn", "w_in_even", "a_qk_norm", "diff_lambda", "a_subln",
              "b_qk_norm", "w_out_even", "w_router", "w_exp_gate", "w_exp_up", "w_exp_down"]
    if cfg.n_odd:
        wnames += ["w_in_odd", "mla_q_norm", "w_q_up", "mla_kv_norm", "w_kv_up", "mla_qk_norm", "w_out_odd"]
    shared = {n: np.ascontiguousarray(np.asarray(inputs[n], dtype=np.float32)) for n in wnames}
    shared.update(consts)
    maps = []
    cctx = np.asarray(inputs["c_ctx"], np.float32)
    for b in range(B):
        m = dict(shared)
        m["x"] = np.ascontiguousarray(np.asarray(inputs["x"][b], np.float32))
        m["ctx"] = np.ascontiguousarray(np.asarray(inputs["ctx"][b], np.float32))
        cb = np.asarray(inputs["c"][b], np.float32)
        m["cc"] = np.ascontiguousarray(np.stack([cb.reshape(8, 128).T, cctx.reshape(8, 128).T], axis=-1))
        maps.append(m)
    return maps


def kernel(**inputs):
    cfg = Cfg()
    kb = build(cfg)
    maps = make_in_maps(cfg, inputs)
    res = run_bass_kernel_spmd(kb.nc, maps, core_ids=list(range(len(maps))))
    return np.stack([np.asarray(r["out"], dtype=np.float32) for r in res.results], axis=0)
```

```python
import math
from contextlib import ExitStack
import numpy as np
import concourse.bass as bass
import concourse.mybir as mybir
from concourse.bass_utils import run_bass_kernel_spmd

F32 = mybir.dt.float32
BF16 = mybir.dt.bfloat16
I32 = mybir.dt.int32
AF = mybir.ActivationFunctionType
ALU = mybir.AluOpType
AX = mybir.AxisListType

D = 1024
NE = 16
EPS = 1e-6
GRID_W = 64
ROPE_THETA = 10000.0


class Cfg:
    def __init__(self, T=8192, C=256, depth=4):
        self.T, self.C, self.L = T, C, depth
        self.TA = T + C
        self.NTL, self.NTC = T // 128, C // 128
        self.NT = self.NTL + self.NTC
        self.capL = 2 * T // NE
        self.capC = 2 * C // NE
        self.SLOTS = self.capL + self.capC
        self.n_even = (depth + 1) // 2
        self.n_odd = depth // 2
        self.QB = 512


class Buf:
    __slots__ = ("name", "w", "r", "sid")

    def __init__(self, name):
        self.name, self.w, self.r, self.sid = name, None, {}, None


class Eng:
    def __init__(self, e, sid):
        self.e, self.sid, self.waited = e, sid, {}


class KB:
    def __init__(self, cfg):
        self.cfg = cfg
        nc = self.nc = bass.Bass("TRN2", target_bir_lowering=False)
        self.sem = {}
        self.E = {}
        for n, e in (("pe", nc.tensor), ("act", nc.scalar), ("dve", nc.vector),
                     ("pool", nc.gpsimd), ("sp", nc.sync)):
            self.E[n] = Eng(e, self.newsem("e_" + n))
        self.bufs = {}
        self.uid = 0
        self.free_sids = {}

    def dsem(self, q, name):
        fl = self.free_sids.get(q)
        if fl:
            return fl.pop()
        return self.newsem(name)

    def _release(self, nm):
        b = self.bufs.pop(nm, None)
        if b is not None and b.sid:
            for q, sid in b.sid.items():
                self.free_sids.setdefault(q, []).append(sid)

    def newsem(self, name):
        sid = len(self.sem)
        self.sem[sid] = [self.nc.alloc_semaphore(name), 0]
        return sid

    def token(self, name):
        b = Buf(name)
        self.bufs[name] = b
        return b

    def _wait(self, E, sid, val):
        if E.waited.get(sid, 0) < val:
            E.e.wait_ge(self.sem[sid][0], val)
            E.waited[sid] = val

    def _deps(self, E, rb, wb, is_pe):
        need = {}
        for b in rb:
            if b.w is not None:
                need[b.w[0]] = max(need.get(b.w[0], 0), b.w[1])
        for b in wb:
            if b.w is not None:
                need[b.w[0]] = max(need.get(b.w[0], 0), b.w[1])
            for sid, v in b.r.items():
                need[sid] = max(need.get(sid, 0), v)
        for sid, v in need.items():
            if is_pe and sid == E.sid:
                continue
            self._wait(E, sid, v)

    def _mark(self, rb, wb, mark):
        sid, v = mark
        for b in rb:
            if b.r.get(sid, 0) < v:
                b.r[sid] = v
        for b in wb:
            b.w = mark
            b.r = {}

    def _bufs_of(self, aps, extra):
        out = []
        for a in aps:
            if a is None or isinstance(a, (int, float)):
                continue
            b = self.bufs.get(a.name)
            if b is not None and b not in out:
                out.append(b)
        for b in extra:
            if b not in out:
                out.append(b)
        return out

    def op(self, en, fn, outs, ins, xr=(), xw=()):
        E = self.E[en]
        rb = self._bufs_of(ins, xr)
        wb = self._bufs_of(outs, xw)
        self._deps(E, rb, wb, en == "pe")
        ins_ = fn(E.e)
        s = self.sem[E.sid]
        s[1] += 1
        ins_.then_inc(s[0], 1)
        self._mark(rb, wb, (E.sid, s[1]))

    def dma(self, q, pairs, xr=(), xw=(), sembuf=None, **kw):
        E = self.E[q]
        outs = [p[0] for p in pairs]
        ins = [p[1] for p in pairs]
        rb = self._bufs_of(ins, xr)
        wb = self._bufs_of(outs, xw)
        sb = sembuf
        if sb is None:
            cand = self._bufs_of(outs, ()) or self._bufs_of(ins, ())
            sb = cand[0]
        if sb.sid is None:
            sb.sid = {}
        if q not in sb.sid:
            sb.sid[q] = self.dsem(q, "d_%s_%s" % (q, sb.name))
        self._deps(E, rb, wb, False)
        s = self.sem[sb.sid[q]]
        for o, i in pairs:
            E.e.dma_start(out=o, in_=i, **kw).then_inc(s[0], 16)
            s[1] += 16
        self._mark(rb, wb, (sb.sid[q], s[1]))

    def idma(self, out, in_, idx_ap, scatter, bounds, add=False, xr=(), xw=()):
        E = self.E["pool"]
        rb = self._bufs_of([in_, idx_ap], xr)
        wb = self._bufs_of([out], xw)
        cand = self._bufs_of([in_] if scatter else [out], ())
        sb = cand[0]
        if sb.sid is None:
            sb.sid = {}
        if "ind" not in sb.sid:
            sb.sid["ind"] = self.dsem("ind", "d_ind_" + sb.name)
        self._deps(E, rb, wb, False)
        s = self.sem[sb.sid["ind"]]
        off = bass.IndirectOffsetOnAxis(ap=idx_ap, axis=0)
        kw = {}
        if add:
            kw["compute_op"] = ALU.add
            kw["oob_is_err"] = True
        else:
            kw["oob_is_err"] = False
        E.e.indirect_dma_start(out=out, out_offset=off if scatter else None, in_=in_,
                               in_offset=None if scatter else off,
                               bounds_check=bounds, **kw).then_inc(s[0], 16)
        s[1] += 16
        self._mark(rb, wb, (sb.sid["ind"], s[1]))

    def barrier(self):
        for E in self.E.values():
            for sid, (h, c) in self.sem.items():
                if c > 0:
                    self._wait(E, sid, c)
        for b in self.bufs.values():
            b.w, b.r = None, {}

    def sb(self, st, name, shape, dt):
        self.uid += 1
        nm = "%s_%d" % (name, self.uid)
        t = st.enter_context(self.nc.sbuf_tensor(nm, list(shape), dt))
        self.bufs[nm] = Buf(nm)
        st.callback(self._release, nm)
        return t

    def ps(self, st, name, shape, dt=F32):
        self.uid += 1
        nm = "%s_%d" % (name, self.uid)
        t = st.enter_context(self.nc.psum_tensor(nm, list(shape), dt))
        self.bufs[nm] = Buf(nm)
        st.callback(self._release, nm)
        return t

    def mm(self, out, lhsT, rhs, start=True, stop=True):
        self.op("pe", lambda e: e.matmul(out, lhsT, rhs, start=start, stop=stop), [out], [lhsT, rhs])

    def tr(self, out, in_, ident):
        self.op("pe", lambda e: e.transpose(out, in_, ident), [out], [in_, ident])

    def act(self, out, in_, func, scale=1.0, bias=0.0, accum=None, en="act"):
        kw = {}
        if accum is not None:
            kw["accum_out"] = accum
        self.op("act", lambda e: e.activation(out=out, in_=in_, func=func, bias=bias, scale=scale, **kw),
                [out, accum], [in_, bias, scale])

    def tt(self, out, in0, in1, op, en="dve"):
        self.op(en, lambda e: e.tensor_tensor(out=out, in0=in0, in1=in1, op=op), [out], [in0, in1])

    def ts(self, out, in0, s1, s2, op0, op1=None, en="dve"):
        if op1 is None:
            self.op(en, lambda e: e.tensor_scalar(out=out, in0=in0, scalar1=s1, scalar2=None, op0=op0),
                    [out], [in0, s1])
        else:
            self.op(en, lambda e: e.tensor_scalar(out=out, in0=in0, scalar1=s1, scalar2=s2, op0=op0, op1=op1),
                    [out], [in0, s1, s2])

    def stt(self, out, in0, sc, in1, op0, op1, en="dve"):
        self.op(en, lambda e: e.scalar_tensor_tensor(out=out, in0=in0, scalar=sc, in1=in1, op0=op0, op1=op1),
                [out], [in0, sc, in1])

    def red(self, out, in_, op=ALU.add, en="dve"):
        self.op(en, lambda e: e.tensor_reduce(out=out, in_=in_, axis=AX.X, op=op), [out], [in_])

    def cp(self, out, in_, en="dve"):
        if en == "act":
            self.op("act", lambda e: e.activation(out=out, in_=in_, func=AF.Copy), [out], [in_])
        else:
            self.op(en, lambda e: e.tensor_copy(out=out, in_=in_), [out], [in_])

    def recip(self, out, in_):
        self.op("dve", lambda e: e.reciprocal(out=out, in_=in_), [out], [in_])

    def memset(self, ap, v, en="dve"):
        self.op(en, lambda e: e.memset(ap, v), [ap], [])

    def rstd(self, out, ss, inv_n):
        self.act(out, ss, AF.Sqrt, scale=inv_n, bias=EPS)
        self.recip(out, out)


def build(cfg):
    k = KB(cfg)
    nc = k.nc
    T, C, TA, L, NT, NTL, NTC = cfg.T, cfg.C, cfg.TA, cfg.L, cfg.NT, cfg.NTL, cfg.NTC
    SLOTS, capL, capC = cfg.SLOTS, cfg.capL, cfg.capC
    ne, no = cfg.n_even, cfg.n_odd

    def din(name, shape, dt=F32):
        return nc.dram_tensor(name, list(shape), dt, kind="ExternalInput").ap()

    def dint(name, shape, dt):
        return nc.dram_tensor(name, list(shape), dt, kind=("ExternalOutput" if getattr(cfg, "debug", False) else "Internal")).ap()

    x_in = din("x", [T, D]); ctx_in = din("ctx", [C, D]); cc_in = din("cc", [128, 8, 2])
    w_ada = din("w_ada", [L, D, 6 * D]); b_ada = din("b_ada", [L, 6 * D])
    norm_mix = din("norm_mix", [L, D]); norm_ffn = din("norm_ffn", [L, D])
    w_in_even = din("w_in_even", [ne, D, 2304]); a_qk = din("a_qk_norm", [ne, 2, 64])
    dlam = din("diff_lambda", [ne, 4, 64]); a_subln = din("a_subln", [ne, 128])
    b_qk = din("b_qk_norm", [ne, 2, 64]); w_out_even = din("w_out_even", [ne, D, D])
    if no:
        w_in_odd = din("w_in_odd", [no, D, 416]); q_norm = din("mla_q_norm", [no, 256])
        w_q_up = din("w_q_up", [no, 256, 1536]); kv_norm = din("mla_kv_norm", [no, 128])
        w_kv_up = din("w_kv_up", [no, 128, 2048]); qk_norm = din("mla_qk_norm", [no, 2, 96])
        w_out_odd = din("w_out_odd", [no, D, D])
    w_router = din("w_router", [L, D, NE])
    w_eg = din("w_exp_gate", [L, NE, D, D]); w_eu = din("w_exp_up", [L, NE, D, D])
    w_ed = din("w_exp_down", [L, NE, D, D])
    k_ident = din("k_ident", [128, 128]); k_tri = din("k_tri", [128, 128])
    k_tokidx = din("k_tokidx", [128, NT], I32)
    k_cos64 = din("k_cos64", [T, 64]); k_sin64 = din("k_sin64", [T, 64])
    k_cos32 = din("k_cos32", [T, 32]); k_sin32 = din("k_sin32", [T, 32])
    k_base = din("k_base", [128, NT * NE]); k_capv = din("k_capv", [128, 2 * NE])
    out_d = nc.dram_tensor("out", [T, D], F32, kind="ExternalOutput").ap()

    X = dint("X", [TA, D], F32)
    MODS = dint("MODS", [L, 2, 6 * D], F32)
    QKT = dint("QKT", [14, 128, TA], BF16)
    QTo = dint("QTo", [16, 96, TA], BF16)
    KTo = dint("KTo", [16, 96, TA], BF16)
    Vd = dint("Vd", [TA, D], BF16)
    OT = dint("OT", [D, TA], BF16)
    Hd = dint("Hd", [TA, D + 2], BF16)
    XS = dint("XS", [NE * SLOTS + 128, D + 2], BF16)
    BIG = float(NE * SLOTS + 4096)
    reg_xs = nc.gpsimd.alloc_register("bnd_xs")
    nc.gpsimd.reg_mov(reg_xs, NE * SLOTS - 1)
    reg_x = nc.gpsimd.alloc_register("bnd_x")
    nc.gpsimd.reg_mov(reg_x, TA - 1)

    gst = ExitStack()
    ident_f = k.sb(gst, "identf", [128, 128], F32)
    ident_b = k.sb(gst, "identb", [128, 128], BF16)
    tri_f = k.sb(gst, "trif", [128, 128], F32)
    tri_b = k.sb(gst, "trib", [128, 128], BF16)
    ones_f = k.sb(gst, "onesf", [128, 128], F32)
    ones_b = k.sb(gst, "onesb", [128, 128], BF16)
    tokidx = k.sb(gst, "tokidx", [128, NT], I32)
    xtok = k.token("Xtok")

    k.dma("sp", [(ident_f[:, :], k_ident)])
    k.dma("sp", [(tri_f[:, :], k_tri)])
    k.dma("sp", [(tokidx[:, :], k_tokidx)])
    k.cp(ident_b[:, :], ident_f[:, :])
    k.cp(tri_b[:, :], tri_f[:, :])
    k.memset(ones_f[:, :], 1.0)
    k.memset(ones_b[:, :], 1.0)
    k.dma("sp", [(X[0:T, :], x_in), (X[T:TA, :], ctx_in)], sembuf=xtok)

    with ExitStack() as st:
        cc = k.sb(st, "cc", [128, 8, 2], F32)
        k.dma("sp", [(cc[:, :, :], cc_in)])
        k.act(cc[:, :, :], cc[:, :, :], AF.Silu)
        wbuf = [k.sb(st, "wada", [128, 8, 512], F32) for _ in range(2)]
        mrow = k.sb(st, "mrow", [2, 6 * D], F32)
        bias = k.sb(st, "bias", [2, 6 * D], F32)
        gg = k.sb(st, "gg", [2, 2, D], F32)
        pm = [k.ps(st, "pm", [128, 512]) for _ in range(2)]
        for li in range(L):
            k.dma("sp", [(bias[0:1, :], b_ada[li:li + 1, :]), (bias[1:2, :], b_ada[li:li + 1, :])])
            k.dma("sp", [(gg[0:1, 0, :], norm_mix[li:li + 1, :]), (gg[1:2, 0, :], norm_mix[li:li + 1, :]),
                         (gg[0:1, 1, :], norm_ffn[li:li + 1, :]), (gg[1:2, 1, :], norm_ffn[li:li + 1, :])])
            for n in range(12):
                wb = wbuf[n % 2]
                k.dma("sp", [(wb[:, :, :], w_ada[li, :, n * 512:(n + 1) * 512].rearrange("(c p) n -> p c n", p=128))])
                for c in range(8):
                    k.mm(pm[n % 2][0:2, :], cc[:, c, :], wb[:, c, :], start=(c == 0), stop=(c == 7))
                k.tt(mrow[:, n * 512:(n + 1) * 512], pm[n % 2][0:2, :], bias[:, n * 512:(n + 1) * 512], ALU.add)
            k.stt(mrow[:, D:2 * D], mrow[:, D:2 * D], 1.0, gg[:, 0, :], ALU.add, ALU.mult)
            k.stt(mrow[:, 4 * D:5 * D], mrow[:, 4 * D:5 * D], 1.0, gg[:, 1, :], ALU.add, ALU.mult)
            k.dma("sp", [(MODS[li], mrow[:, :])])
        k.barrier()

    def bc_load(dst, src_row):
        k.dma("sp", [(dst, src_row.partition_broadcast(128))])

    def mod_tiles(st, li, slots):
        res = {}
        for s in slots:
            res[s] = []
            for r in range(2):
                t = k.sb(st, "mod%d" % s, [128, D], F32)
                bc_load(t[:, :], MODS[li, r, s * D:(s + 1) * D])
                res[s].append(t)
        return res

    def normmod(xt_ap, out_ap, gs, sh, tmp, ss, rs):
        k.act(tmp[:, :], xt_ap, AF.Square, accum=ss[:, 0:1])
        k.rstd(rs[:, 0:1], ss[:, 0:1], 1.0 / D)
        k.stt(tmp[:, :], xt_ap, rs[:, 0:1], gs[:, :], ALU.mult, ALU.mult)
        k.tt(out_ap, tmp[:, :], sh[:, :], ALU.add)

    def tok_groups():
        gs_ = []
        for g0 in range(0, NTL, 4):
            gs_.append(list(range(g0, min(NTL, g0 + 4))))
        gs_.append(list(range(NTL, NT)))
        return gs_

    def m1_even(li, j):
        with ExitStack() as st:
            Win = k.sb(st, "win", [128, 8, 2304], BF16)
            k.dma("pool", [(Win[:, c, :], w_in_even[j, c * 128:(c + 1) * 128, :]) for c in range(8)], max_dma_last_dim=4096)
            mods = mod_tiles(st, li, [0, 1])
            gain = k.sb(st, "gain", [128, 1664], F32)
            for (c0, n, src) in ((0, 8, a_qk[j, 0, :]), (512, 8, a_qk[j, 1, :]), (1024, 8, b_qk[j, 0, :]), (1536, 2, b_qk[j, 1, :])):
                k.dma("sp", [(gain[:, c0:c0 + n * 64].rearrange("p (g d) -> p g d", d=64),
                              src.partition_broadcast(128).unsqueeze(1).broadcast_to([128, n, 64]))])
            xts = [k.sb(st, "xt", [128, D], F32) for _ in range(2)]
            tmp = k.sb(st, "tmp", [128, D], F32)
            ss = k.sb(st, "ss", [128, 1], F32); rs = k.sb(st, "rs", [128, 1], F32)
            hb = k.sb(st, "hb", [128, D], BF16)
            hT = k.sb(st, "hT", [128, 8, 128], BF16)
            sq = k.sb(st, "sq", [128, 1664], F32)
            ssg = k.sb(st, "ssg", [128, 26], F32)
            nrm = k.sb(st, "nrm", [128, 1664], F32)
            t1 = k.sb(st, "t1", [128, 1664], F32)
            t2 = k.sb(st, "t2", [128, 1664], F32)
            cs = [k.sb(st, "cs", [128, 2, 64], F32) for _ in range(2)]
            qkb = k.sb(st, "qkb", [128, 1792], BF16)
            vst = [k.sb(st, "vst", [128, 640], BF16) for _ in range(2)]
            stage = [k.sb(st, "stage", [128, 14, 512], BF16) for _ in range(2)]
            tpT = k.ps(st, "tpT", [128, 512])
            P = k.ps(st, "P", [128, 2560])
            TQ = k.ps(st, "TQ", [128, 1024])
            tpTb = tpT[:, :].bitcast(BF16)
            TQb = TQ[:, :].bitcast(BF16)
            secs = ((0, 0, 512), (512, 512, 512), (1536, 1024, 512), (2048, 1536, 128))
            for gi, tiles in enumerate(tok_groups()):
                stg = stage[gi % 2]
                for jt, ti in enumerate(tiles):
                    kind = 0 if ti < NTL else 1
                    xt = xts[ti % 2]
                    k.dma("sp", [(xt[:, :], X[ti * 128:(ti + 1) * 128, :])])
                    if kind == 0:
                        c_ = cs[ti % 2]
                        k.dma("sp", [(c_[:, 0, :], k_cos64[ti * 128:(ti + 1) * 128, :]),
                                     (c_[:, 1, :], k_sin64[ti * 128:(ti + 1) * 128, :])])
                    normmod(xt[:, :], hb[:, :], mods[1][kind], mods[0][kind], tmp, ss, rs)
                    for c in range(8):
                        k.tr(tpTb[:, c * 128:(c + 1) * 128], hb[:, c * 128:(c + 1) * 128], ident_b[:, :])
                    k.cp(hT[:, :, :], tpTb.rearrange("p (c t) -> p c t", t=128), en="act")
                    for n in range(5):
                        w = min(2304, (n + 1) * 512) - n * 512
                        for c in range(8):
                            k.mm(P[:, n * 512:n * 512 + w], hT[:, c, :], Win[:, c, n * 512:n * 512 + w],
                                 start=(c == 0), stop=(c == 7))
                    for (ps0, s0, w) in secs:
                        k.act(sq[:, s0:s0 + w], P[:, ps0:ps0 + w], AF.Square)
                    k.red(ssg[:, :], sq[:, :].rearrange("p (g d) -> p g d", d=64))
                    k.rstd(ssg[:, :], ssg[:, :], 1.0 / 64)
                    for (ps0, s0, w) in secs:
                        g0, ng = s0 // 64, w // 64
                        k.tt(nrm[:, s0:s0 + w].rearrange("p (g d) -> p g d", d=64),
                             P[:, ps0:ps0 + w].rearrange("p (g d) -> p g d", d=64),
                             ssg[:, g0:g0 + ng].unsqueeze(2).broadcast_to([128, ng, 64]), ALU.mult)
                    k.tt(nrm[:, :], nrm[:, :], gain[:, :], ALU.mult)
                    v_ = vst[ti % 2]
                    k.cp(v_[:, 0:512], P[:, 1024:1536], en="act")
                    k.cp(v_[:, 512:640], P[:, 2176:2304], en="act")
                    k.dma("pool", [(Vd[ti * 128:(ti + 1) * 128, 0:640], v_[:, :])])
                    kbd = qkb[:, 1536:1792].rearrange("p (a b d) -> p a b d", a=2, b=2)
                    if kind == 0:
                        c_ = cs[ti % 2]
                        k.tt(t1[:, :].rearrange("p (g d) -> p g d", d=64), nrm[:, :].rearrange("p (g d) -> p g d", d=64),
                             c_[:, 0, :].unsqueeze(1).broadcast_to([128, 26, 64]), ALU.mult)
                        nv = nrm[:, :].rearrange("p (g r h j) -> p g r h j", r=2, h=2, j=16)
                        tv = t2[:, :].rearrange("p (g r h j) -> p g r h j", r=2, h=2, j=16)
                        sv = c_[:, 1, :].rearrange("p (r h j) -> p r h j", r=2, h=2)
                        for r in range(2):
                            for h in range(2):
                                k.tt(tv[:, :, r, h, :], nv[:, :, r, 1 - h, :],
                                     sv[:, r, h, :].unsqueeze(1).broadcast_to([128, 26, 16]), ALU.mult)
                        k.tt(qkb[:, 0:1536], t1[:, 0:1536], t2[:, 0:1536], ALU.add)
                        k.tt(kbd, t1[:, 1536:1664].rearrange("p (a d) -> p a d", d=64).unsqueeze(2).broadcast_to([128, 2, 2, 64]),
                             t2[:, 1536:1664].rearrange("p (a d) -> p a d", d=64).unsqueeze(2).broadcast_to([128, 2, 2, 64]), ALU.add)
                    else:
                        k.cp(qkb[:, 0:1536], nrm[:, 0:1536])
                        k.cp(kbd, nrm[:, 1536:1664].rearrange("p (a d) -> p a d", d=64).unsqueeze(2).broadcast_to([128, 2, 2, 64]))
                    for c in range(14):
                        k.tr(TQb[:, c * 128:(c + 1) * 128], qkb[:, c * 128:(c + 1) * 128], ident_b[:, :])
                    k.cp(stg[:, :, jt * 128:(jt + 1) * 128], TQb[:, 0:1792].rearrange("p (c t) -> p c t", t=128), en="act")
                n = len(tiles) * 128
                t0 = tiles[0] * 128
                k.dma("pool", [(QKT[:, :, t0:t0 + n].rearrange("c p t -> p c t"), stg[:, :, 0:n])])
            k.barrier()

    sctr = [0]

    def attention(groups, scale, lam_cols=None):
        with ExitStack() as st:
            KTb = [k.sb(st, "ktb", [128, TA], BF16) for _ in range(2)]
            VAb = [k.sb(st, "vab", [128, NT, 128], BF16) for _ in range(2)]
            Qb = [k.sb(st, "qb", [128, 512], BF16) for _ in range(3)]
            half_k = any(g["kdim"] == 64 for g in groups)
            if half_k:
                Qz = [[k.sb(st, "qz%d" % h_, [128, 512], BF16) for _ in range(3)] for h_ in range(2)]
                for h_ in range(2):
                    for q_ in Qz[h_]:
                        k.memset(q_[:, :], 0.0)
                qzn = [0, 0]
            PT = [k.sb(st, "pt", [128, 512], BF16) for _ in range(3)]
            S = [k.ps(st, "S", [128, 512]) for _ in range(3)]
            accO = k.ps(st, "accO", [128, 512]); accD = k.ps(st, "accD", [128, 512])
            bR = k.ps(st, "bR", [128, 512]); bS = k.ps(st, "bS", [128, 512])
            osb = [k.sb(st, "osb", [128, 512], F32) for _ in range(2)]
            onA = [k.sb(st, "onA", [128, 512], F32) for _ in range(2)]
            rrow = k.sb(st, "rrow", [128, 512], F32)
            Dt = k.sb(st, "Dt", [128, 512], F32)
            sqd = k.sb(st, "sqd", [128, 512], F32)
            obf = [k.sb(st, "obf", [128, 512], BF16) for _ in range(2)]
            for v_ in VAb:
                k.memset(v_[:, :, :], 1.0)
            qblocks = [(b * 512, 512, 0, NT) for b in range(T // 512)] + [(T, C, NTL, NT)]
            qn = 0
            on_ = 0
            def load_kv(gi_):
                g_ = groups[gi_]
                kb_ = KTb[gi_ % 2]; vb_ = VAb[gi_ % 2]
                k.dma("sp", [(kb_[0:g_["kr"], :], g_["kt"])])
                if not g_["typeA"]:
                    k.memset(vb_[:, :, 64:65], 1.0)
                dv_ = g_["dv"]
                vsrc = Vd[:, g_["vcol"]:g_["vcol"] + dv_].rearrange("(t p) d -> p t d", p=128)
                k.dma("sp", [(vb_[:, t0_:min(NT, t0_ + 8), 0:dv_], vsrc[:, t0_:min(NT, t0_ + 8), :]) for t0_ in range(0, NT, 8)])

            load_kv(0)
            for gi, g in enumerate(groups):
                kb = KTb[gi % 2]; vb = VAb[gi % 2]
                kr, kdim, dv, typeA = g["kr"], g["kdim"], g["dv"], g["typeA"]
                if gi + 1 < len(groups):
                    load_kv(gi + 1)
                for (q0, nq, klo, khi) in qblocks:
                    for (qap, units) in g["qcs"]:
                        if kdim != 64:
                            qb = Qb[qn % 3]; qn += 1
                            k.dma("sp", [(qb[0:kr, 0:nq], qap[:, q0:q0 + nq])])
                        for ui, (pb, orow) in enumerate(units):
                            n = khi - klo
                            base = sctr[0]; sctr[0] += n
                            if kdim == 64:
                                hsel = pb // 64
                                qb = Qz[hsel][qzn[hsel] % 3]; qzn[hsel] += 1
                                k.dma("sp", [(qb[pb:pb + 64, 0:nq], qap[pb:pb + 64, q0:q0 + nq])])
                                k0, k1 = 0, 128
                            else:
                                k0, k1 = pb, pb + kdim

                            def QK(i, qb=qb, k0=k0, k1=k1, base=base):
                                kt = klo + i
                                k.mm(S[(base + i) % 3][:, 0:nq], kb[k0:k1, kt * 128:(kt + 1) * 128], qb[k0:k1, 0:nq])

                            QK(0)
                            if n > 1:
                                QK(1)
                            for i in range(n):
                                kt = klo + i
                                ix = (base + i) % 3
                                k.act(PT[ix][:, 0:nq], S[ix][:, 0:nq], AF.Exp, scale=scale)
                                if i + 2 < n:
                                    QK(i + 2)
                                if typeA:
                                    k.mm(accO[:, 0:nq], vb[:, kt, 0:128], PT[ix][:, 0:nq], start=(i == 0), stop=(i == n - 1))
                                    k.mm(accD[:, 0:nq], ones_b[:, 0:128], PT[ix][:, 0:nq], start=(i == 0), stop=(i == n - 1))
                                else:
                                    k.mm(accO[0:65, 0:nq], vb[:, kt, 0:65], PT[ix][:, 0:nq], start=(i == 0), stop=(i == n - 1))
                            if typeA:
                                c = ui
                                k.cp(osb[c][:, 0:nq], accO[:, 0:nq], en="act")
                                k.recip(rrow[:, 0:nq], accD[:, 0:nq])
                                k.tt(onA[c][:, 0:nq], osb[c][:, 0:nq], rrow[:, 0:nq], ALU.mult)
                                if c == 1:
                                    nlam, subg = lam_cols
                                    k.stt(Dt[:, 0:nq], onA[1][:, 0:nq], nlam[:, 0:1], onA[0][:, 0:nq], ALU.mult, ALU.add)
                                    k.act(sqd[:, 0:nq], Dt[:, 0:nq], AF.Square)
                                    k.mm(bS[0:1, 0:nq], ones_f[:, 0:1], sqd[:, 0:nq])
                                    k.rstd(rrow[0:1, 0:nq], bS[0:1, 0:nq], 1.0 / 128)
                                    k.mm(bR[:, 0:nq], ones_f[0:1, 0:128], rrow[0:1, 0:nq])
                                    ob = obf[on_ % 2]; on_ += 1
                                    k.stt(ob[:, 0:nq], Dt[:, 0:nq], subg[:, 0:1], bR[:, 0:nq], ALU.mult, ALU.mult)
                                    k.dma("pool", [(OT[orow:orow + 128, q0:q0 + nq], ob[:, 0:nq])])
                            else:
                                k.cp(osb[0][0:65, 0:nq], accO[0:65, 0:nq], en="act")
                                k.recip(rrow[64:65, 0:nq], osb[0][64:65, 0:nq])
                                k.mm(bR[0:64, 0:nq], ones_f[64:65, 0:64], rrow[64:65, 0:nq])
                                ob = obf[on_ % 2]; on_ += 1
                                k.tt(ob[0:64, 0:nq], osb[0][0:64, 0:nq], bR[0:64, 0:nq], ALU.mult)
                                k.dma("pool", [(OT[orow:orow + 64, q0:q0 + nq], ob[0:64, 0:nq])])
            k.barrier()

    def attn_even(li, j):
        lam_init = 0.8 - 0.6 * math.exp(-0.3 * li)
        with ExitStack() as st:
            dl = k.sb(st, "dl", [1, 256], F32)
            pr = k.sb(st, "pr", [1, 128], F32)
            s2 = k.sb(st, "s2", [1, 2], F32)
            nl = k.sb(st, "nl", [1, 1], F32)
            nlam = k.sb(st, "nlam", [128, 1], F32)
            subg = k.sb(st, "subg", [128, 1], F32)
            pl = k.ps(st, "pl", [128, 512])
            k.dma("sp", [(dl[0:1, :], dlam[j:j + 1].rearrange("o a d -> o (a d)"))])
            k.dma("sp", [(subg[:, 0:1], a_subln[j].rearrange("(p o) -> p o", o=1))])
            k.tt(pr[:, 0:64], dl[:, 0:64], dl[:, 64:128], ALU.mult)
            k.tt(pr[:, 64:128], dl[:, 128:192], dl[:, 192:256], ALU.mult)
            k.red(s2[:, :], pr[:, :].rearrange("o (a d) -> o a d", d=64))
            k.act(s2[:, :], s2[:, :], AF.Exp)
            k.tt(nl[:, :], s2[:, 1:2], s2[:, 0:1], ALU.subtract)
            k.ts(nl[:, :], nl[:, :], -lam_init, None, ALU.add)
            k.mm(pl[:, 0:1], ones_f[0:1, 0:128], nl[0:1, 0:1])
            k.cp(nlam[:, :], pl[:, 0:1])
            k.ts(subg[:, :], subg[:, :], 1.0 - lam_init, None, ALU.mult)
            groups = []
            for h in range(4):
                groups.append(dict(kt=QKT[4 + h], kr=128, kdim=64, vcol=h * 128, dv=128, typeA=True,
                                   qcs=[(QKT[h], [(0, h * 128), (64, h * 128)])]))
            for kv in range(2):
                qcs = []
                for qc in (8 + 2 * kv, 9 + 2 * kv):
                    hb0 = 2 * (qc - 8)
                    qcs.append((QKT[qc], [(0, 512 + hb0 * 64), (64, 512 + (hb0 + 1) * 64)]))
                groups.append(dict(kt=QKT[12 + kv], kr=128, kdim=64, vcol=512 + kv * 64, dv=64, typeA=False, qcs=qcs))
            attention(groups, 64 ** -0.5, (nlam, subg))

    def attn_odd(li, j):
        groups = []
        for h in range(16):
            groups.append(dict(kt=KTo[h], kr=96, kdim=96, vcol=h * 64, dv=64, typeA=False,
                               qcs=[(QTo[h], [(0, h * 64)])]))
        attention(groups, 96 ** -0.5)

    def mixout_ffnin(li, wout_d, AFF, IDX):
        with ExitStack() as st:
            Wout = k.sb(st, "wout", [128, 8, D], BF16)
            k.dma("pool", [(Wout[:, c, :], wout_d[c * 128:(c + 1) * 128, :]) for c in range(8)], max_dma_last_dim=4096)
            wr = k.sb(st, "wr", [128, 8, NE], F32)
            k.dma("sp", [(wr[:, :, :], w_router[li].rearrange("(c p) e -> p c e", p=128))])
            mods = mod_tiles(st, li, [2, 3, 4])
            xts = [k.sb(st, "xt", [128, D], F32) for _ in range(2)]
            oTs = [k.sb(st, "oT", [128, 8, 128], BF16) for _ in range(2)]
            tmp = k.sb(st, "tmp", [128, D], F32)
            xn = [k.sb(st, "xn", [128, D], F32) for _ in range(2)]
            hf = k.sb(st, "hf", [128, D], F32)
            hrow = [k.sb(st, "hrow", [128, D + 2], BF16) for _ in range(2)]
            hTf = k.sb(st, "hTf", [128, 8, 128], F32)
            ss = k.sb(st, "ss", [128, 1], F32); rs = k.sb(st, "rs", [128, 1], F32)
            mx = k.sb(st, "mx", [128, 1], F32); sm = k.sb(st, "sm", [128, 1], F32)
            ex = k.sb(st, "ex", [128, NE], F32)
            Y = k.ps(st, "Y", [128, 1024]); TP = k.ps(st, "TP", [128, 1024]); LG = k.ps(st, "LG", [128, 512])
            for ti in range(NT):
                kind = 0 if ti < NTL else 1
                xt = xts[ti % 2]; oT = oTs[ti % 2]; xn_ = xn[ti % 2]; hr = hrow[ti % 2]
                k.dma("sp", [(xt[:, :], X[ti * 128:(ti + 1) * 128, :])])
                k.dma("sp", [(oT[:, :, :], OT[:, ti * 128:(ti + 1) * 128].rearrange("(c p) t -> p c t", p=128))])
                for h2 in range(2):
                    for c in range(8):
                        k.mm(Y[:, h2 * 512:(h2 + 1) * 512], oT[:, c, :], Wout[:, c, h2 * 512:(h2 + 1) * 512],
                             start=(c == 0), stop=(c == 7))
                for h2 in range(2):
                    sl = slice(h2 * 512, (h2 + 1) * 512)
                    k.tt(tmp[:, sl], Y[:, sl], mods[2][kind][:, sl], ALU.mult)
                k.tt(xn_[:, :], xt[:, :], tmp[:, :], ALU.add)
                k.dma("pool", [(X[ti * 128:(ti + 1) * 128, :], xn_[:, :])])
                normmod(xn_[:, :], hf[:, :], mods[4][kind], mods[3][kind], tmp, ss, rs)
                k.cp(hr[:, 0:D], hf[:, :], en="act")
                k.cp(hr[:, D:D + 2].bitcast(I32), tokidx[:, ti:ti + 1])
                k.dma("pool", [(Hd[ti * 128:(ti + 1) * 128, :], hr[:, :])])
                for c in range(8):
                    k.tr(TP[:, c * 128:(c + 1) * 128], hf[:, c * 128:(c + 1) * 128], ident_f[:, :])
                k.cp(hTf[:, :, :], TP[:, :].rearrange("p (c t) -> p c t", t=128), en="act")
                for c in range(8):
                    k.mm(LG[:, 0:NE], hTf[:, c, :], wr[:, c, :], start=(c == 0), stop=(c == 7))
                k.red(mx[:, :], LG[:, 0:NE], op=ALU.max)
                k.ts(mx[:, :], mx[:, :], -1.0, None, ALU.mult)
                k.act(ex[:, :], LG[:, 0:NE], AF.Exp, bias=mx[:, 0:1], accum=sm[:, 0:1])
                k.recip(sm[:, :], sm[:, :])
                k.ts(AFF[:, ti, :], ex[:, :], sm[:, 0:1], None, ALU.mult)
            k.barrier()
        with ExitStack() as st:
            lo = k.sb(st, "lo", [128, 2 * NE], F32)
            tt_ = k.sb(st, "tthr", [128, 2 * NE], F32)
            ge = k.sb(st, "ge", [128, 2 * NE], F32)
            capv = k.sb(st, "capv", [128, 2 * NE], F32)
            cmp_ = k.sb(st, "cmp", [128, NT, NE], BF16)
            cntp = k.sb(st, "cntp", [128, 2 * NE], F32)
            base_t = k.sb(st, "base", [128, NT, NE], F32)
            pos = k.sb(st, "pos", [128, NT, NE], F32)
            off = k.sb(st, "off", [128, NT, NE], F32)
            val = k.sb(st, "val", [128, NT, NE], F32)
            pc = k.ps(st, "pc", [128, 512])
            ncolp = ((NT * NE + 511) // 512) * 512
            pw = k.ps(st, "pw", [128, ncolp])
            pt_ = k.ps(st, "ptot", [128, ncolp])
            k.dma("sp", [(capv[:, :], k_capv)])
            k.dma("sp", [(base_t[:, :, :], k_base.rearrange("p (t e) -> p t e", e=NE))])
            k.memset(lo[:, :], 0.0)
            sets = ((0, NTL, 0), (NTL, NT, NE))

            def compare(thr):
                for (a, b, o) in sets:
                    k.tt(cmp_[:, a:b, :], AFF[:, a:b, :], thr[:, o:o + NE].unsqueeze(1).broadcast_to([128, b - a, NE]), ALU.is_ge)

            step = 0.5
            for it in range(32):
                k.ts(tt_[:, :], lo[:, :], step, None, ALU.add)
                compare(tt_)
                for (a, b, o) in sets:
                    k.red(cntp[:, o:o + NE], cmp_[:, a:b, :].rearrange("p t e -> p e t"))
                k.mm(pc[:, 0:2 * NE], ones_f[:, :], cntp[:, :])
                k.tt(ge[:, :], pc[:, 0:2 * NE], capv[:, :], ALU.is_ge)
                k.stt(lo[:, :], ge[:, :], step, lo[:, :], ALU.mult, ALU.add)
                step *= 0.5
            compare(lo)
            cf = cmp_[:, :, :].rearrange("p t e -> p (t e)")
            ncol = NT * NE
            for c0 in range(0, ncol, 512):
                w = min(512, ncol - c0)
                k.mm(pw[:, c0:c0 + w], tri_b[:, :], cf[:, c0:c0 + w])
                k.mm(pt_[:, c0:c0 + w], ones_b[:, :], cf[:, c0:c0 + w])
            for (a, b, o) in sets:
                k.memset(off[:, a, :], 0.0)
                for t in range(a, b - 1):
                    k.tt(off[:, t + 1, :], off[:, t, :], pt_[:, t * NE:(t + 1) * NE], ALU.add)
            k.tt(pos[:, :, :], pw[:, 0:ncol].rearrange("p (t e) -> p t e", e=NE), off[:, :, :], ALU.add)
            for (a, b, o), cap in zip(sets, (capL, capC)):
                k.ts(val[:, a:b, :], pos[:, a:b, :], float(cap), None, ALU.is_lt)
            k.tt(val[:, :, :], val[:, :, :], cmp_[:, :, :], ALU.mult)
            k.tt(pos[:, :, :], pos[:, :, :], base_t[:, :, :], ALU.add)
            k.ts(pos[:, :, :], pos[:, :, :], -BIG, None, ALU.add)
            k.tt(pos[:, :, :], pos[:, :, :], val[:, :, :], ALU.mult)
            k.ts(pos[:, :, :], pos[:, :, :], BIG, None, ALU.add)
            k.cp(IDX[:, :, :], pos[:, :, :])
            k.barrier()
        with ExitStack() as st:
            hrow = [k.sb(st, "hrow", [128, D + 2], BF16) for _ in range(3)]
            for ti in range(NT):
                hr = hrow[ti % 3]
                k.dma("sp", [(hr[:, :], Hd[ti * 128:(ti + 1) * 128, :])])
                for e in range(NE):
                    k.idma(XS[:, :], hr[:, :], IDX[:, ti, e:e + 1], scatter=True, bounds=reg_xs)
            k.barrier()

    def experts(li):
        with ExitStack() as st:
            WG = [k.sb(st, "wg", [128, 8, D], BF16) for _ in range(2)]
            WU = [k.sb(st, "wu", [128, 8, D], BF16) for _ in range(2)]
            WD = [k.sb(st, "wd", [128, 8, D], BF16) for _ in range(2)]
            wrf = k.sb(st, "wrf", [128, 8, NE], F32)
            wrb = k.sb(st, "wrb", [128, 8, NE], BF16)
            k.dma("sp", [(wrf[:, :, :], w_router[li].rearrange("(c p) e -> p c e", p=128))])
            k.cp(wrb[:, :, :], wrf[:, :, :])
            mods = mod_tiles(st, li, [5])
            xsT = k.sb(st, "xsT", [128, 8, SLOTS], BF16)
            gT = k.sb(st, "gT", [128, 8, SLOTS], BF16)
            xst = [k.sb(st, "xst", [128, D + 2], BF16) for _ in range(3)]
            sa = [k.sb(st, "sa", [128, 512], F32) for _ in range(2)]
            pay = [k.sb(st, "pay", [128, D], F32) for _ in range(2)]
            mx = k.sb(st, "mx", [128, 1], F32); sm = k.sb(st, "sm", [128, 1], F32)
            ex = k.sb(st, "ex", [128, NE], F32)
            TPb = k.ps(st, "TPx", [128, 512])
            LG = k.ps(st, "LGx", [128, 512])
            PA = [k.ps(st, "PA", [128, 512]) for _ in range(2)]
            PB = [k.ps(st, "PB", [128, 512]) for _ in range(2)]
            PY = k.ps(st, "PY", [128, 1024])
            TPv = TPb[:, :].bitcast(BF16)
            stiles = []
            r = 0
            while r < capL:
                n = min(128, capL - r); stiles.append((r, n, 0)); r += n
            stiles.append((capL, capC, 1))
            nblk = (SLOTS + 511) // 512
            bw = (SLOTS + nblk - 1) // nblk
            blocks = [(b * bw, min(bw, SLOTS - b * bw)) for b in range(nblk)]
            cnt = 0
            pcnt = 0
            nst = len(stiles)
            tix = [k.sb(st, "tix", [128, 1], I32) for _ in range(2 * nst)]
            gate = [k.sb(st, "gate", [128, 1], F32) for _ in range(2 * nst)]
            def load_w(e_):
                for (w_sb, w_d) in ((WG[e_ % 2], w_eg), (WU[e_ % 2], w_eu), (WD[e_ % 2], w_ed)):
                    k.dma("pool", [(w_sb[:, c, :], w_d[li, e_, c * 128:(c + 1) * 128, :]) for c in range(8)], max_dma_last_dim=4096)

            load_w(0)
            for e in range(NE):
                wg, wu, wd = WG[e % 2], WU[e % 2], WD[e % 2]
                if e + 1 < NE:
                    load_w(e + 1)
                tinfo = []
                for si_, (r0, n, kind) in enumerate(stiles):
                    xs = xst[cnt % 3]; cnt += 1
                    tx = tix[(e % 2) * nst + si_]; gt = gate[(e % 2) * nst + si_]
                    k.dma("sp", [(xs[0:n, :], XS[e * SLOTS + r0:e * SLOTS + r0 + n, :])])
                    k.cp(tx[0:n, :], xs[0:n, D:D + 2].bitcast(I32))
                    for c in range(8):
                        k.tr(TPv[:, c * 128:c * 128 + n], xs[0:n, c * 128:(c + 1) * 128], ident_b[0:n, 0:n])
                    k.cp(xsT[:, :, r0:r0 + n], TPv.rearrange("p (c t) -> p c t", t=128)[:, :, 0:n], en="act")
                    for c in range(8):
                        k.mm(LG[0:n, 0:NE], xsT[:, c, r0:r0 + n], wrb[:, c, :], start=(c == 0), stop=(c == 7))
                    k.red(mx[0:n, :], LG[0:n, 0:NE], op=ALU.max)
                    k.ts(mx[0:n, :], mx[0:n, :], -1.0, None, ALU.mult)
                    k.act(ex[0:n, :], LG[0:n, 0:NE], AF.Exp, bias=mx[0:n, 0:1], accum=sm[0:n, 0:1])
                    k.recip(sm[0:n, :], sm[0:n, :])
                    k.tt(gt[0:n, :], ex[0:n, e:e + 1], sm[0:n, :], ALU.mult)
                    tinfo.append((r0, n, kind, tx, gt))
                for f in range(8):
                    for (b0, bn) in blocks:
                        pa = PA[pcnt % 2]; pb_ = PB[pcnt % 2]; sa_ = sa[pcnt % 2]; pcnt += 1
                        for c in range(8):
                            k.mm(pa[:, 0:bn], wg[:, c, f * 128:(f + 1) * 128], xsT[:, c, b0:b0 + bn], start=(c == 0), stop=(c == 7))
                        for c in range(8):
                            k.mm(pb_[:, 0:bn], wu[:, c, f * 128:(f + 1) * 128], xsT[:, c, b0:b0 + bn], start=(c == 0), stop=(c == 7))
                        k.act(sa_[:, 0:bn], pa[:, 0:bn], AF.Silu)
                        k.tt(gT[:, f, b0:b0 + bn], sa_[:, 0:bn], pb_[:, 0:bn], ALU.mult)
                for ti_, (r0, n, kind, tx, gt) in enumerate(tinfo):
                    py = pay[(e * len(tinfo) + ti_) % 2]
                    for h2 in range(2):
                        for f in range(8):
                            k.mm(PY[0:n, h2 * 512:(h2 + 1) * 512], gT[:, f, r0:r0 + n], wd[:, f, h2 * 512:(h2 + 1) * 512],
                                 start=(f == 0), stop=(f == 7))
                    for h2 in range(2):
                        sl = slice(h2 * 512, (h2 + 1) * 512)
                        k.stt(py[0:n, sl], PY[0:n, sl], gt[0:n, 0:1], mods[5][kind][0:n, sl], ALU.mult, ALU.mult)
                    k.idma(X[:, :], py[0:n, :], tx[0:n, 0:1], scatter=True, bounds=reg_x, add=True, xw=[xtok])
            k.barrier()

    def m1_odd(li, j):
        with ExitStack() as st:
            Win = k.sb(st, "wino", [128, 8, 416], BF16)
            Wq = k.sb(st, "wq", [128, 2, 1536], BF16)
            Wkv = k.sb(st, "wkv", [128, 2048], BF16)
            k.dma("pool", [(Win[:, c, :], w_in_odd[j, c * 128:(c + 1) * 128, :]) for c in range(8)], max_dma_last_dim=4096)
            k.dma("pool", [(Wq[:, c, :], w_q_up[j, c * 128:(c + 1) * 128, :]) for c in range(2)], max_dma_last_dim=4096)
            k.dma("pool", [(Wkv[:, :], w_kv_up[j])], max_dma_last_dim=4096)
            mods = mod_tiles(st, li, [0, 1])
            gq = k.sb(st, "gq", [128, 256], F32); gkv = k.sb(st, "gkv", [128, 128], F32)
            gqk = k.sb(st, "gqk", [128, 2, 96], F32)
            bc_load(gq[:, :], q_norm[j]); bc_load(gkv[:, :], kv_norm[j])
            bc_load(gqk[:, 0, :], qk_norm[j, 0]); bc_load(gqk[:, 1, :], qk_norm[j, 1])
            invn = k.sb(st, "invn", [128, 3], F32)
            k.memset(invn[:, 0:1], 1.0 / 256); k.memset(invn[:, 1:2], 1.0 / 128); k.memset(invn[:, 2:3], 1.0 / 32)
            xts = [k.sb(st, "xt", [128, D], F32) for _ in range(2)]
            tmp = k.sb(st, "tmp", [128, D], F32)
            ss = k.sb(st, "ss", [128, 1], F32); rs = k.sb(st, "rs", [128, 1], F32)
            hb = k.sb(st, "hb", [128, D], BF16)
            hT = k.sb(st, "hT", [128, 8, 128], BF16)
            sqp = k.sb(st, "sqp", [128, 416], F32)
            s3 = k.sb(st, "s3", [128, 3], F32)
            cn = k.sb(st, "cn", [128, 384], BF16)
            cT = k.sb(st, "cT", [128, 3, 128], BF16)
            krn = k.sb(st, "krn", [128, 32], F32)
            krr = k.sb(st, "krr", [128, 32], F32)
            kt1 = k.sb(st, "kt1", [128, 32], F32); kt2 = k.sb(st, "kt2", [128, 32], F32)
            sqq = k.sb(st, "sqq", [128, 2048], F32)
            sq16 = k.sb(st, "sq16", [128, 32], F32)
            qn_ = k.sb(st, "qn", [128, 16, 96], F32)
            qt1 = k.sb(st, "qt1", [128, 16, 32], F32); qt2 = k.sb(st, "qt2", [128, 16, 32], F32)
            qbf = k.sb(st, "qbf", [128, 16, 96], BF16)
            kbf = k.sb(st, "kbf", [128, 16, 96], BF16)
            cs = [k.sb(st, "cs", [128, 2, 32], F32) for _ in range(2)]
            vst = [k.sb(st, "vst", [128, 16, 64], BF16) for _ in range(2)]
            stq = [k.sb(st, "stq", [96, 16, 512], BF16) for _ in range(2)]
            stk = [k.sb(st, "stk", [96, 16, 512], BF16) for _ in range(2)]
            tpT = k.ps(st, "tpT", [128, 512]); P = k.ps(st, "P", [128, 512]); cTp = k.ps(st, "cTp", [128, 512])
            Q3 = k.ps(st, "Q3", [128, 1536]); KV = k.ps(st, "KV", [128, 1024])
            tpTb = tpT[:, :].bitcast(BF16); cTb = cTp[:, :].bitcast(BF16)
            Q3b = Q3[:, :].bitcast(BF16); KVb = KV[:, :].bitcast(BF16)

            def rope32(dst, src, c_, shp, t1, t2):
                G = shp
                k.tt(t1, src, c_[:, 0, :].unsqueeze(1).broadcast_to([128, G, 32]), ALU.mult)
                sv = c_[:, 1, :].rearrange("p (r h j) -> p r h j", r=2, h=2)
                s5 = src.rearrange("p g (r h j) -> p g r h j", r=2, h=2)
                t5 = t2.rearrange("p g (r h j) -> p g r h j", r=2, h=2)
                for r in range(2):
                    for h in range(2):
                        k.tt(t5[:, :, r, h, :], s5[:, :, r, 1 - h, :], sv[:, r, h, :].unsqueeze(1).broadcast_to([128, G, 8]), ALU.mult)
                k.tt(dst, t1, t2, ALU.add)

            for gi, tiles in enumerate(tok_groups()):
                sq_, sk_ = stq[gi % 2], stk[gi % 2]
                for jt, ti in enumerate(tiles):
                    kind = 0 if ti < NTL else 1
                    xt = xts[ti % 2]
                    k.dma("sp", [(xt[:, :], X[ti * 128:(ti + 1) * 128, :])])
                    c_ = cs[ti % 2]
                    if kind == 0:
                        k.dma("sp", [(c_[:, 0, :], k_cos32[ti * 128:(ti + 1) * 128, :]),
                                     (c_[:, 1, :], k_sin32[ti * 128:(ti + 1) * 128, :])])
                    normmod(xt[:, :], hb[:, :], mods[1][kind], mods[0][kind], tmp, ss, rs)
                    for c in range(8):
                        k.tr(tpTb[:, c * 128:(c + 1) * 128], hb[:, c * 128:(c + 1) * 128], ident_b[:, :])
                    k.cp(hT[:, :, :], tpTb.rearrange("p (c t) -> p c t", t=128), en="act")
                    for c in range(8):
                        k.mm(P[:, 0:416], hT[:, c, :], Win[:, c, :], start=(c == 0), stop=(c == 7))
                    k.act(sqp[:, :], P[:, 0:416], AF.Square)
                    k.red(s3[:, 0:1], sqp[:, 0:256]); k.red(s3[:, 1:2], sqp[:, 256:384]); k.red(s3[:, 2:3], sqp[:, 384:416])
                    k.tt(s3[:, :], s3[:, :], invn[:, :], ALU.mult)
                    k.rstd(s3[:, :], s3[:, :], 1.0)
                    k.stt(cn[:, 0:256], P[:, 0:256], s3[:, 0:1], gq[:, :], ALU.mult, ALU.mult)
                    k.stt(cn[:, 256:384], P[:, 256:384], s3[:, 1:2], gkv[:, :], ALU.mult, ALU.mult)
                    k.stt(krn[:, :], P[:, 384:416], s3[:, 2:3], gqk[:, 1, 64:96], ALU.mult, ALU.mult)
                    if kind == 0:
                        rope32(krr[:, :].unsqueeze(1), krn[:, :].unsqueeze(1), c_, 1, kt1[:, :].unsqueeze(1), kt2[:, :].unsqueeze(1))
                    else:
                        k.cp(krr[:, :], krn[:, :])
                    for c in range(3):
                        k.tr(cTb[:, c * 128:(c + 1) * 128], cn[:, c * 128:(c + 1) * 128], ident_b[:, :])
                    k.cp(cT[:, :, :], cTb[:, 0:384].rearrange("p (c t) -> p c t", t=128), en="act")
                    for n in range(3):
                        for c in range(2):
                            k.mm(Q3[:, n * 512:(n + 1) * 512], cT[:, c, :], Wq[:, c, n * 512:(n + 1) * 512], start=(c == 0), stop=(c == 1))
                    for n in range(3):
                        k.act(sqq[:, n * 512:(n + 1) * 512], Q3[:, n * 512:(n + 1) * 512], AF.Square)
                    sv3 = sqq[:, 0:1536].rearrange("p (h d) -> p h d", d=96)
                    q3v = Q3[:, :].rearrange("p (h d) -> p h d", d=96)
                    k.red(sq16[:, 0:16], sv3[:, :, 0:64]); k.red(sq16[:, 16:32], sv3[:, :, 64:96])
                    k.rstd(sq16[:, 0:16], sq16[:, 0:16], 1.0 / 64)
                    k.rstd(sq16[:, 16:32], sq16[:, 16:32], 1.0 / 32)
                    k.tt(qn_[:, :, 0:64], q3v[:, :, 0:64], sq16[:, 0:16].unsqueeze(2).broadcast_to([128, 16, 64]), ALU.mult)
                    k.tt(qn_[:, :, 64:96], q3v[:, :, 64:96], sq16[:, 16:32].unsqueeze(2).broadcast_to([128, 16, 32]), ALU.mult)
                    k.tt(qn_[:, :, :], qn_[:, :, :], gqk[:, 0, :].unsqueeze(1).broadcast_to([128, 16, 96]), ALU.mult)
                    k.cp(qbf[:, :, 0:64], qn_[:, :, 0:64])
                    if kind == 0:
                        rope32(qbf[:, :, 64:96], qn_[:, :, 64:96], c_, 16, qt1[:, :, :], qt2[:, :, :])
                    else:
                        k.cp(qbf[:, :, 64:96], qn_[:, :, 64:96])
                    for h in range(16):
                        k.tr(Q3b[0:96, h * 128:(h + 1) * 128], qbf[:, h, :], ident_b[:, :])
                    k.cp(sq_[:, :, jt * 128:(jt + 1) * 128], Q3b[0:96, 0:2048].rearrange("p (h t) -> p h t", t=128), en="act")
                    v_ = vst[ti % 2]
                    for hf_ in range(2):
                        for n in range(2):
                            c0 = hf_ * 1024 + n * 512
                            k.mm(KV[:, n * 512:(n + 1) * 512], cT[:, 2, :], Wkv[:, c0:c0 + 512])
                        kv3 = KV[:, :].rearrange("p (h d) -> p h d", d=128)
                        for n in range(2):
                            k.act(sqq[:, n * 512:(n + 1) * 512], KV[:, n * 512:(n + 1) * 512], AF.Square)
                        k.red(sq16[:, 0:8], sqq[:, 0:1024].rearrange("p (h d) -> p h d", d=128)[:, :, 0:64])
                        k.rstd(sq16[:, 0:8], sq16[:, 0:8], 1.0 / 64)
                        hs = slice(hf_ * 8, hf_ * 8 + 8)
                        k.tt(qn_[:, 0:8, 0:64], kv3[:, :, 0:64], sq16[:, 0:8].unsqueeze(2).broadcast_to([128, 8, 64]), ALU.mult)
                        k.tt(kbf[:, hs, 0:64], qn_[:, 0:8, 0:64], gqk[:, 1, 0:64].unsqueeze(1).broadcast_to([128, 8, 64]), ALU.mult)
                        k.cp(v_[:, hs, :], kv3[:, :, 64:128], en="act")
                    k.cp(kbf[:, :, 64:96], krr[:, :].unsqueeze(1).broadcast_to([128, 16, 32]))
                    k.dma("pool", [(Vd[ti * 128:(ti + 1) * 128, :], v_[:, :, :].rearrange("p h d -> p (h d)"))])
                    for h in range(16):
                        k.tr(KVb[0:96, h * 128:(h + 1) * 128], kbf[:, h, :], ident_b[:, :])
                    k.cp(sk_[:, :, jt * 128:(jt + 1) * 128], KVb[0:96, 0:2048].rearrange("p (h t) -> p h t", t=128), en="act")
                n = len(tiles) * 128
                t0 = tiles[0] * 128
                k.dma("pool", [(QTo[:, :, t0:t0 + n].rearrange("h p t -> p h t"), sq_[:, :, 0:n])])
                k.dma("pool", [(KTo[:, :, t0:t0 + n].rearrange("h p t -> p h t"), sk_[:, :, 0:n])])
            k.barrier()

    lst = ExitStack()
    AFF = k.sb(lst, "AFF", [128, NT, NE], F32)
    IDX = k.sb(lst, "IDX", [128, NT, NE], I32)
    k.barrier()
    upto = getattr(cfg, "upto", None)
    for li in range(L):
        j = li // 2
        if li % 2 == 0:
            m1_even(li, j)
            if upto == "m1" and li == L - 1:
                break
            attn_even(li, j)
            if upto == "attn" and li == L - 1:
                break
            mixout_ffnin(li, w_out_even[j], AFF, IDX)
        else:
            m1_odd(li, j)
            if upto == "m1" and li == L - 1:
                break
            attn_odd(li, j)
            if upto == "attn" and li == L - 1:
                break
            mixout_ffnin(li, w_out_odd[j], AFF, IDX)
        if upto == "ffnin" and li == L - 1:
            break
        experts(li)
    k.barrier()
    k.dma("sp", [(out_d, X[0:T, :])], sembuf=xtok)
    k.barrier()
    lst.close()
    gst.close()
    k.dbg = dict(X=X, MODS=MODS, QKT=QKT, Vd=Vd, OT=OT, Hd=Hd, XS=XS, QTo=QTo, KTo=KTo)
    return k


def host_consts(cfg):
    T, NT, NTL = cfg.T, cfg.NT, cfg.NTL
    c = {}
    c["k_ident"] = np.eye(128, dtype=np.float32)
    c["k_tri"] = np.triu(np.ones((128, 128), np.float32), 1)
    c["k_tokidx"] = (np.arange(NT, dtype=np.int32)[None, :] * 128 + np.arange(128, dtype=np.int32)[:, None]).astype(np.int32)
    t = np.arange(T)
    row = (t // GRID_W).astype(np.float32); col = (t % GRID_W).astype(np.float32)
    for rd, nm in ((64, "64"), (32, "32")):
        half = rd // 2
        fr = (ROPE_THETA ** (-np.arange(0, half, 2, dtype=np.float32) / half)).astype(np.float32)
        ar = (row[:, None] * fr[None, :]).astype(np.float32); ac = (col[:, None] * fr[None, :]).astype(np.float32)
        cr, sr, cc_, sc_ = np.cos(ar), np.sin(ar), np.cos(ac), np.sin(ac)
        c["k_cos" + nm] = np.concatenate([cr, cr, cc_, cc_], axis=1).astype(np.float32)
        c["k_sin" + nm] = np.concatenate([-sr, sr, -sc_, sc_], axis=1).astype(np.float32)
    base = np.zeros((128, NT, NE), np.float32)
    base[:, :, :] = (np.arange(NE, dtype=np.float32) * cfg.SLOTS)[None, None, :]
    base[:, NTL:, :] += cfg.capL
    c["k_base"] = base.reshape(128, NT * NE)
    capv = np.zeros((128, 2 * NE), np.float32); capv[:, :NE] = cfg.capL; capv[:, NE:] = cfg.capC
    c["k_capv"] = capv
    return c


def make_in_maps(cfg, inputs):
    consts = host_consts(cfg)
    B = inputs["x"].shape[0]
    wnames = ["w_ada", "b_ada", "norm_mix", "norm_ffn", "w_in_even", "a_qk_norm", "diff_lambda", "a_subln",
              "b_qk_norm", "w_out_even", "w_router", "w_exp_gate", "w_exp_up", "w_exp_down"]
    if cfg.n_odd:
        wnames += ["w_in_odd", "mla_q_norm", "w_q_up", "mla_kv_norm", "w_kv_up", "mla_qk_norm", "w_out_odd"]
    shared = {n: np.ascontiguousarray(np.asarray(inputs[n], dtype=np.float32)) for n in wnames}
    shared.update(consts)
    maps = []
    cctx = np.asarray(inputs["c_ctx"], np.float32)
    for b in range(B):
        m = dict(shared)
        m["x"] = np.ascontiguousarray(np.asarray(inputs["x"][b], np.float32))
        m["ctx"] = np.ascontiguousarray(np.asarray(inputs["ctx"][b], np.float32))
        cb = np.asarray(inputs["c"][b], np.float32)
        m["cc"] = np.ascontiguousarray(np.stack([cb.reshape(8, 128).T, cctx.reshape(8, 128).T], axis=-1))
        maps.append(m)
    return maps


def kernel(**inputs):
    cfg = Cfg()
    kb = build(cfg)
    maps = make_in_maps(cfg, inputs)
    res = run_bass_kernel_spmd(kb.nc, maps, core_ids=list(range(len(maps))))
    return np.stack([np.asarray(r["out"], dtype=np.float32) for r in res.results], axis=0)
```
